# Optimizing a Trainium2 kernel written in Bass

```python
import math
import jax, jax.numpy as jnp
from jax import lax
import numpy as np


D_MODEL = 1024
BATCH = 4
SEQ = 8192
DEPTH = 1

CHUNK = 64
EPS = 1e-6
D_MIX = D_MODEL
D_S5 = D_MIX // 2
S5_GROUP = 16
S5_GROUPS = D_S5 // S5_GROUP
S5_STATE = 64
DT_MIN = 1e-3
DT_MAX = 1e-1
D_RET = D_MIX - D_S5
RET_HEADS = 8
RET_HEAD_DIM = D_RET // RET_HEADS
ROPE_BASE = 10000.0
D_IN = D_S5 + 4 * D_RET
N_GROUPS = 4
EXPERTS_PER_GROUP = 8
N_EXPERTS = N_GROUPS * EXPERTS_PER_GROUP
TOP_K = 2
D_EXPERT = 512
MOE_BLOCK = 128

kernel_name = 'hymba_s5_retention_hiermoe_block'


def rms_norm(x, w):
    xf = x.astype(jnp.float32)
    y = xf * lax.rsqrt(jnp.mean(xf * xf, axis=-1, keepdims=True) + EPS)
    return (y * w.astype(jnp.float32)).astype(x.dtype)


def _cmul_scan_op(e1, e2):
    a1r, a1i, b1r, b1i = e1
    a2r, a2i, b2r, b2i = e2
    return (a1r * a2r - a1i * a2i,
            a1r * a2i + a1i * a2r,
            a2r * b1r - a2i * b1i + b2r,
            a2r * b1i + a2i * b1r + b2i)


def s5_mixer(u, a_re, a_im, b_re, b_im, c_re, c_im, d_skip, log_dt, w_glu, b_glu):
    bsz, seq, _ = u.shape
    n_chunks = seq // CHUNK
    f32 = jnp.float32
    lam_r = a_re.astype(f32)
    lam_i = a_im.astype(f32)
    dt = jnp.exp(log_dt.astype(f32))[:, None]
    mag = jnp.exp(lam_r * dt)
    ab_r = mag * jnp.cos(lam_i * dt)
    ab_i = mag * jnp.sin(lam_i * dt)
    den = lam_r * lam_r + lam_i * lam_i
    zr = ((ab_r - 1.0) * lam_r + ab_i * lam_i) / den
    zi = (ab_i * lam_r - (ab_r - 1.0) * lam_i) / den
    br_, bi_ = b_re.astype(f32), b_im.astype(f32)
    bb_r = zr[..., None] * br_ - zi[..., None] * bi_
    bb_i = zr[..., None] * bi_ + zi[..., None] * br_
    cr, ci = c_re.astype(f32), c_im.astype(f32)
    dd = d_skip.astype(f32).reshape(S5_GROUPS, S5_GROUP)
    a_seq_r = jnp.broadcast_to(ab_r, (CHUNK, 1, S5_GROUPS, S5_STATE))
    a_seq_i = jnp.broadcast_to(ab_i, (CHUNK, 1, S5_GROUPS, S5_STATE))
    uc = u.astype(f32).reshape(bsz, n_chunks, CHUNK, S5_GROUPS, S5_GROUP).transpose(1, 2, 0, 3, 4)

    def step(carry, u_blk):
        s0r, s0i = carry
        bu_r = jnp.einsum('tbgh,gph->tbgp', u_blk, bb_r)
        bu_i = jnp.einsum('tbgh,gph->tbgp', u_blk, bb_i)
        acr, aci, sr, si = lax.associative_scan(
            _cmul_scan_op, (a_seq_r, a_seq_i, bu_r, bu_i), axis=0)
        sr = sr + acr * s0r - aci * s0i
        si = si + acr * s0i + aci * s0r
        y = (jnp.einsum('tbgp,ghp->tbgh', sr, cr)
             - jnp.einsum('tbgp,ghp->tbgh', si, ci)
             + dd * u_blk)
        return (sr[-1], si[-1]), y

    s_init = jnp.zeros((bsz, S5_GROUPS, S5_STATE), f32)
    _, ys = lax.scan(step, (s_init, s_init), uc)
    y = ys.transpose(2, 0, 1, 3, 4).reshape(bsz, seq, D_S5)
    y = jax.nn.gelu(y)
    y = y * jax.nn.sigmoid(y @ w_glu.astype(f32) + b_glu.astype(f32))
    return y.astype(u.dtype)


def rotary(x):
    seq = x.shape[1]
    half = RET_HEAD_DIM // 2
    inv = ROPE_BASE ** (-jnp.arange(half, dtype=jnp.float32) / half)
    ang = jnp.arange(seq, dtype=jnp.float32)[:, None] * inv[None, :]
    cos = jnp.cos(ang)[None, :, None, :]
    sin = jnp.sin(ang)[None, :, None, :]
    xf = x.astype(jnp.float32)
    x1, x2 = xf[..., :half], xf[..., half:]
    return jnp.concatenate([x1 * cos - x2 * sin, x1 * sin + x2 * cos], axis=-1).astype(x.dtype)


def retention_mixer(q, k, v, g, norm_w):
    bsz, seq, _ = q.shape
    n_chunks = seq // CHUNK
    f32 = jnp.float32
    shp = (bsz, seq, RET_HEADS, RET_HEAD_DIM)
    q = rotary(q.reshape(shp))
    k = rotary(k.reshape(shp)) * (RET_HEAD_DIM ** -0.5)
    v = v.reshape(shp)
    cshp = (bsz, n_chunks, CHUNK, RET_HEADS, RET_HEAD_DIM)
    qc, kc, vc = q.reshape(cshp), k.reshape(cshp), v.reshape(cshp)
    log_gamma = jnp.log(1.0 - 2.0 ** (-5.0 - jnp.arange(RET_HEADS, dtype=f32)))
    t = jnp.arange(CHUNK, dtype=f32)
    diff = t[:, None] - t[None, :]
    decay = jnp.where(diff >= 0, jnp.exp(log_gamma[:, None, None] * jnp.maximum(diff, 0.0)), 0.0)
    scores = jnp.einsum('bcthd,bcshd->bchts', qc, kc) * decay
    inner = jnp.einsum('bchts,bcshe->bcthe', scores, vc)
    k_decay = jnp.exp(log_gamma[:, None] * (CHUNK - 1 - t)[None, :])
    kv = jnp.einsum('bcshd,bcshe,hs->cbhde', kc, vc, k_decay)
    chunk_decay = jnp.exp(log_gamma * CHUNK)[None, :, None, None]

    def step(state, kv_c):
        return state * chunk_decay + kv_c, state

    _, r_prev = lax.scan(step, jnp.zeros((bsz, RET_HEADS, RET_HEAD_DIM, RET_HEAD_DIM), f32), kv)
    q_decay = jnp.exp(log_gamma[:, None] * (t + 1.0)[None, :])
    cross = jnp.einsum('bcthd,cbhde,ht->bcthe', qc, r_prev, q_decay)
    o = (inner + cross).astype(f32).reshape(shp)
    mu = jnp.mean(o, axis=-1, keepdims=True)
    var = jnp.mean(jnp.square(o - mu), axis=-1, keepdims=True)
    o = (o - mu) * lax.rsqrt(var + EPS) * norm_w.astype(f32).reshape(RET_HEADS, RET_HEAD_DIM)
    o = o.reshape(bsz, seq, D_RET)
    return (jax.nn.silu(g.astype(f32)) * o).astype(q.dtype)


def hier_moe(hn, rg_w, rg_b, re_w, re_b, w_gate, w_up, w_down):
    bsz, seq, d = hn.shape
    n_tok = bsz * seq
    f32 = jnp.float32
    xf = hn.reshape(n_tok, d)
    g_logits = (xf @ rg_w + rg_b).astype(f32)
    g_prob = jax.nn.softmax(g_logits, axis=-1)
    g_idx = jnp.argmax(g_logits, axis=-1).astype(jnp.int32)
    g_w = jnp.take_along_axis(g_prob, g_idx[:, None], axis=1)[:, 0]
    e_logits = (xf @ re_w + re_b).astype(f32).reshape(n_tok, N_GROUPS, EXPERTS_PER_GROUP)
    e_sel = jnp.take_along_axis(e_logits, g_idx[:, None, None], axis=1)[:, 0]
    top_v, top_i = lax.top_k(e_sel, TOP_K)
    top_w = jax.nn.softmax(top_v, axis=-1) * g_w[:, None]
    eid = (g_idx[:, None] * EXPERTS_PER_GROUP + top_i).reshape(-1).astype(jnp.int32)
    tok = jnp.repeat(jnp.arange(n_tok, dtype=jnp.int32), TOP_K)
    wt = top_w.reshape(-1)
    n_asg = n_tok * TOP_K
    order = jnp.argsort(eid)
    s_e, s_tok, s_w = eid[order], tok[order], wt[order]
    counts = jnp.bincount(eid, length=N_EXPERTS)
    starts = jnp.cumsum(counts) - counts
    padded = (counts + MOE_BLOCK - 1) // MOE_BLOCK * MOE_BLOCK
    pends = jnp.cumsum(padded)
    pstarts = pends - padded
    dest = pstarts[s_e] + jnp.arange(n_asg, dtype=jnp.int32) - starts[s_e]
    n_blocks = -(-n_asg // MOE_BLOCK) + N_EXPERTS
    buf_len = n_blocks * MOE_BLOCK
    buf_tok = jnp.zeros((buf_len,), jnp.int32).at[dest].set(s_tok)
    buf_w = jnp.zeros((buf_len,), hn.dtype).at[dest].set(s_w.astype(hn.dtype))
    block_start = jnp.arange(n_blocks, dtype=jnp.int32) * MOE_BLOCK
    block_e = jnp.minimum(jnp.searchsorted(pends, block_start, side='right'), N_EXPERTS - 1)

    def expert_block(args):
        tok_b, w_b, e = args
        xb = xf[tok_b]
        hid = jax.nn.silu(xb @ w_gate[e]) * (xb @ w_up[e])
        return ((hid @ w_down[e]) * w_b[:, None]).astype(hn.dtype)

    ys = lax.map(expert_block, (buf_tok.reshape(n_blocks, MOE_BLOCK),
                                buf_w.reshape(n_blocks, MOE_BLOCK), block_e))
    out = jnp.zeros((n_tok, d), hn.dtype).at[buf_tok].add(ys.reshape(buf_len, d))
    return out.reshape(bsz, seq, d)


def setup_inputs(seed: int = 0) -> dict:
    key = jax.random.key(seed)
    ks = jax.random.split(key, 24)
    f32 = jnp.float32

    def nrm(k, shape, scale):
        return jax.random.normal(k, shape, f32) * scale

    L_ = DEPTH
    x = jax.random.normal(ks[0], (BATCH, SEQ, D_MODEL), f32)
    norm1_w = 1.0 + nrm(ks[1], (L_, D_MODEL), 0.01)
    w_in = nrm(ks[2], (L_, D_MODEL, D_IN), D_MODEL ** -0.5)
    s5_a_re = -0.5 + nrm(ks[3], (L_, S5_GROUPS, S5_STATE), 0.01)
    s5_a_im = (math.pi * jnp.arange(S5_STATE, dtype=f32))[None, None, :] + nrm(ks[4], (L_, S5_GROUPS, S5_STATE), 0.01)
    s5_b_re = nrm(ks[5], (L_, S5_GROUPS, S5_STATE, S5_GROUP), (2.0 * S5_GROUP) ** -0.5)
    s5_b_im = nrm(ks[6], (L_, S5_GROUPS, S5_STATE, S5_GROUP), (2.0 * S5_GROUP) ** -0.5)
    s5_c_re = nrm(ks[7], (L_, S5_GROUPS, S5_GROUP, S5_STATE), S5_STATE ** -0.5)
    s5_c_im = nrm(ks[8], (L_, S5_GROUPS, S5_GROUP, S5_STATE), S5_STATE ** -0.5)
    s5_d = nrm(ks[9], (L_, D_S5), 1.0)
    s5_log_dt = math.log(DT_MIN) + jax.random.uniform(ks[10], (L_, S5_GROUPS), f32) * (math.log(DT_MAX) - math.log(DT_MIN))
    s5_w_glu = nrm(ks[11], (L_, D_S5, D_S5), D_S5 ** -0.5)
    s5_b_glu = nrm(ks[12], (L_, D_S5), 0.01)
    ret_norm_w = 1.0 + nrm(ks[13], (L_, D_RET), 0.01)
    w_out = nrm(ks[14], (L_, D_MIX, D_MODEL), D_MIX ** -0.5)
    norm2_w = 1.0 + nrm(ks[15], (L_, D_MODEL), 0.01)
    router_group_w = nrm(ks[16], (L_, D_MODEL, N_GROUPS), D_MODEL ** -0.5)
    router_group_b = nrm(ks[17], (L_, N_GROUPS), 0.01)
    router_expert_w = nrm(ks[18], (L_, D_MODEL, N_EXPERTS), D_MODEL ** -0.5)
    router_expert_b = nrm(ks[19], (L_, N_EXPERTS), 0.01)
    moe_w_gate = nrm(ks[20], (L_, N_EXPERTS, D_MODEL, D_EXPERT), D_MODEL ** -0.5)
    moe_w_up = nrm(ks[21], (L_, N_EXPERTS, D_MODEL, D_EXPERT), D_MODEL ** -0.5)
    moe_w_down = nrm(ks[22], (L_, N_EXPERTS, D_EXPERT, D_MODEL), D_EXPERT ** -0.5)
    final_norm_w = 1.0 + nrm(ks[23], (D_MODEL,), 0.01)
    return {'x': x, 'norm1_w': norm1_w, 'w_in': w_in,
            's5_a_re': s5_a_re, 's5_a_im': s5_a_im, 's5_b_re': s5_b_re, 's5_b_im': s5_b_im,
            's5_c_re': s5_c_re, 's5_c_im': s5_c_im, 's5_d': s5_d, 's5_log_dt': s5_log_dt,
            's5_w_glu': s5_w_glu, 's5_b_glu': s5_b_glu, 'ret_norm_w': ret_norm_w,
            'w_out': w_out, 'norm2_w': norm2_w,
            'router_group_w': router_group_w, 'router_group_b': router_group_b,
            'router_expert_w': router_expert_w, 'router_expert_b': router_expert_b,
            'moe_w_gate': moe_w_gate, 'moe_w_up': moe_w_up, 'moe_w_down': moe_w_down,
            'final_norm_w': final_norm_w}


def reference(x, norm1_w, w_in, s5_a_re, s5_a_im, s5_b_re, s5_b_im, s5_c_re, s5_c_im,
              s5_d, s5_log_dt, s5_w_glu, s5_b_glu, ret_norm_w, w_out, norm2_w,
              router_group_w, router_group_b, router_expert_w, router_expert_b,
              moe_w_gate, moe_w_up, moe_w_down, final_norm_w):
    h = x
    for l in range(DEPTH):
        u = rms_norm(h, norm1_w[l]) @ w_in[l]
        u_s5 = u[..., :D_S5]
        q = u[..., D_S5:D_S5 + D_RET]
        k = u[..., D_S5 + D_RET:D_S5 + 2 * D_RET]
        v = u[..., D_S5 + 2 * D_RET:D_S5 + 3 * D_RET]
        g = u[..., D_S5 + 3 * D_RET:]
        y_s5 = s5_mixer(u_s5, s5_a_re[l], s5_a_im[l], s5_b_re[l], s5_b_im[l],
                        s5_c_re[l], s5_c_im[l], s5_d[l], s5_log_dt[l], s5_w_glu[l], s5_b_glu[l])
        y_ret = retention_mixer(q, k, v, g, ret_norm_w[l])
        h = h + jnp.concatenate([y_s5, y_ret], axis=-1) @ w_out[l]
        h = h + hier_moe(rms_norm(h, norm2_w[l]), router_group_w[l], router_group_b[l],
                         router_expert_w[l], router_expert_b[l],
                         moe_w_gate[l], moe_w_up[l], moe_w_down[l])
    return rms_norm(h, final_norm_w)
```

```python
import math
from contextlib import ExitStack

import numpy as np
import concourse.bass as bass
import concourse.mybir as mybir
from concourse.bass_utils import run_bass_kernel_spmd

F32 = mybir.dt.float32
BF16 = mybir.dt.bfloat16
I32 = mybir.dt.int32
U32 = mybir.dt.uint32
ALU = mybir.AluOpType
AF = mybir.ActivationFunctionType
AX = mybir.AxisListType

NCORES = 8
TOK = 4096
NT = TOK // 128
D = 1024
NSUB = TOK // 8
CAP = 384
NEXP = 32
EPS = 1e-6
TWO_PI = 2.0 * math.pi
HS_STEPS = 9

ENGS = ("pe", "act", "dve", "pool", "sp")
NDMA = 16


class Prog:
    def __init__(self, nc, stack):
        self.nc = nc
        self.sem = {e: stack.enter_context(nc.semaphore("s_" + e)) for e in ENGS}
        self.dsem = {q: [stack.enter_context(nc.semaphore(f"d_{q}{i}")) for i in range(NDMA)]
                     for q in ("sp", "pool", "act")}
        self.cnt = {e: 0 for e in ENGS}
        self.dcnt = {q: 0 for q in self.dsem}
        self.phase = 0
        self.reset_phase()

    def reset_phase(self):
        self.phase += 1
        self.ops = []
        self.lw = {}
        self.rd = {}

    def op(self, eng, fn, reads=(), writes=(), dma=False):
        deps = set()
        raw = set()
        for r in reads:
            if r in self.lw:
                deps.add(self.lw[r])
                raw.add(self.lw[r])
        for w in writes:
            if w in self.lw:
                deps.add(self.lw[w])
            deps.update(self.rd.get(w, ()))
        idx = len(self.ops)
        if not dma:
            deps = {d for d in deps if d in raw or self.ops[d]["eng"] != eng or self.ops[d]["dma"]}
        self.ops.append(dict(eng=eng, fn=fn, deps=deps, dma=dma, need=False))
        for r in reads:
            self.rd.setdefault(r, []).append(idx)
        for w in writes:
            self.lw[w] = idx
            self.rd[w] = []
        return idx

    def emit(self):
        nc = self.nc
        ops = self.ops
        for o in ops:
            if o["eng"] == "pe":
                o["deps"] = {d for d in o["deps"] if ops[d]["eng"] != "pe"}
            for d in o["deps"]:
                ops[d]["need"] = True
        for o in ops:
            e = o["eng"]
            if o["dma"]:
                i = self.dcnt[e]
                self.dcnt[e] += 1
                o["sem"] = self.dsem[e][i % NDMA]
                o["val"] = 16 * (i // NDMA + 1)
                o["prev"] = 16 * (i // NDMA)
            elif o["need"]:
                self.cnt[e] += 1
                o["sem"] = self.sem[e]
                o["val"] = self.cnt[e]
        per = {e: [o for o in ops if o["eng"] == e] for e in ENGS}
        sem_final = [(self.sem[e], self.cnt[e]) for e in ENGS]
        for q in self.dsem:
            n = self.dcnt[q]
            for i in range(NDMA):
                if n > i:
                    sem_final.append((self.dsem[q][i], 16 * ((n - 1 - i) // NDMA + 1)))

        def run(engname, engobj):
            waited = {}

            def w(sem, val):
                if val <= 0:
                    return
                k = id(sem)
                if waited.get(k, 0) >= val:
                    return
                engobj.wait_ge(sem, val)
                waited[k] = val

            for o in per[engname]:
                for d in sorted(o["deps"]):
                    po = ops[d]
                    w(po["sem"], po["val"])
                if o["dma"]:
                    w(o["sem"], o["prev"])
                ins = o["fn"](engobj)
                if o["dma"]:
                    ins.then_inc(o["sem"], 16)
                elif o["need"]:
                    ins.then_inc(o["sem"], 1)
            for sem, val in sem_final:
                w(sem, val)

        with nc.Block() as block:
            @block.tensor
            def _(e):
                run("pe", e)

            @block.scalar
            def _(e):
                run("act", e)

            @block.vector
            def _(e):
                run("dve", e)

            @block.gpsimd
            def _(e):
                run("pool", e)

            @block.sync
            def _(e):
                run("sp", e)
        self.reset_phase()


def host_consts(half):
    c = {}
    c["identf"] = np.eye(128, dtype=np.float32)
    inv = np.power(np.float32(10000.0), -(np.arange(32, dtype=np.float32) / np.float32(32))).astype(np.float32)

    def rot(pos0, scale):
        pos = (pos0 + np.arange(TOK)).astype(np.float32)
        ang = (pos[:, None] * inv[None, :]).astype(np.float32).astype(np.float64)
        t = np.concatenate([np.cos(ang), np.sin(ang)], axis=1) * scale
        return np.ascontiguousarray(t.reshape(NT, 128, 64).transpose(1, 0, 2)).astype(np.float32)

    c["rot_q"] = rot(half * TOK, 1.0)
    c["rot_k"] = rot(half * TOK, 0.125)
    c["rot_kpre"] = rot(0, 0.125)
    gam = 1.0 - 2.0 ** (-5.0 - np.arange(8, dtype=np.float64))
    t = np.arange(128, dtype=np.float64)
    diff = t[None, :] - t[:, None]
    decT = np.where(diff[:, None, :] >= 0, gam[None, :, None] ** np.maximum(diff, 0)[:, None, :], 0.0)
    c["decT"] = decT.astype(np.float32)
    qd = np.zeros((128, 4, 128))
    g128 = np.zeros((128, 4))
    for j in range(4):
        for hl in range(2):
            qd[64 * hl:64 * hl + 64, j, :] = gam[2 * j + hl] ** (t + 1.0)[None, :]
            g128[64 * hl:64 * hl + 64, j] = gam[2 * j + hl] ** 128.0
    c["qdec"] = qd.astype(np.float32)
    c["gam128"] = g128.astype(np.float32)
    kd = np.zeros((128, 8, 64))
    for h in range(8):
        kd[:, h, :] = (gam[h] ** (127.0 - t))[:, None]
    c["kdec"] = kd.reshape(128, 512).astype(np.float32)
    bm = np.zeros((128, 128), np.float32)
    for b in range(8):
        bm[16 * b:16 * b + 16, 16 * b:16 * b + 16] = 1.0
    c["bmask"] = bm
    c["triu"] = np.triu(np.ones((128, 128), np.float32), 1)
    c["ecap"] = np.tile((np.arange(NEXP, dtype=np.float32) * CAP)[None, :], (128, 1))
    klist = list(range(9)) + [8 * 2 ** s_ for s_ in range(1, 9)]
    c["kvals"] = np.tile(np.asarray(klist, np.float32)[None, :, None], (128, 1, 16))
    return c


CONST_SHAPES = {
    "identf": [128, 128], "rot_q": [128, NT, 64], "rot_k": [128, NT, 64], "rot_kpre": [128, NT, 64],
    "decT": [128, 8, 128], "qdec": [128, 4, 128], "gam128": [128, 4], "kdec": [128, 512],
    "bmask": [128, 128], "triu": [128, 128], "ecap": [128, NEXP], "kvals": [128, 17, 16],
}

PARAM_SHAPES = {
    "norm1_w": [D], "w_in": [D, 2560], "s5_a_re": [32, 64], "s5_a_im": [32, 64],
    "s5_b_re": [32, 64, 16], "s5_b_im": [32, 64, 16], "s5_c_re": [32, 16, 64], "s5_c_im": [32, 16, 64],
    "s5_d": [512], "s5_log_dt": [32], "s5_w_glu": [512, 512], "s5_b_glu": [512], "ret_norm_w": [512],
    "w_out": [D, D], "norm2_w": [D], "router_group_w": [D, 4], "router_group_b": [4],
    "router_expert_w": [D, 32], "router_expert_b": [32],
    "moe_w_gate": [NEXP, D, 512], "moe_w_up": [NEXP, D, 512], "moe_w_down": [NEXP, 512, D],
    "final_norm_w": [D],
}


def build_nc(dbg=None, ntile=NT, skip_p1=False, cut=None, p2tiles=None):
    nc = bass.Bass("TRN2", target_bir_lowering=False)
    I = {}
    I["x"] = nc.dram_tensor("x", [TOK, D], F32, kind="ExternalInput").ap()
    I["xpre"] = nc.dram_tensor("xpre", [TOK, D], F32, kind="ExternalInput").ap()
    for k, s in PARAM_SHAPES.items():
        I[k] = nc.dram_tensor(k, s, F32, kind="ExternalInput").ap()
    for k, s in CONST_SHAPES.items():
        I[k] = nc.dram_tensor(k, s, F32, kind="ExternalInput").ap()
    out = nc.dram_tensor("out", [TOK, D], F32, kind="ExternalOutput").ap()
    dbg_out = None
    if dbg == "s5":
        dbg_out = nc.dram_tensor("dbg", [128, 4, TOK], BF16, kind="ExternalOutput").ap()
    if dbg == "h":
        dbg_out = nc.dram_tensor("dbg", [TOK, D], F32, kind="ExternalOutput").ap()
    d_inj = nc.dram_tensor("d_inj", [128, 4 * 2 * 8 * 128], BF16, kind="Internal").ap()
    d_fir = nc.dram_tensor("d_fir", [128, 4 * 8 * 128], BF16, kind="Internal").ap()
    d_nout = nc.dram_tensor("d_nout", [128, 8 * 2 * 16 * 64], BF16, kind="Internal").ap()
    d_xbuf = nc.dram_tensor("d_xbuf", [NEXP * CAP, D], BF16, kind="Internal").ap()
    d_ybuf = nc.dram_tensor("d_ybuf", [NEXP * CAP, D], BF16, kind="Internal").ap()
    d_hbuf = nc.dram_tensor("d_hbuf", [TOK, D], F32, kind="Internal").ap()

    with ExitStack() as gst:
        P = Prog(nc, gst)

        uid = [0]
        regcache = {}

        def bc_reg(e):
            if P.phase not in regcache:
                regcache[P.phase] = e.to_reg(NEXP * CAP - 1)
            return regcache[P.phase]

        def mk(stack):
            uid[0] += 1
            u = uid[0]
            sb = lambda n, s, d=F32: stack.enter_context(nc.sbuf_tensor(f"t{u}_{n}", list(s), d))
            ps = lambda n, s, d=F32: stack.enter_context(nc.psum_tensor(f"q{u}_{n}", list(s), d))
            return sb, ps

        gsb, _ = mk(gst)
        identf = gsb("identf", [128, 128])
        identb = gsb("identb", [128, 128], BF16)
        coef = gsb("coef", [128, HS_STEPS, 3, 16])
        S0 = gsb("S0", [128, 2, 16])
        R = gsb("R", [128, 4, 64])
        Rb = gsb("Rb", [128, 4, 128], BF16)
        gam128 = gsb("gam128", [128, 4])
        mhalf = gsb("mhalf", [128, 1])
        bglu = gsb("bglu", [128, 4])
        ss_dummy = gsb("ss_dummy", [128, 1])
        gates = gsb("gates", [128, NT, 2])
        dests = gsb("dests", [128, NT, 2], I32)

        P.op("sp", lambda e: e.dma_start(out=identf[:], in_=I["identf"]), writes=["identf"], dma=True)
        P.op("sp", lambda e: e.dma_start(out=gam128[:], in_=I["gam128"]), writes=["gam128"], dma=True)
        P.op("sp", lambda e: e.dma_start(out=bglu[:], in_=I["s5_b_glu"].rearrange("(c p) -> p c", p=128),
                                         allow_slow_non_contiguous=True), writes=["bglu"], dma=True)
        P.op("dve", lambda e: e.tensor_copy(out=identb[:], in_=identf[:]), reads=["identf"], writes=["identb"])
        P.op("pool", lambda e: e.memset(mhalf[:], -0.5), writes=["mhalf"])
        P.op("pool", lambda e: e.memset(R[:], 0.0), writes=["R"])
        P.op("pool", lambda e: e.memset(Rb[:], 0.0), writes=["Rb"])

        with ExitStack() as st:
            sb, ps = mk(st)
            lr = sb("lr", [128, 16]); li = sb("li", [128, 16]); ldt = sb("ldt", [128, 16])
            dt_ = sb("dt_", [128, 16]); ph = sb("ph", [128, 16]); lrdt = sb("lrdt", [128, 16])
            NPW = 9 + 8
            pw = sb("pw", [128, NPW, 2, 16])
            t1 = sb("t1", [128, 16]); t2 = sb("t2", [128, 16]); t3 = sb("t3", [128, 16]); t4 = sb("t4", [128, 16])
            ti = sb("ti", [128, 16], I32)
            mag = sb("mag", [128, 16]); sc = sb("sc", [128, 2, 16])
            br = sb("br", [128, 16, 16]); bi = sb("bi", [128, 16, 16])
            cr = sb("cr", [128, 16, 16]); ci = sb("ci", [128, 16, 16])
            bbr = sb("bbr", [128, 16, 16]); bbi = sb("bbi", [128, 16, 16])
            u1 = sb("u1", [128, 16, 16]); u2 = sb("u2", [128, 16, 16])
            zr = sb("zr", [128, 16]); zi = sb("zi", [128, 16])
            XP = [[sb(f"XP{a}{b}", [128, 16, 2, 16]) for b in range(2)] for a in range(2)]
            YP = [sb(f"YP{b}", [128, 16, 2, 16]) for b in range(2)]
            kvals = sb("kvals", [128, NPW, 16]); A0 = sb("A0", [128, NPW, 16]); M0 = sb("M0", [128, NPW, 16])
            A1_ = sb("A1_", [128, NPW, 16]); A2_ = sb("A2_", [128, NPW, 16]); Ai = sb("Ai", [128, NPW, 16], I32)
            Yall = sb("Yall", [128, 9, 2, 16, 16]); Xall = sb("Xall", [128, 8, 2, 16, 16])
            w1 = sb("w1", [128, 9, 16, 16]); w2 = sb("w2", [128, 9, 16, 16])
            INJ = sb("INJ", [128, 4, 2, 8, 128], BF16)
            FIR = sb("FIR", [128, 4, 8, 128], BF16)
            NOUT = sb("NOUT", [128, 8, 2, 8, 2, 2, 2, 16], BF16)
            bmask = sb("bmask", [128, 128]); dcol = sb("dcol", [128, 4])
            tmpF = sb("tmpF", [128, 4, 128])
            pI = ps("pI", [128, 4, 128]); pF = ps("pF", [128, 4, 128]); pCT = ps("pCT", [128, 4, 128])
            Cins = {"cr": sb("Cinr", [128, 4, 128]), "ci": sb("Cini", [128, 4, 128])}

            nc_allow = nc.allow_non_contiguous_dma(reason="tiny parameter re-layouts")
            st.enter_context(nc_allow)
            P.op("sp", lambda e: e.dma_start(out=lr[:], in_=I["s5_a_re"].rearrange("(a b) p -> (b p) a", b=2)), writes=["lr"], dma=True)
            P.op("sp", lambda e: e.dma_start(out=li[:], in_=I["s5_a_im"].rearrange("(a b) p -> (b p) a", b=2)), writes=["li"], dma=True)
            for e2 in range(2):
                P.op("sp", lambda e, e2=e2: e.dma_start(out=ldt[64 * e2:64 * e2 + 64, :],
                                                        in_=I["s5_log_dt"].rearrange("(a b) -> b a", b=2)[e2, :].partition_broadcast(64)),
                     writes=["ldt"], dma=True)
            P.op("sp", lambda e: e.dma_start(out=br[:], in_=I["s5_b_re"].rearrange("(a b) p j -> (b p) a j", b=2)), writes=["br"], dma=True)
            P.op("sp", lambda e: e.dma_start(out=bi[:], in_=I["s5_b_im"].rearrange("(a b) p j -> (b p) a j", b=2)), writes=["bi"], dma=True)
            for nm, dst, tg in (("s5_c_re", cr, "cr"), ("s5_c_im", ci, "ci")):
                Cin = Cins[tg]
                P.op("sp", lambda e, nm=nm, Cin=Cin: e.dma_start(out=Cin[:, :, 0:64], in_=I[nm].rearrange("(c g) h p -> (g h) c p", c=4)), writes=[f"Cin{tg}"], dma=True)
                P.op("act", lambda e, Cin=Cin: e.activation(out=Cin[:, :, 64:128], in_=Cin[:, :, 0:64], func=AF.Copy), reads=[f"Cin{tg}"], writes=[f"Cin{tg}b"])
                for ch in range(4):
                    P.op("pe", lambda e, ch=ch, Cin=Cin: e.transpose(out=pCT[:, ch, :], in_=Cin[:, ch, :], identity=identf[:]), reads=[f"Cin{tg}", f"Cin{tg}b", "identf"], writes=["pCT"])
                for e2 in range(2):
                    sl = slice(64 * e2, 64 * e2 + 64)
                    P.op("dve", lambda e, sl=sl, e2=e2, dst=dst: e.tensor_copy(
                        out=dst[sl, :, :].rearrange("p (c q) h -> p c q h", c=4),
                        in_=pCT[sl, :, :].rearrange("p c (q e h) -> p c q e h", q=4, e=2)[:, :, :, e2, :]), reads=["pCT"], writes=[tg])
            P.op("sp", lambda e: e.dma_start(out=bmask[:], in_=I["bmask"]), writes=["bmask"], dma=True)
            P.op("sp", lambda e: e.dma_start(out=dcol[:], in_=I["s5_d"].rearrange("(c p) -> p c", p=128)), writes=["dcol"], dma=True)

            P.op("act", lambda e: e.activation(out=dt_[:], in_=ldt[:], func=AF.Exp), reads=["ldt"], writes=["dt_"])
            P.op("dve", lambda e: e.tensor_tensor(out=ph[:], in0=li[:], in1=dt_[:], op=ALU.mult), reads=["li", "dt_"], writes=["ph"])
            P.op("dve", lambda e: e.tensor_tensor(out=lrdt[:], in0=lr[:], in1=dt_[:], op=ALU.mult), reads=["lr", "dt_"], writes=["lrdt"])
            P.op("sp", lambda e: e.dma_start(out=kvals[:], in_=I["kvals"]), writes=["kvals"], dma=True)
            bk17 = lambda a: a.unsqueeze(1).broadcast_to([128, NPW, 16])
            P.op("dve", lambda e: e.tensor_tensor(out=A0[:], in0=kvals[:], in1=bk17(ph[:]), op=ALU.mult), reads=["kvals", "ph"], writes=["A0"])
            P.op("dve", lambda e: e.tensor_tensor(out=M0[:], in0=kvals[:], in1=bk17(lrdt[:]), op=ALU.mult), reads=["kvals", "lrdt"], writes=["M0"])
            P.op("act", lambda e: e.activation(out=M0[:], in_=M0[:], func=AF.Exp), reads=["M0"], writes=["M0"])
            for cidx, off in ((0, 0.25), (1, 0.0)):
                P.op("dve", lambda e, off=off: e.tensor_scalar(out=A1_[:], in0=A0[:], scalar1=1.0 / TWO_PI, scalar2=64.0 + off, op0=ALU.mult, op1=ALU.add), reads=["A0"], writes=["A1"])
                P.op("dve", lambda e: e.tensor_copy(out=Ai[:], in_=A1_[:]), reads=["A1"], writes=["Ai"])
                P.op("dve", lambda e: e.tensor_copy(out=A2_[:], in_=Ai[:]), reads=["Ai"], writes=["A2"])
                P.op("dve", lambda e: e.tensor_tensor(out=A1_[:], in0=A1_[:], in1=A2_[:], op=ALU.subtract), reads=["A1", "A2"], writes=["A1"])
                P.op("dve", lambda e: e.tensor_single_scalar(out=A2_[:], in_=A1_[:], scalar=0.5, op=ALU.is_gt), reads=["A1"], writes=["A2"])
                P.op("dve", lambda e: e.tensor_tensor(out=A1_[:], in0=A1_[:], in1=A2_[:], op=ALU.subtract), reads=["A1", "A2"], writes=["A1"])
                P.op("act", lambda e: e.activation(out=A2_[:], in_=A1_[:], func=AF.Sin, scale=TWO_PI), reads=["A1"], writes=["A2"])
                P.op("dve", lambda e, cidx=cidx: e.tensor_tensor(out=pw[:, :, cidx, :], in0=A2_[:], in1=M0[:], op=ALU.mult), reads=["A2", "M0"], writes=["pw"])
            P.op("dve", lambda e: e.tensor_copy(out=coef[:, :, 0:2, :], in_=pw[:, 8:8 + HS_STEPS, :, :]), reads=["pw"], writes=["coef"])
            P.op("dve", lambda e: e.tensor_scalar(out=coef[:, :, 2, :], in0=pw[:, 8:8 + HS_STEPS, 1, :], scalar1=-1.0, scalar2=None, op0=ALU.mult), reads=["pw"], writes=["coef"])
            P.op("dve", lambda e: e.tensor_tensor(out=t1[:], in0=lr[:], in1=lr[:], op=ALU.mult), reads=["lr"], writes=["t1"])
            P.op("dve", lambda e: e.tensor_tensor(out=t2[:], in0=li[:], in1=li[:], op=ALU.mult), reads=["li"], writes=["t2"])
            P.op("dve", lambda e: e.tensor_tensor(out=t1[:], in0=t1[:], in1=t2[:], op=ALU.add), reads=["t1", "t2"], writes=["t1"])
            P.op("dve", lambda e: e.reciprocal(out=t1[:], in_=t1[:]), reads=["t1"], writes=["t1"])
            P.op("dve", lambda e: e.tensor_scalar(out=t2[:], in0=pw[:, 1, 0, :], scalar1=-1.0, scalar2=None, op0=ALU.add), reads=["pw"], writes=["t2"])
            P.op("dve", lambda e: e.tensor_tensor(out=t3[:], in0=t2[:], in1=lr[:], op=ALU.mult), reads=["t2", "lr"], writes=["t3"])
            P.op("dve", lambda e: e.tensor_tensor(out=t4[:], in0=pw[:, 1, 1, :], in1=li[:], op=ALU.mult), reads=["pw", "li"], writes=["t4"])
            P.op("dve", lambda e: e.tensor_tensor(out=t3[:], in0=t3[:], in1=t4[:], op=ALU.add), reads=["t3", "t4"], writes=["t3"])
            P.op("dve", lambda e: e.tensor_tensor(out=zr[:], in0=t3[:], in1=t1[:], op=ALU.mult), reads=["t3", "t1"], writes=["zr"])
            P.op("dve", lambda e: e.tensor_tensor(out=t3[:], in0=pw[:, 1, 1, :], in1=lr[:], op=ALU.mult), reads=["pw", "lr"], writes=["t3"])
            P.op("dve", lambda e: e.tensor_tensor(out=t4[:], in0=t2[:], in1=li[:], op=ALU.mult), reads=["t2", "li"], writes=["t4"])
            P.op("dve", lambda e: e.tensor_tensor(out=t3[:], in0=t3[:], in1=t4[:], op=ALU.subtract), reads=["t3", "t4"], writes=["t3"])
            P.op("dve", lambda e: e.tensor_tensor(out=zi[:], in0=t3[:], in1=t1[:], op=ALU.mult), reads=["t3", "t1"], writes=["zi"])

            def bc(a):
                return a.unsqueeze(2).broadcast_to([128, 16, 16])

            P.op("dve", lambda e: e.tensor_tensor(out=u1[:], in0=br[:], in1=bc(zr[:]), op=ALU.mult), reads=["br", "zr"], writes=["u1"])
            P.op("dve", lambda e: e.tensor_tensor(out=u2[:], in0=bi[:], in1=bc(zi[:]), op=ALU.mult), reads=["bi", "zi"], writes=["u2"])
            P.op("dve", lambda e: e.tensor_tensor(out=bbr[:], in0=u1[:], in1=u2[:], op=ALU.subtract), reads=["u1", "u2"], writes=["bbr"])
            P.op("dve", lambda e: e.tensor_tensor(out=u1[:], in0=bi[:], in1=bc(zr[:]), op=ALU.mult), reads=["bi", "zr"], writes=["u1"])
            P.op("dve", lambda e: e.tensor_tensor(out=u2[:], in0=br[:], in1=bc(zi[:]), op=ALU.mult), reads=["br", "zi"], writes=["u2"])
            P.op("dve", lambda e: e.tensor_tensor(out=bbi[:], in0=u1[:], in1=u2[:], op=ALU.add), reads=["u1", "u2"], writes=["bbi"])

            def cmul_all(dst, nk, k0, b_r, b_i, btags, dtag):
                pr = pw[:, k0:k0 + nk, 0, :].unsqueeze(3).broadcast_to([128, nk, 16, 16])
                pi = pw[:, k0:k0 + nk, 1, :].unsqueeze(3).broadcast_to([128, nk, 16, 16])
                Br = b_r.unsqueeze(1).broadcast_to([128, nk, 16, 16])
                Bi = b_i.unsqueeze(1).broadcast_to([128, nk, 16, 16])
                W1 = w1[:, 0:nk, :, :]
                W2 = w2[:, 0:nk, :, :]
                P.op("dve", lambda e: e.tensor_tensor(out=W1, in0=Br, in1=pr, op=ALU.mult), reads=btags + ["pw"], writes=["w1"])
                P.op("dve", lambda e: e.tensor_tensor(out=W2, in0=Bi, in1=pi, op=ALU.mult), reads=btags + ["pw"], writes=["w2"])
                P.op("dve", lambda e: e.tensor_tensor(out=dst[:, :, 0, :, :], in0=W1, in1=W2, op=ALU.subtract), reads=["w1", "w2"], writes=[dtag])
                P.op("dve", lambda e: e.tensor_tensor(out=W1, in0=Bi, in1=pr, op=ALU.mult), reads=btags + ["pw"], writes=["w1"])
                P.op("dve", lambda e: e.tensor_tensor(out=W2, in0=Br, in1=pi, op=ALU.mult), reads=btags + ["pw"], writes=["w2"])
                P.op("dve", lambda e: e.tensor_tensor(out=dst[:, :, 1, :, :], in0=W1, in1=W2, op=ALU.add), reads=["w1", "w2"], writes=[dtag])

            for a in range(2):
                for b in range(2):
                    P.op("pool", lambda e, a=a, b=b: e.memset(XP[a][b][:], 0.0), writes=[f"XP{a}{b}"])
            for b in range(2):
                P.op("pool", lambda e, b=b: e.memset(YP[b][:], 0.0), writes=[f"YP{b}"])
            P.op("pool", lambda e: e.memset(NOUT[:], 0.0), writes=["NOUT"])

            cmul_all(Yall, 9, 0, cr[:], ci[:], ["cr", "ci"], "Yall")
            P.op("dve", lambda e: e.tensor_scalar(out=Yall[:, :, 1, :, :], in0=Yall[:, :, 1, :, :], scalar1=-1.0, scalar2=None, op0=ALU.mult), reads=["Yall"], writes=["Yall"])
            for e2 in range(2):
                sl = slice(64 * e2, 64 * e2 + 64)
                for ri in range(2):
                    P.op("dve", lambda e, ri=ri, sl=sl, e2=e2: e.tensor_copy(out=YP[ri][sl, :, e2, :], in_=Yall[sl, 0, ri, :, :]), reads=["Yall"], writes=[f"YP{ri}"])
                for lo in range(2):
                    P.op("dve", lambda e, sl=sl, e2=e2, lo=lo: e.tensor_copy(
                        out=NOUT[sl, :, :, :, lo, lo, e2, :].rearrange("p r i hi h -> p (r i) hi h"),
                        in_=Yall[sl, 1:9, :, :, :].rearrange("p k r (hi lo) h -> p (k r) hi lo h", lo=2)[:, :, :, lo, :]),
                        reads=["Yall"], writes=["NOUT"])
            cmul_all(Xall, 8, 0, bbr[:], bbi[:], ["bbr", "bbi"], "Xall")
            for k in range(8):
                a = k % 2
                for ri in range(2):
                    for e2 in range(2):
                        sl = slice(64 * e2, 64 * e2 + 64)
                        eng = "pool" if e2 == 0 else "act"
                        if eng == "pool":
                            P.op("pool", lambda e, a=a, ri=ri, sl=sl, e2=e2, k=k: e.tensor_copy(out=XP[a][ri][sl, :, e2, :], in_=Xall[sl, k, ri, :, :]),
                                 reads=["Xall"], writes=[f"XP{a}{ri}"])
                        else:
                            P.op("act", lambda e, a=a, ri=ri, sl=sl, e2=e2, k=k: e.activation(out=XP[a][ri][sl, :, e2, :], in_=Xall[sl, k, ri, :, :], func=AF.Copy),
                                 reads=["Xall"], writes=[f"XP{a}{ri}"])
                for ri in range(2):
                    for ch in range(4):
                        P.op("pe", lambda e, a=a, ri=ri, ch=ch: e.matmul(
                            pI[:, ch, :], lhsT=XP[a][ri][:, 4 * ch:4 * ch + 4, :, :].rearrange("p a b c -> p (a b c)"),
                            rhs=identf[:], start=True, stop=True), reads=[f"XP{a}{ri}", "identf"], writes=["pI"])
                    P.op("act", lambda e, ri=ri, k=k: e.activation(out=INJ[:, :, ri, 7 - k, :], in_=pI[:], func=AF.Copy), reads=["pI"], writes=["INJ"])
                for ch in range(4):
                    for ri in range(2):
                        P.op("pe", lambda e, a=a, ri=ri, ch=ch: e.matmul(
                            pF[:, ch, :], lhsT=XP[a][ri][:, 4 * ch:4 * ch + 4, :, :].rearrange("p a b c -> p (a b c)"),
                            rhs=YP[ri][:, 4 * ch:4 * ch + 4, :, :].rearrange("p a b c -> p (a b c)"),
                            start=(ri == 0), stop=(ri == 1)), reads=[f"XP{a}{ri}", f"YP{ri}"], writes=["pF"])
                if k == 0:
                    P.op("dve", lambda e: e.tensor_tensor(out=tmpF[:], in0=pF[:], in1=bmask[:].unsqueeze(1).broadcast_to([128, 4, 128]), op=ALU.mult),
                         reads=["pF", "bmask"], writes=["tmpF"])
                    for ch in range(4):
                        P.op("dve", lambda e, ch=ch: e.scalar_tensor_tensor(out=FIR[:, ch, 0, :], in0=identf[:], scalar=dcol[:, ch:ch + 1], in1=tmpF[:, ch, :],
                                                                            op0=ALU.mult, op1=ALU.add), reads=["tmpF", "identf", "dcol"], writes=["FIR"])
                else:
                    P.op("dve", lambda e, k=k: e.tensor_tensor(out=FIR[:, :, k, :], in0=pF[:], in1=bmask[:].unsqueeze(1).broadcast_to([128, 4, 128]), op=ALU.mult),
                         reads=["pF", "bmask"], writes=["FIR"])
            P.op("sp", lambda e: e.dma_start(out=d_inj, in_=INJ[:].rearrange("p a b c d -> p (a b c d)")), reads=["INJ"], writes=["d_inj"], dma=True)
            P.op("sp", lambda e: e.dma_start(out=d_fir, in_=FIR[:].rearrange("p a b c -> p (a b c)")), reads=["FIR"], writes=["d_fir"], dma=True)
            P.op("sp", lambda e: e.dma_start(out=d_nout, in_=NOUT[:].rearrange("p a b c d e f g -> p (a b c d e f g)")), reads=["NOUT"], writes=["d_nout"], dma=True)
            P.emit()


        with ExitStack() as st_mix:
            sbm, _ = mk(st_mix)
            mixT = sbm("mixT", [128, 4, TOK], BF16)
            with ExitStack() as st_s5:
                sbs, _ = mk(st_s5)
                uT = sbs("uT", [128, 4, TOK], BF16)
                Sb = sbs("Sb", [128, 2, 16, NSUB], BF16)
                with ExitStack() as st:
                    sb, ps = mk(st)
                    Wb1 = sb("Wb1", [128, 8, 1536], BF16)
                    wst = [sb(f"wst{i}", [128, 768]) for i in range(2)]
                    n1col = sb("n1col", [128, 8])
                    INJs = sb("INJs", [128, 4, 2, 8, 128], BF16)
                    L = sb("L", [128, 2, 4, NSUB])
                    tmpA = [sb(f"tmpA{i}", [128, 2, NSUB]) for i in range(4)]
                    xt = [sb(f"xt{i}", [128, D]) for i in range(2)]
                    xbb = [sb(f"xb{i}", [128, D], BF16) for i in range(2)]
                    xT = [sb(f"xT{i}", [128, 8, 128], BF16) for i in range(2)]
                    ssb = sb("ssb", [128, 2]); msb = sb("msb", [128, 2]); rsb = sb("rsb", [128, 2])
                    rtm = [sb(f"rtm{i}", [128, 8, 32]) for i in range(4)]
                    rotk = sb("rotk", [128, 2, 64]); kdec = sb("kdec", [128, 512])
                    kr = sb("kr", [128, 8, 64])
                    ktil = sb("ktil", [128, 512], BF16); vb = sb("vb", [128, 512], BF16)
                    pT = ps("pT", [128, 8, 128], BF16)
                    pU = ps("pU", [128, 4, 128])
                    pbig = ps("pbig", [128, 4, 512])

                    P.op("sp", lambda e: e.dma_start(out=n1col[:], in_=I["norm1_w"].rearrange("(c p) -> p c", p=128), allow_slow_non_contiguous=True),
                         writes=["n1col"], dma=True)
                    P.op("sp", lambda e: e.dma_start(out=INJs[:].rearrange("p a b c d -> p (a b c d)"), in_=d_inj), writes=["INJs"], dma=True)
                    P.op("sp", lambda e: e.dma_start(out=kdec[:], in_=I["kdec"]), writes=["kdec"], dma=True)
                    for c in range(8):
                        for b in range(2):
                            if b == 0:
                                P.op("sp", lambda e, c=c, b=b: e.dma_start(out=wst[b][:, 0:512], in_=I["w_in"][c * 128:(c + 1) * 128, 0:512]), writes=[f"wst{b}"], dma=True)
                                P.op("sp", lambda e, c=c, b=b: e.dma_start(out=wst[b][:, 512:768], in_=I["w_in"][c * 128:(c + 1) * 128, 1024:1280]), writes=[f"wst{b}"], dma=True)
                                P.op("dve", lambda e, c=c, b=b: e.tensor_scalar(out=Wb1[:, c, 0:768], in0=wst[b][:], scalar1=n1col[:, c:c + 1], scalar2=None, op0=ALU.mult),
                                     reads=[f"wst{b}", "n1col"], writes=["Wb1"])
                            else:
                                P.op("sp", lambda e, c=c, b=b: e.dma_start(out=wst[b][:], in_=I["w_in"][c * 128:(c + 1) * 128, 1280:2048]), writes=[f"wst{b}"], dma=True)
                                P.op("act", lambda e, c=c, b=b: e.activation(out=Wb1[:, c, 768:1536], in_=wst[b][:], func=AF.Copy, scale=n1col[:, c:c + 1]),
                                     reads=[f"wst{b}", "n1col"], writes=["Wb1"])

                    def A1(ti, prefix, sl):
                        src = I["xpre"] if prefix else I["x"]
                        b = sl % 2
                        P.op("sp", lambda e: e.dma_start(out=xt[b][:], in_=src[ti * 128:(ti + 1) * 128, :]), writes=[f"xt{b}"], dma=True)
                        if prefix:
                            P.op("sp", lambda e: e.dma_start(out=rotk[:, b, :], in_=I["rot_kpre"][:, ti, :]), writes=[f"rotk{b}"], dma=True)
                        P.op("act", lambda e: e.activation(out=xbb[b][:], in_=xt[b][:], func=AF.Square, accum_out=ssb[:, b:b + 1]), reads=[f"xt{b}"], writes=[f"xb{b}", f"ss{b}"])
                        P.op("dve", lambda e: e.tensor_scalar(out=msb[:, b:b + 1], in0=ssb[:, b:b + 1], scalar1=1.0 / D, scalar2=EPS, op0=ALU.mult, op1=ALU.add), reads=[f"ss{b}"], writes=[f"ms{b}"])
                        P.op("pool", lambda e: e.tensor_tensor(out=rsb[:, b:b + 1], in0=msb[:, b:b + 1], in1=mhalf[:], op=ALU.pow), reads=[f"ms{b}", "mhalf"], writes=[f"rs{b}"])
                        P.op("act", lambda e: e.activation(out=xbb[b][:], in_=xt[b][:], func=AF.Copy, scale=rsb[:, b:b + 1]), reads=[f"xt{b}", f"rs{b}"], writes=[f"xb{b}"])

                    def A2(ti, prefix, sl):
                        b = sl % 2
                        for c in range(8):
                            P.op("pe", lambda e, c=c: e.transpose(out=pT[:, c, :], in_=xbb[b][:, c * 128:(c + 1) * 128], identity=identb[:]),
                                 reads=[f"xb{b}", "identb"], writes=["pT"])
                        P.op("dve", lambda e: e.tensor_copy(out=xT[b][:], in_=pT[:]), reads=["pT"], writes=[f"xT{b}"])

                    def B1(ti, prefix, sl):
                        b = sl % 2
                        for m in range(4):
                            for c in range(8):
                                P.op("pe", lambda e, m=m, c=c: e.matmul(pU[:, m, :], lhsT=Wb1[:, c, m * 128:(m + 1) * 128], rhs=xT[b][:, c, :],
                                                                        start=(c == 0), stop=(c == 7)), reads=["Wb1", f"xT{b}"], writes=["pU"])
                        if not prefix:
                            return
                        for c in range(8):
                            P.op("pe", lambda e, c=c: e.matmul(pbig[:, 0, :], lhsT=xT[b][:, c, :], rhs=Wb1[:, c, 512:1024], start=(c == 0), stop=(c == 7)),
                                 reads=["Wb1", f"xT{b}"], writes=["pb0"])
                        for c in range(8):
                            P.op("pe", lambda e, c=c: e.matmul(pbig[:, 1, :], lhsT=xT[b][:, c, :], rhs=Wb1[:, c, 1024:1536], start=(c == 0), stop=(c == 7)),
                                 reads=["Wb1", f"xT{b}"], writes=["pb1"])

                    def B2(ti, prefix, sl):
                        b = sl % 2
                        P.op("act", lambda e: e.activation(out=uT[:, :, ti * 128:(ti + 1) * 128], in_=pU[:], func=AF.Copy), reads=["pU"], writes=[f"uT{ti}"])
                        if not prefix:
                            return
                        pk = pbig[:, 0, :].rearrange("p (h d) -> p h d", h=8)
                        cosb = rotk[:, b, 0:32].unsqueeze(1).broadcast_to([128, 8, 32])
                        sinb = rotk[:, b, 32:64].unsqueeze(1).broadcast_to([128, 8, 32])
                        rtag = ["pb0", f"rotk{b}"]
                        P.op("dve", lambda e: e.tensor_tensor(out=rtm[0][:], in0=pk[:, :, 0:32], in1=cosb, op=ALU.mult), reads=rtag, writes=["rtm0"])
                        P.op("dve", lambda e: e.tensor_tensor(out=rtm[1][:], in0=pk[:, :, 32:64], in1=sinb, op=ALU.mult), reads=rtag, writes=["rtm1"])
                        P.op("dve", lambda e: e.tensor_tensor(out=rtm[2][:], in0=pk[:, :, 0:32], in1=sinb, op=ALU.mult), reads=rtag, writes=["rtm2"])
                        P.op("dve", lambda e: e.tensor_tensor(out=rtm[3][:], in0=pk[:, :, 32:64], in1=cosb, op=ALU.mult), reads=rtag, writes=["rtm3"])
                        P.op("pool", lambda e: e.tensor_tensor(out=kr[:, :, 0:32], in0=rtm[0][:], in1=rtm[1][:], op=ALU.subtract), reads=["rtm0", "rtm1"], writes=["kr1"])
                        P.op("pool", lambda e: e.tensor_tensor(out=kr[:, :, 32:64], in0=rtm[2][:], in1=rtm[3][:], op=ALU.add), reads=["rtm2", "rtm3"], writes=["kr2"])
                        P.op("dve", lambda e: e.tensor_tensor(out=ktil[:], in0=kr[:].rearrange("p h d -> p (h d)"), in1=kdec[:], op=ALU.mult),
                             reads=["kr1", "kr2", "kdec"], writes=["ktil"])
                        P.op("act", lambda e: e.activation(out=vb[:], in_=pbig[:, 1, :], func=AF.Copy), reads=["pb1"], writes=["vb"])
                        pkv = pbig[:, 2, 0:256].rearrange("p (j e) -> p j e", j=4)
                        for h in range(8):
                            j, hl = h // 2, h % 2
                            P.op("pe", lambda e, h=h, j=j, hl=hl: e.matmul(pkv[64 * hl:64 * hl + 64, j, :], lhsT=ktil[:, h * 64:(h + 1) * 64],
                                                                           rhs=vb[:, h * 64:(h + 1) * 64], start=True, stop=True),
                                 reads=["ktil", "vb"], writes=["pb2"])
                        for j in range(4):
                            P.op("dve", lambda e, j=j: e.scalar_tensor_tensor(out=R[:, j, :], in0=R[:, j, :], scalar=gam128[:, j:j + 1], in1=pkv[:, j, :],
                                                                              op0=ALU.mult, op1=ALU.add), reads=["pb2", "gam128", "R"], writes=["R"])

                    slot = [0]

                    def run_tiles(prefix):
                        seq = [(ti, prefix, slot[0] + ti) for ti in range(ntile)]
                        slot[0] += ntile
                        A1(*seq[0])
                        A2(*seq[0])
                        for i in range(len(seq)):
                            if i + 1 < len(seq):
                                A1(*seq[i + 1])
                            B1(*seq[i])
                            if i + 1 < len(seq):
                                A2(*seq[i + 1])
                            B2(*seq[i])

                    def inject(hf, main):
                        for gi in range(4):
                            g16 = 4 * hf + gi
                            ch, q = g16 // 4, g16 % 4
                            for ri in range(2):
                                bk = 2 * (gi % 2) + ri
                                n = NSUB - 1 if main else NSUB
                                o0 = 1 if main else 0
                                for r in range(8):
                                    P.op("pe", lambda e, ch=ch, q=q, ri=ri, r=r, bk=bk, n=n, o0=o0: e.matmul(
                                        pbig[:, bk, o0:o0 + n], lhsT=INJs[32 * q:32 * q + 32, ch, ri, r, :],
                                        rhs=uT[32 * q:32 * q + 32, ch, :].rearrange("p (c r) -> p c r", r=8)[:, 0:n, r],
                                        start=(r == 0), stop=(r == 7), tile_position=(32 * q, 0)),
                                        reads=["INJs"] + [f"uT{t}" for t in range(NT)], writes=[f"pb{bk}"])
                                P.op("act", lambda e, ri=ri, gi=gi, bk=bk, o0=o0, n=n: e.activation(out=L[:, ri, gi, o0:o0 + n], in_=pbig[:, bk, o0:o0 + n], func=AF.Copy),
                                     reads=[f"pb{bk}"], writes=[f"L{gi}"])
                            if main:
                                P.op("pool", lambda e, gi=gi, g16=g16: e.tensor_copy(out=L[:, :, gi, 0:1], in_=S0[:, :, g16:g16 + 1]), reads=["S0"], writes=[f"L{gi}"])

                    def scan(hf, main):
                        N = NSUB
                        for s in range(HS_STEPS):
                            d = 2 ** s
                            G = []
                            for gi in range(4):
                                g16 = 4 * hf + gi
                                bufs = [(L[:, 0, gi, :], L[:, 1, gi, :], L[:, :, gi, :], f"L{gi}"), (tmpA[gi][:, 0, :], tmpA[gi][:, 1, :], tmpA[gi][:, :, :], f"tmpA{gi}")]
                                G.append(bufs[s % 2] + bufs[(s + 1) % 2] + (coef[:, s, 0, g16:g16 + 1], coef[:, s, 1, g16:g16 + 1], coef[:, s, 2, g16:g16 + 1]))
                            for (sr, si, sall, stag, dr, di, dall, dtag, car, cai, cnai) in G:
                                P.op("pool", lambda e, dall=dall, sall=sall, d=d: e.tensor_copy(out=dall[:, :, 0:d], in_=sall[:, :, 0:d]), reads=[stag], writes=[dtag])
                            for (sr, si, sall, stag, dr, di, dall, dtag, car, cai, cnai) in G:
                                P.op("dve", lambda e, dr=dr, sr=sr, car=car, d=d: e.scalar_tensor_tensor(out=dr[:, d:N], in0=sr[:, 0:N - d], scalar=car, in1=sr[:, d:N], op0=ALU.mult, op1=ALU.add),
                                     reads=[stag, "coef"], writes=[dtag])
                            for (sr, si, sall, stag, dr, di, dall, dtag, car, cai, cnai) in G:
                                P.op("dve", lambda e, di=di, si=si, car=car, d=d: e.scalar_tensor_tensor(out=di[:, d:N], in0=si[:, 0:N - d], scalar=car, in1=si[:, d:N], op0=ALU.mult, op1=ALU.add),
                                     reads=[stag, "coef"], writes=[dtag])
                            for (sr, si, sall, stag, dr, di, dall, dtag, car, cai, cnai) in G:
                                P.op("dve", lambda e, dr=dr, si=si, cnai=cnai, d=d: e.scalar_tensor_tensor(out=dr[:, d:N], in0=si[:, 0:N - d], scalar=cnai, in1=dr[:, d:N], op0=ALU.mult, op1=ALU.add),
                                     reads=[stag, "coef"], writes=[dtag])
                            for (sr, si, sall, stag, dr, di, dall, dtag, car, cai, cnai) in G:
                                P.op("dve", lambda e, di=di, sr=sr, cai=cai, d=d: e.scalar_tensor_tensor(out=di[:, d:N], in0=sr[:, 0:N - d], scalar=cai, in1=di[:, d:N], op0=ALU.mult, op1=ALU.add),
                                     reads=[stag, "coef"], writes=[dtag])
                        for gi in range(4):
                            g16 = 4 * hf + gi
                            if main:
                                P.op("act", lambda e, g16=g16, gi=gi: e.activation(out=Sb[:, :, g16, :], in_=tmpA[gi][:], func=AF.Copy), reads=[f"tmpA{gi}"], writes=["Sb"])
                            else:
                                P.op("act", lambda e, g16=g16, gi=gi: e.activation(out=S0[:, :, g16:g16 + 1], in_=tmpA[gi][:, :, N - 1:N], func=AF.Copy), reads=[f"tmpA{gi}"], writes=["S0"])

                    def tree(hf):
                        n = NSUB
                        for s in range(HS_STEPS):
                            n //= 2
                            G = []
                            for gi in range(4):
                                g16 = 4 * hf + gi
                                bufs = [(L[:, :, gi, :], f"L{gi}"), (tmpA[gi][:, :, :], f"tmpA{gi}")]
                                src, stag = bufs[s % 2]
                                dst, dtag = bufs[(s + 1) % 2]
                                ev = lambda ri, src=src, n=n: src[:, ri, 0:2 * n].rearrange("p (m t) -> p m t", t=2)[:, :, 0]
                                od = lambda ri, src=src, n=n: src[:, ri, 0:2 * n].rearrange("p (m t) -> p m t", t=2)[:, :, 1]
                                G.append((ev, od, stag, dst, dtag, coef[:, s, 0, g16:g16 + 1], coef[:, s, 1, g16:g16 + 1], coef[:, s, 2, g16:g16 + 1]))
                            for (ev, od, stag, dst, dtag, car, cai, cnai) in G:
                                P.op("dve", lambda e, dst=dst, ev=ev, od=od, car=car, n=n: e.scalar_tensor_tensor(out=dst[:, 0, 0:n], in0=ev(0), scalar=car, in1=od(0), op0=ALU.mult, op1=ALU.add),
                                     reads=[stag, "coef"], writes=[dtag])
                            for (ev, od, stag, dst, dtag, car, cai, cnai) in G:
                                P.op("dve", lambda e, dst=dst, ev=ev, od=od, car=car, n=n: e.scalar_tensor_tensor(out=dst[:, 1, 0:n], in0=ev(1), scalar=car, in1=od(1), op0=ALU.mult, op1=ALU.add),
                                     reads=[stag, "coef"], writes=[dtag])
                            for (ev, od, stag, dst, dtag, car, cai, cnai) in G:
                                P.op("dve", lambda e, dst=dst, ev=ev, cnai=cnai, n=n: e.scalar_tensor_tensor(out=dst[:, 0, 0:n], in0=ev(1), scalar=cnai, in1=dst[:, 0, 0:n], op0=ALU.mult, op1=ALU.add),
                                     reads=[stag, "coef"], writes=[dtag])
                            for (ev, od, stag, dst, dtag, car, cai, cnai) in G:
                                P.op("dve", lambda e, dst=dst, ev=ev, cai=cai, n=n: e.scalar_tensor_tensor(out=dst[:, 1, 0:n], in0=ev(0), scalar=cai, in1=dst[:, 1, 0:n], op0=ALU.mult, op1=ALU.add),
                                     reads=[stag, "coef"], writes=[dtag])
                        for gi in range(4):
                            g16 = 4 * hf + gi
                            P.op("act", lambda e, g16=g16, gi=gi: e.activation(out=S0[:, :, g16:g16 + 1], in_=tmpA[gi][:, :, 0:1], func=AF.Copy), reads=[f"tmpA{gi}"], writes=["S0"])

                    run_tiles(True)
                    for hf in range(4):
                        inject(hf, False)
                        tree(hf)
                    run_tiles(False)
                    for hf in range(4):
                        inject(hf, True)
                        scan(hf, True)
                    if skip_p1:
                        P.reset_phase()
                    P.emit()

                with ExitStack() as st:
                    sb, ps = mk(st)
                    FIRs = sb("FIRs", [128, 4, 8, 128], BF16)
                    NOUTs = sb("NOUTs", [128, 8, 2, 16, 64], BF16)
                    Wg = sb("Wg", [128, 4, 512], BF16)
                    yf = sb("yf", [128, 4, 512]); sq = sb("sq", [128, 4, 512]); ygb = sb("ygb", [128, 4, 512], BF16)
                    pY = ps("pY", [128, 4, 512]); pZ = ps("pZ", [128, 4, 512])
                    P.op("sp", lambda e: e.dma_start(out=FIRs[:].rearrange("p a b c -> p (a b c)"), in_=d_fir), writes=["FIRs"], dma=True)
                    P.op("sp", lambda e: e.dma_start(out=NOUTs[:].rearrange("p a b c d -> p (a b c d)"), in_=d_nout), writes=["NOUTs"], dma=True)
                    P.op("pool", lambda e: e.dma_start(out=Wg[:], in_=I["s5_w_glu"].rearrange("(c p) n -> p c n", p=128)), writes=["Wg"], dma=True)
                    GC = 2.0 * math.sqrt(2.0 / math.pi)
                    for bk in range(ntile // 4):
                        cols = slice(512 * bk, 512 * bk + 512)
                        for ch in range(4):
                            py3 = pY[:, ch, :].rearrange("p (c r) -> p c r", r=8)
                            u3 = uT[:, ch, cols].rearrange("p (c r) -> p c r", r=8)
                            for tau in range(8):
                                P.op("pe", lambda e, ch=ch, tau=tau, py3=py3, u3=u3: e.matmul(
                                    py3[:, :, tau:8], lhsT=FIRs[:, ch, tau, :], rhs=u3[:, :, 0:8 - tau], start=(tau == 0), stop=False, skip_group_check=True),
                                    reads=["FIRs", "uT"], writes=[f"pY{ch}"])
                            for q in range(4):
                                g16 = 4 * ch + q
                                hc = q // 2
                                py3h = pY[64 * hc:64 * hc + 64, ch, :].rearrange("p (c r) -> p c r", r=8)
                                for r in range(8):
                                    for ri in range(2):
                                        P.op("pe", lambda e, g16=g16, r=r, ri=ri, py3h=py3h, bk=bk: e.matmul(
                                            py3h[:, :, r], lhsT=NOUTs[:, r, ri, g16, :], rhs=Sb[:, ri, g16, 64 * bk:64 * bk + 64],
                                            start=False, stop=True, skip_group_check=True), reads=["NOUTs", "Sb"], writes=[f"pY{ch}"])
                        ptags = [f"pY{ch}" for ch in range(4)]
                        P.op("act", lambda e: e.activation(out=sq[:], in_=pY[:], func=AF.Square, scale=math.sqrt(0.044715)), reads=ptags, writes=["sq"])
                        P.op("dve", lambda e: e.scalar_tensor_tensor(out=sq[:], in0=sq[:], scalar=1.0, in1=pY[:], op0=ALU.add, op1=ALU.mult), reads=ptags + ["sq"], writes=["sq"])
                        P.op("act", lambda e: e.activation(out=sq[:], in_=sq[:], func=AF.Sigmoid, scale=GC), reads=["sq"], writes=["sq"])
                        P.op("dve", lambda e: e.tensor_tensor(out=yf[:], in0=pY[:], in1=sq[:], op=ALU.mult), reads=ptags + ["sq"], writes=["yf"])
                        P.op("act", lambda e: e.activation(out=ygb[:], in_=yf[:], func=AF.Copy), reads=["yf"], writes=["ygb"])
                        for m in range(4):
                            for c in range(4):
                                P.op("pe", lambda e, m=m, c=c: e.matmul(pZ[:, m, :], lhsT=Wg[:, c, m * 128:(m + 1) * 128], rhs=ygb[:, c, :], start=(c == 0), stop=(c == 3)),
                                     reads=["Wg", "ygb"], writes=[f"pZ{m}"])
                            P.op("act", lambda e, m=m: e.activation(out=sq[:, m, :], in_=pZ[:, m, :], func=AF.Sigmoid, bias=bglu[:, m:m + 1]),
                                 reads=[f"pZ{m}", "bglu", "yf"], writes=[f"sg{m}"])
                            P.op("dve", lambda e, m=m, cols=cols: e.tensor_tensor(out=mixT[:, m, cols], in0=yf[:, m, :], in1=sq[:, m, :], op=ALU.mult),
                                 reads=["yf", f"sg{m}"], writes=["mixT"])
                        P.op("pool", lambda e: e.memset(ss_dummy[:], 0.0), writes=["sq"] + [f"sg{m}" for m in range(4)])
                    if dbg == "s5":
                        P.op("sp", lambda e: e.dma_start(out=dbg_out, in_=mixT[:]), reads=["mixT"], writes=["dbg"], dma=True)
                    if skip_p1:
                        P.reset_phase()
                    P.emit()
            if dbg == "s5":
                return nc
            BIG = 1.0e4
            with ExitStack() as st:
                sb, ps = mk(st)
                Wb2 = sb("Wb2", [128, 8, 2048], BF16)
                wst = [sb(f"wst{i}", [128, 1024]) for i in range(2)]
                n1col = sb("n1col", [128, 8])
                Wo = sb("Wo", [128, 8, D], BF16)
                rt = sb("rt", [128, 2, 2, 64])
                decT = sb("decT", [128, 8, 128]); qdec = sb("qdec", [128, 4, 128]); kdec = sb("kdec", [128, 512])
                n2w = sb("n2w", [128, D]); rnw = sb("rnw", [128, 512])
                Wr = sb("Wr", [128, 8, 36]); bfull = sb("bfull", [128, 36])
                triub = sb("triub", [128, 128], BF16); onesb = sb("onesb", [128, 128], BF16)
                tmpc = sb("tmpc", [128, 128])
                base_b = sb("base_b", [128, NEXP]); ecapl = sb("ecapl", [128, NEXP])
                xt = [sb(f"xt{i}", [128, D]) for i in range(4)]
                xbb = [sb(f"xb{i}", [128, D], BF16) for i in range(2)]
                xT = sb("xT", [128, 8, 128], BF16)
                ssA = sb("ssA", [128, 2]); msA = sb("msA", [128, 2]); rsA = sb("rsA", [128, 2])
                ssC = sb("ssC", [128, 1]); msC = sb("msC", [128, 1]); rsC = sb("rsC", [128, 1])
                kr = sb("kr", [128, 8, 64]); ta = sb("ta", [128, 8, 32]); tb = sb("tb", [128, 8, 32])
                rtmp = [[sb(f"rtmp{a}{i}", [128, 8, 32]) for i in range(4)] for a in range(2)]
                qrb = sb("qrb", [128, 8, 64], BF16); krb = sb("krb", [128, 512], BF16)
                ktil = [sb(f"ktil{i}", [128, 512], BF16) for i in range(2)]
                vb = [sb(f"vb{i}", [128, 512], BF16) for i in range(2)]
                sg = [sb(f"sg{i}", [128, 512]) for i in range(2)]
                qT = [sb(f"qT{i}", [128, 4, 128], BF16) for i in range(2)]
                qTd = [sb(f"qTd{i}", [128, 4, 128], BF16) for i in range(2)]
                kT = [sb(f"kT{i}", [128, 4, 128], BF16) for i in range(2)]
                PT = sb("PT", [128, 8, 128], BF16)
                osq = sb("osq", [128, 512]); oc = sb("oc", [128, 8, 64])
                s1 = sb("s1", [128, 8]); s2 = sb("s2", [128, 8]); mean = sb("mean", [128, 8]); var = sb("var", [128, 8]); rstd = sb("rstd", [128, 8])
                mh8 = sb("mh8", [128, 8])
                yret = sb("yret", [128, 512], BF16)
                yretT = [sb(f"yretT{i}", [128, 4, 128], BF16) for i in range(2)]
                hh = sb("hh", [128, D]); hn = sb("hn", [128, D])
                hnb = [sb(f"hnb{i}", [128, D], BF16) for i in range(2)]
                hnT = sb("hnT", [128, 8, 128])
                lg = sb("lg", [128, 36]); em = sb("em", [128, 32]); em2 = sb("em2", [128, 32])
                goh = sb("goh", [128, 4]); pen = sb("pen", [128, 4])
                maskb = sb("maskb", [128, 32], BF16); posf = sb("posf", [128, 32]); okm = sb("okm", [128, 32]); jk = sb("jk", [128, 32])
                sm = sb("sm", [128, 16]); ex5 = sb("ex5", [128, 5]); ex5t = sb("ex5t", [128, 5])
                oh12 = sb("oh12", [128, 2, 32]); t2 = sb("t2", [128, 2, 32]); d2 = sb("d2", [128, 2]); ok2 = sb("ok2", [128, 2]); pad2 = sb("pad2", [128, 2])
                dstf = sb("dstf", [128, 2])
                oh1 = oh12[:, 0, :]; oh2 = oh12[:, 1, :]
                pT = ps("pT", [128, 8, 128], BF16)
                pP = ps("pP", [128, 2, 512])
                pS = ps("pS", [128, 2, 512])
                pO = ps("pO", [128, 512])
                pK = ps("pK", [128, 512])
                pH = ps("pH", [128, 512])

                P.op("sp", lambda e: e.dma_start(out=n1col[:], in_=I["norm1_w"].rearrange("(c p) -> p c", p=128), allow_slow_non_contiguous=True),
                     writes=["n1col"], dma=True)
                for c in range(8):
                    for b in range(2):
                        P.op("sp", lambda e, c=c, b=b: e.dma_start(out=wst[b][:], in_=I["w_in"][c * 128:(c + 1) * 128, 512 + 1024 * b:512 + 1024 * (b + 1)]),
                             writes=[f"wst{b}"], dma=True)
                        if b == 0:
                            P.op("dve", lambda e, c=c, b=b: e.tensor_scalar(out=Wb2[:, c, 0:1024], in0=wst[b][:], scalar1=n1col[:, c:c + 1], scalar2=None, op0=ALU.mult),
                                 reads=[f"wst{b}", "n1col"], writes=["Wb2"])
                        else:
                            P.op("act", lambda e, c=c, b=b: e.activation(out=Wb2[:, c, 1024:2048], in_=wst[b][:], func=AF.Copy, scale=n1col[:, c:c + 1]),
                                 reads=[f"wst{b}", "n1col"], writes=["Wb2"])
                P.op("pool", lambda e: e.dma_start(out=Wo[:], in_=I["w_out"].rearrange("(c p) n -> p c n", p=128)), writes=["Wo"], dma=True)
                P.op("sp", lambda e: e.dma_start(out=decT[:], in_=I["decT"]), writes=["decT"], dma=True)
                P.op("sp", lambda e: e.dma_start(out=qdec[:], in_=I["qdec"]), writes=["qdec"], dma=True)
                P.op("sp", lambda e: e.dma_start(out=kdec[:], in_=I["kdec"]), writes=["kdec"], dma=True)
                P.op("sp", lambda e: e.dma_start(out=n2w[:], in_=I["norm2_w"].partition_broadcast(128)), writes=["n2w"], dma=True)
                P.op("sp", lambda e: e.dma_start(out=rnw[:], in_=I["ret_norm_w"].partition_broadcast(128)), writes=["rnw"], dma=True)
                P.op("sp", lambda e: e.dma_start(out=Wr[:, :, 0:4], in_=I["router_group_w"].rearrange("(c p) n -> p c n", p=128), allow_slow_non_contiguous=True),
                     writes=["Wr"], dma=True)
                P.op("sp", lambda e: e.dma_start(out=Wr[:, :, 4:36], in_=I["router_expert_w"].rearrange("(c p) n -> p c n", p=128), allow_slow_non_contiguous=True),
                     writes=["Wr"], dma=True)
                P.op("sp", lambda e: e.dma_start(out=bfull[:, 0:4], in_=I["router_group_b"].partition_broadcast(128)), writes=["bfull"], dma=True)
                P.op("sp", lambda e: e.dma_start(out=bfull[:, 4:36], in_=I["router_expert_b"].partition_broadcast(128)), writes=["bfull"], dma=True)
                P.op("sp", lambda e: e.dma_start(out=tmpc[:], in_=I["triu"]), writes=["tmpc"], dma=True)
                P.op("dve", lambda e: e.tensor_copy(out=triub[:], in_=tmpc[:]), reads=["tmpc"], writes=["triub"])
                P.op("pool", lambda e: e.memset(onesb[:], 1.0), writes=["onesb"])
                P.op("sp", lambda e: e.dma_start(out=base_b[:], in_=I["ecap"]), writes=["base_b"], dma=True)
                P.op("sp", lambda e: e.dma_start(out=ecapl[:], in_=I["ecap"]), writes=["ecapl"], dma=True)
                P.op("dve", lambda e: e.tensor_scalar(out=ecapl[:], in0=ecapl[:], scalar1=float(CAP), scalar2=None, op0=ALU.add), reads=["ecapl"], writes=["ecapl"])
                P.op("pool", lambda e: e.memset(mh8[:], -0.5), writes=["mh8"])

                def upd_Rb():
                    for hl in range(2):
                        sl = slice(64 * hl, 64 * hl + 64)
                        P.op("act", lambda e, sl=sl, hl=hl: e.activation(out=Rb[sl, :, 64 * hl:64 * hl + 64], in_=R[sl, :, :], func=AF.Copy), reads=["R"], writes=["Rb"])
                upd_Rb()

                def rotary(bank, tab, out1, out2, wtags):
                    pk = pP[:, bank, :].rearrange("p (h d) -> p h d", h=8)
                    cosb = tab[:, 0:32].unsqueeze(1).broadcast_to([128, 8, 32])
                    sinb = tab[:, 32:64].unsqueeze(1).broadcast_to([128, 8, 32])
                    rtag = [f"pP{bank}", "rt"]
                    P.op("dve", lambda e: e.tensor_tensor(out=ta[:], in0=pk[:, :, 0:32], in1=cosb, op=ALU.mult), reads=rtag, writes=["ta"])
                    P.op("dve", lambda e: e.tensor_tensor(out=tb[:], in0=pk[:, :, 32:64], in1=sinb, op=ALU.mult), reads=rtag, writes=["tb"])
                    P.op("pool", lambda e: e.tensor_tensor(out=out1, in0=ta[:], in1=tb[:], op=ALU.subtract), reads=["ta", "tb"], writes=[wtags[0]])
                    P.op("dve", lambda e: e.tensor_tensor(out=ta[:], in0=pk[:, :, 0:32], in1=sinb, op=ALU.mult), reads=rtag, writes=["ta"])
                    P.op("dve", lambda e: e.tensor_tensor(out=tb[:], in0=pk[:, :, 32:64], in1=cosb, op=ALU.mult), reads=rtag, writes=["tb"])
                    P.op("pool", lambda e: e.tensor_tensor(out=out2, in0=ta[:], in1=tb[:], op=ALU.add), reads=["ta", "tb"], writes=[wtags[1]])

                def a0(ti):
                    b4, b = ti % 4, ti % 2
                    rows = slice(ti * 128, (ti + 1) * 128)
                    P.op("sp", lambda e: e.dma_start(out=xt[b4][:], in_=I["x"][rows, :]), writes=[f"xt{b4}"], dma=True)
                    P.op("sp", lambda e: e.dma_start(out=rt[:, b, 0, :], in_=I["rot_q"][:, ti, :]), writes=[f"rt{b}"], dma=True)
                    P.op("sp", lambda e: e.dma_start(out=rt[:, b, 1, :], in_=I["rot_k"][:, ti, :]), writes=[f"rt{b}"], dma=True)
                    P.op("act", lambda e: e.activation(out=xbb[b][:], in_=xt[b4][:], func=AF.Square, accum_out=ssA[:, b:b + 1]), reads=[f"xt{b4}"], writes=[f"xb{b}", f"ssA{b}"])
                    P.op("dve", lambda e: e.tensor_scalar(out=msA[:, b:b + 1], in0=ssA[:, b:b + 1], scalar1=1.0 / D, scalar2=EPS, op0=ALU.mult, op1=ALU.add), reads=[f"ssA{b}"], writes=[f"msA{b}"])
                    P.op("pool", lambda e: e.tensor_tensor(out=rsA[:, b:b + 1], in0=msA[:, b:b + 1], in1=mhalf[:], op=ALU.pow), reads=[f"msA{b}", "mhalf"], writes=[f"rsA{b}"])
                    P.op("act", lambda e: e.activation(out=xbb[b][:], in_=xt[b4][:], func=AF.Copy, scale=rsA[:, b:b + 1]), reads=[f"xt{b4}", f"rsA{b}"], writes=[f"xb{b}"])

                def a1(ti):
                    b = ti % 2
                    for c in range(8):
                        P.op("pe", lambda e, c=c: e.transpose(out=pT[:, c, :], in_=xbb[b][:, c * 128:(c + 1) * 128], identity=identb[:]), reads=[f"xb{b}", "identb"], writes=["pT"])
                    P.op("dve", lambda e: e.tensor_copy(out=xT[:], in_=pT[:]), reads=["pT"], writes=["xT"])

                def proj(blk, bank):
                    for c in range(8):
                        P.op("pe", lambda e, c=c: e.matmul(pP[:, bank, :], lhsT=xT[:, c, :], rhs=Wb2[:, c, blk * 512:(blk + 1) * 512], start=(c == 0), stop=(c == 7)),
                             reads=["xT", "Wb2"], writes=[f"pP{bank}"])

                def a2(ti):
                    b = ti % 2
                    proj(0, 0)
                    proj(1, 1)
                    rtb = rt[:, b, :, :]
                    pk_rt = [f"rt{b}"]
                    pk = pP[:, 0, :].rearrange("p (h d) -> p h d", h=8)
                    def rot(bank, tab, out1, out2, wtags):
                        pk = pP[:, bank, :].rearrange("p (h d) -> p h d", h=8)
                        cosb = tab[:, 0:32].unsqueeze(1).broadcast_to([128, 8, 32])
                        sinb = tab[:, 32:64].unsqueeze(1).broadcast_to([128, 8, 32])
                        rtag = [f"pP{bank}", f"rt{b}"]
                        T = rtmp[bank]
                        tg = [f"rtmp{bank}{i}" for i in range(4)]
                        P.op("dve", lambda e: e.tensor_tensor(out=T[0][:], in0=pk[:, :, 0:32], in1=cosb, op=ALU.mult), reads=rtag, writes=[tg[0]])
                        P.op("dve", lambda e: e.tensor_tensor(out=T[1][:], in0=pk[:, :, 32:64], in1=sinb, op=ALU.mult), reads=rtag, writes=[tg[1]])
                        P.op("dve", lambda e: e.tensor_tensor(out=T[2][:], in0=pk[:, :, 0:32], in1=sinb, op=ALU.mult), reads=rtag, writes=[tg[2]])
                        P.op("dve", lambda e: e.tensor_tensor(out=T[3][:], in0=pk[:, :, 32:64], in1=cosb, op=ALU.mult), reads=rtag, writes=[tg[3]])
                        P.op("pool", lambda e: e.tensor_tensor(out=out1, in0=T[0][:], in1=T[1][:], op=ALU.subtract), reads=[tg[0], tg[1]], writes=[wtags[0]])
                        P.op("pool", lambda e: e.tensor_tensor(out=out2, in0=T[2][:], in1=T[3][:], op=ALU.add), reads=[tg[2], tg[3]], writes=[wtags[1]])
                    rot(0, rt[:, b, 0, :], qrb[:, :, 0:32], qrb[:, :, 32:64], ["qrb1", "qrb2"])
                    rot(1, rt[:, b, 1, :], kr[:, :, 0:32], kr[:, :, 32:64], ["kr1", "kr2"])
                    P.op("act", lambda e: e.activation(out=krb[:], in_=kr[:].rearrange("p h d -> p (h d)"), func=AF.Copy), reads=["kr1", "kr2"], writes=["krb"])
                    P.op("pool", lambda e: e.tensor_tensor(out=ktil[b][:], in0=kr[:].rearrange("p h d -> p (h d)"), in1=kdec[:], op=ALU.mult), reads=["kr1", "kr2", "kdec"], writes=[f"ktil{b}"])

                def a3(ti):
                    b = ti % 2
                    proj(2, 0)
                    proj(3, 1)
                    P.op("act", lambda e: e.activation(out=vb[b][:], in_=pP[:, 0, :], func=AF.Copy), reads=["pP0"], writes=[f"vb{b}"])
                    P.op("act", lambda e: e.activation(out=sg[b][:], in_=pP[:, 1, :], func=AF.Silu), reads=["pP1"], writes=[f"sg{b}"])
                    P.op("pool", lambda e: e.tensor_tensor(out=sg[b][:], in0=sg[b][:], in1=rnw[:], op=ALU.mult), reads=[f"sg{b}", "rnw"], writes=[f"sg{b}"])

                def a4(ti):
                    b = ti % 2
                    for j in range(4):
                        P.op("pe", lambda e, j=j: e.transpose(out=pT[:, j, :], in_=qrb[:].rearrange("p h d -> p (h d)")[:, j * 128:(j + 1) * 128], identity=identb[:]),
                             reads=["qrb1", "qrb2", "identb"], writes=["pT"])
                    for j in range(4):
                        P.op("pe", lambda e, j=j: e.transpose(out=pT[:, 4 + j, :], in_=krb[:, j * 128:(j + 1) * 128], identity=identb[:]), reads=["krb", "identb"], writes=["pT"])
                    P.op("act", lambda e: e.activation(out=qT[b][:], in_=pT[:, 0:4, :], func=AF.Copy), reads=["pT"], writes=[f"qT{b}"])
                    P.op("act", lambda e: e.activation(out=kT[b][:], in_=pT[:, 4:8, :], func=AF.Copy), reads=["pT"], writes=[f"kT{b}"])
                    P.op("pool", lambda e: e.tensor_tensor(out=qTd[b][:], in0=qT[b][:], in1=qdec[:], op=ALU.mult), reads=[f"qT{b}", "qdec"], writes=[f"qTd{b}"])

                def b1(ti):
                    b = ti % 2
                    for h in range(8):
                        j, hl = h // 2, h % 2
                        sl = slice(64 * hl, 64 * hl + 64)
                        P.op("pe", lambda e, j=j, hl=hl, sl=sl: e.matmul(pS[:, hl, j * 128:(j + 1) * 128], lhsT=kT[b][sl, j, :], rhs=qT[b][sl, j, :], start=True, stop=True),
                             reads=[f"kT{b}", f"qT{b}"], writes=[f"pS{hl}"])
                    for hl in range(2):
                        P.op("dve", lambda e, hl=hl: e.tensor_tensor(out=PT[:].rearrange("p (j l) t -> p j l t", l=2)[:, :, hl, :],
                                                                     in0=pS[:, hl, :].rearrange("p (j t) -> p j t", j=4),
                                                                     in1=decT[:].rearrange("p (j l) t -> p j l t", l=2)[:, :, hl, :], op=ALU.mult),
                             reads=[f"pS{hl}", "decT"], writes=[f"PT{hl}"])

                def b2(ti):
                    b = ti % 2
                    for h in range(8):
                        P.op("pe", lambda e, h=h: e.matmul(pO[:, h * 64:(h + 1) * 64], lhsT=PT[:, h, :], rhs=vb[b][:, h * 64:(h + 1) * 64], start=(h == 0), stop=False,
                                                           skip_group_check=True), reads=["PT0", "PT1", f"vb{b}"], writes=["pO"])
                    for j in range(4):
                        P.op("pe", lambda e, j=j: e.matmul(pO[:, j * 128:(j + 1) * 128], lhsT=qTd[b][:, j, :], rhs=Rb[:, j, :], start=False, stop=True,
                                                           skip_group_check=True), reads=[f"qTd{b}", "Rb"], writes=["pO"])
                    import os
                    SUB = int(os.environ.get("P2SUB", "9"))
                    if SUB < 1:
                        return
                    pkv = pK[:, 0:256].rearrange("p (j e) -> p j e", j=4)
                    for h in range(8):
                        j, hl = h // 2, h % 2
                        P.op("pe", lambda e, h=h, j=j, hl=hl: e.matmul(pkv[64 * hl:64 * hl + 64, j, :], lhsT=ktil[b][:, h * 64:(h + 1) * 64], rhs=vb[b][:, h * 64:(h + 1) * 64],
                                                                       start=True, stop=True), reads=[f"ktil{b}", f"vb{b}"], writes=["pKkv"])
                    for j in range(4):
                        P.op("dve", lambda e, j=j: e.scalar_tensor_tensor(out=R[:, j, :], in0=R[:, j, :], scalar=gam128[:, j:j + 1], in1=pkv[:, j, :], op0=ALU.mult, op1=ALU.add),
                             reads=["pKkv", "gam128", "R"], writes=["R"])
                    if SUB < 2:
                        return
                    upd_Rb()
                    if SUB < 3:
                        return
                    o3 = pO[:].rearrange("p (h d) -> p h d", h=8)
                    P.op("dve", lambda e: e.tensor_reduce(out=s1[:], in_=o3, axis=AX.X, op=ALU.add), reads=["pO"], writes=["s1"])
                    P.op("act", lambda e: e.activation(out=osq[:], in_=pO[:], func=AF.Square), reads=["pO"], writes=["osq"])
                    P.op("dve", lambda e: e.tensor_reduce(out=s2[:], in_=osq[:].rearrange("p (h d) -> p h d", h=8), axis=AX.X, op=ALU.add), reads=["osq"], writes=["s2"])
                    P.op("dve", lambda e: e.tensor_scalar(out=mean[:], in0=s1[:], scalar1=1.0 / 64, scalar2=None, op0=ALU.mult), reads=["s1"], writes=["mean"])
                    P.op("dve", lambda e: e.tensor_tensor(out=var[:], in0=mean[:], in1=mean[:], op=ALU.mult), reads=["mean"], writes=["var"])
                    P.op("dve", lambda e: e.scalar_tensor_tensor(out=var[:], in0=s2[:], scalar=1.0 / 64, in1=var[:], op0=ALU.mult, op1=ALU.subtract), reads=["s2", "var"], writes=["var"])
                    P.op("dve", lambda e: e.tensor_scalar(out=var[:], in0=var[:], scalar1=EPS, scalar2=None, op0=ALU.add), reads=["var"], writes=["var"])
                    P.op("pool", lambda e: e.tensor_tensor(out=rstd[:], in0=var[:], in1=mh8[:], op=ALU.pow), reads=["var", "mh8"], writes=["rstd"])
                    P.op("dve", lambda e: e.tensor_tensor(out=oc[:], in0=o3, in1=mean[:].unsqueeze(2).broadcast_to([128, 8, 64]), op=ALU.subtract), reads=["pO", "mean"], writes=["oc"])
                    P.op("dve", lambda e: e.tensor_tensor(out=oc[:], in0=oc[:], in1=rstd[:].unsqueeze(2).broadcast_to([128, 8, 64]), op=ALU.mult), reads=["oc", "rstd"], writes=["oc"])
                    P.op("dve", lambda e: e.tensor_tensor(out=yret[:], in0=oc[:].rearrange("p h d -> p (h d)"), in1=sg[b][:], op=ALU.mult), reads=["oc", f"sg{b}"], writes=["yret"])

                def b3(ti):
                    b = ti % 2
                    for j in range(4):
                        P.op("pe", lambda e, j=j: e.transpose(out=pT[:, j, :], in_=yret[:, j * 128:(j + 1) * 128], identity=identb[:]), reads=["yret", "identb"], writes=["pT"])
                    P.op("act", lambda e: e.activation(out=yretT[b][:], in_=pT[:, 0:4, :], func=AF.Copy), reads=["pT"], writes=[f"yretT{b}"])

                def c1(ti):
                    b3_, b = ti % 4, ti % 2
                    rows = slice(ti * 128, (ti + 1) * 128)
                    for nh in range(2):
                        cs = slice(nh * 512, (nh + 1) * 512)
                        for c in range(4):
                            P.op("pe", lambda e, c=c, cs=cs: e.matmul(pH[:], lhsT=mixT[:, c, rows], rhs=Wo[:, c, cs], start=(c == 0), stop=False), reads=["Wo"], writes=["pH"])
                        for c in range(4):
                            P.op("pe", lambda e, c=c, cs=cs: e.matmul(pH[:], lhsT=yretT[b][:, c, :], rhs=Wo[:, 4 + c, cs], start=False, stop=(c == 3)),
                                 reads=["Wo", f"yretT{b}"], writes=["pH"])
                        P.op("dve", lambda e, cs=cs: e.tensor_tensor(out=hh[:, cs], in0=pH[:], in1=xt[b3_][:, cs], op=ALU.add), reads=["pH", f"xt{b3_}"], writes=[f"hh{nh}"])
                    P.op("sp", lambda e: e.dma_start(out=d_hbuf[rows, :], in_=hh[:]), reads=["hh0", "hh1"], writes=[f"d_hbuf{ti}"], dma=True)
                    if dbg == "h":
                        P.op("sp", lambda e: e.dma_start(out=dbg_out[rows, :], in_=hh[:]), reads=["hh0", "hh1"], writes=[f"dbg{ti}"], dma=True)
                    P.op("act", lambda e: e.activation(out=hn[:], in_=hh[:], func=AF.Square, accum_out=ssC[:]), reads=["hh0", "hh1"], writes=["hn", "ssC"])
                    P.op("dve", lambda e: e.tensor_scalar(out=msC[:], in0=ssC[:], scalar1=1.0 / D, scalar2=EPS, op0=ALU.mult, op1=ALU.add), reads=["ssC"], writes=["msC"])
                    P.op("pool", lambda e: e.tensor_tensor(out=rsC[:], in0=msC[:], in1=mhalf[:], op=ALU.pow), reads=["msC", "mhalf"], writes=["rsC"])
                    P.op("dve", lambda e: e.scalar_tensor_tensor(out=hn[:], in0=hh[:], scalar=rsC[:], in1=n2w[:], op0=ALU.mult, op1=ALU.mult), reads=["hh0", "hh1", "rsC", "n2w"], writes=["hn"])
                    P.op("act", lambda e: e.activation(out=hnb[b][:], in_=hn[:], func=AF.Copy), reads=["hn"], writes=[f"hnb{b}"])

                def c2(ti):
                    banks = [(pH, "pH"), (pO, "pO")]
                    for half in range(2):
                        bk, btag = banks[half]
                        bk3 = bk[:].rearrange("p (c t) -> p c t", c=4)
                        for c in range(4):
                            cc = 4 * half + c
                            P.op("pe", lambda e, c=c, cc=cc, bk3=bk3: e.transpose(out=bk3[:, c, :], in_=hn[:, cc * 128:(cc + 1) * 128], identity=identf[:]), reads=["hn", "identf"], writes=[btag])
                    for half in range(2):
                        bk, btag = banks[half]
                        bk3 = bk[:].rearrange("p (c t) -> p c t", c=4)
                        P.op("act", lambda e, half=half, bk3=bk3: e.activation(out=hnT[:, 4 * half:4 * half + 4, :], in_=bk3, func=AF.Copy), reads=[btag], writes=[f"hnT{half}"])

                def c3(ti):
                    b = ti % 2
                    for c in range(8):
                        P.op("pe", lambda e, c=c: e.matmul(pH[:, 0:36], lhsT=hnT[:, c, :], rhs=Wr[:, c, :], start=(c == 0), stop=(c == 7)), reads=["hnT0", "hnT1", "Wr"], writes=["pH"])
                    P.op("dve", lambda e: e.tensor_tensor(out=lg[:], in0=pH[:, 0:36], in1=bfull[:], op=ALU.add), reads=["pH", "bfull"], writes=["lg"])
                    c_ = lambda i: sm[:, i:i + 1]
                    import os
                    C3SUB = int(os.environ.get("C3SUB", "9"))
                    if C3SUB < 2:
                        return
                    P.op("dve", lambda e: e.tensor_reduce(out=c_(0), in_=lg[:, 0:4], axis=AX.X, op=ALU.max), reads=["lg"], writes=["sm0"])
                    P.op("dve", lambda e: e.tensor_scalar(out=goh[:], in0=lg[:, 0:4], scalar1=c_(0), scalar2=None, op0=ALU.is_equal), reads=["lg", "sm0"], writes=["goh"])
                    P.op("dve", lambda e: e.tensor_scalar(out=pen[:], in0=goh[:], scalar1=BIG, scalar2=-BIG, op0=ALU.mult, op1=ALU.add), reads=["goh"], writes=["pen"])
                    P.op("dve", lambda e: e.tensor_tensor(out=em[:].rearrange("p (g k) -> p g k", g=4), in0=lg[:, 4:36].rearrange("p (g k) -> p g k", g=4),
                                                          in1=pen[:].unsqueeze(2).broadcast_to([128, 4, 8]), op=ALU.add), reads=["lg", "pen"], writes=["em"])
                    P.op("dve", lambda e: e.tensor_reduce(out=c_(4), in_=em[:], axis=AX.X, op=ALU.max), reads=["em"], writes=["sm4"])
                    P.op("dve", lambda e: e.tensor_scalar(out=oh1, in0=em[:], scalar1=c_(4), scalar2=None, op0=ALU.is_equal), reads=["em", "sm4"], writes=["oh1"])
                    P.op("dve", lambda e: e.scalar_tensor_tensor(out=em2[:], in0=oh1, scalar=-BIG, in1=em[:], op0=ALU.mult, op1=ALU.add), reads=["oh1", "em"], writes=["em2"])
                    P.op("dve", lambda e: e.tensor_reduce(out=c_(5), in_=em2[:], axis=AX.X, op=ALU.max), reads=["em2"], writes=["sm5"])
                    P.op("dve", lambda e: e.tensor_scalar(out=oh2, in0=em2[:], scalar1=c_(5), scalar2=None, op0=ALU.is_equal), reads=["em2", "sm5"], writes=["oh2"])
                    P.op("dve", lambda e: e.tensor_scalar(out=ex5[:, 0:4], in0=lg[:, 0:4], scalar1=c_(0), scalar2=None, op0=ALU.subtract), reads=["lg", "sm0"], writes=["ex5a"])
                    P.op("dve", lambda e: e.tensor_tensor(out=ex5[:, 4:5], in0=c_(5), in1=c_(4), op=ALU.subtract), reads=["sm4", "sm5"], writes=["ex5b"])
                    P.op("act", lambda e: e.activation(out=ex5[:], in_=ex5[:], func=AF.Sigmoid), reads=["ex5a", "ex5b"], writes=["ex5s"])
                    P.op("dve", lambda e: e.tensor_scalar(out=ex5t[:], in0=ex5[:], scalar1=-1.0, scalar2=1.0, op0=ALU.mult, op1=ALU.add), reads=["ex5s"], writes=["ex5t"])
                    P.op("dve", lambda e: e.reciprocal(out=ex5t[:], in_=ex5t[:]), reads=["ex5t"], writes=["ex5t"])
                    P.op("dve", lambda e: e.tensor_tensor(out=ex5[:], in0=ex5[:], in1=ex5t[:], op=ALU.mult), reads=["ex5s", "ex5t"], writes=["ex5"])
                    P.op("dve", lambda e: e.tensor_reduce(out=c_(2), in_=ex5[:, 0:4], axis=AX.X, op=ALU.add), reads=["ex5"], writes=["sm2"])
                    P.op("dve", lambda e: e.reciprocal(out=c_(3), in_=c_(2)), reads=["sm2"], writes=["sm3"])
                    P.op("dve", lambda e: e.tensor_scalar(out=c_(8), in0=ex5[:, 4:5], scalar1=1.0, scalar2=None, op0=ALU.add), reads=["ex5"], writes=["sm8"])
                    P.op("dve", lambda e: e.reciprocal(out=c_(8), in_=c_(8)), reads=["sm8"], writes=["sm8"])
                    P.op("dve", lambda e: e.tensor_tensor(out=c_(9), in0=ex5[:, 4:5], in1=c_(8), op=ALU.mult), reads=["ex5", "sm8"], writes=["sm9"])
                    P.op("dve", lambda e: e.tensor_tensor(out=maskb[:], in0=oh1, in1=oh2, op=ALU.add), reads=["oh1", "oh2"], writes=["maskb"])

                def dstage(ti):
                    b = ti % 2
                    c_ = lambda i: sm[:, i:i + 1]
                    P.op("pe", lambda e: e.matmul(pH[:, 64:96], lhsT=triub[:], rhs=maskb[:], start=True, stop=True), reads=["triub", "maskb", "lg"], writes=["pH", "pHp"])
                    P.op("pe", lambda e: e.matmul(pH[:, 128:160], lhsT=onesb[:], rhs=maskb[:], start=True, stop=True), reads=["onesb", "maskb", "lg"], writes=["pH", "pHc"])
                    P.op("dve", lambda e: e.tensor_tensor(out=posf[:], in0=pH[:, 64:96], in1=base_b[:], op=ALU.add), reads=["pHp", "base_b"], writes=["posf"])
                    P.op("dve", lambda e: e.tensor_tensor(out=base_b[:], in0=pH[:, 128:160], in1=base_b[:], op=ALU.add), reads=["pHc", "base_b", "posf"], writes=["base_b"])
                    P.op("pool", lambda e: e.memset(ss_dummy[:], 0.0), reads=["posf", "base_b"], writes=["pH"])
                    P.op("dve", lambda e: e.tensor_tensor(out=okm[:], in0=posf[:], in1=ecapl[:], op=ALU.is_lt), reads=["posf", "ecapl"], writes=["okm"])
                    P.op("dve", lambda e: e.tensor_tensor(out=t2[:], in0=oh12[:], in1=posf[:].unsqueeze(1).broadcast_to([128, 2, 32]), op=ALU.mult), reads=["oh1", "oh2", "posf"], writes=["t2"])
                    P.op("dve", lambda e: e.tensor_reduce(out=d2[:], in_=t2[:], axis=AX.X, op=ALU.add), reads=["t2"], writes=["d2"])
                    P.op("dve", lambda e: e.tensor_tensor(out=t2[:], in0=oh12[:], in1=okm[:].unsqueeze(1).broadcast_to([128, 2, 32]), op=ALU.mult), reads=["oh1", "oh2", "okm", "d2"], writes=["t2"])
                    P.op("dve", lambda e: e.tensor_reduce(out=ok2[:], in_=t2[:], axis=AX.X, op=ALU.add), reads=["t2"], writes=["ok2"])
                    P.op("dve", lambda e: e.scalar_tensor_tensor(out=gates[:, ti, :], in0=sm[:, 8:10], scalar=c_(3), in1=ok2[:], op0=ALU.mult, op1=ALU.mult),
                         reads=["sm8", "sm9", "sm3", "ok2"], writes=[f"gates{ti}"])
                    P.op("dve", lambda e: e.tensor_tensor(out=dstf[:], in0=d2[:], in1=ok2[:], op=ALU.mult), reads=["d2", "ok2"], writes=["dstf"])
                    P.op("dve", lambda e: e.tensor_scalar(out=pad2[:], in0=ok2[:], scalar1=-1.0e6, scalar2=1.0e6, op0=ALU.mult, op1=ALU.add), reads=["ok2"], writes=["pad2"])
                    P.op("dve", lambda e: e.tensor_tensor(out=dstf[:], in0=dstf[:], in1=pad2[:], op=ALU.add), reads=["dstf", "pad2"], writes=["dstf"])
                    P.op("dve", lambda e: e.tensor_copy(out=dests[:, ti, :], in_=dstf[:]), reads=["dstf"], writes=[f"dests{ti}"])
                    import os
                    for k_ in range(2 if os.environ.get("NOSCAT") is None else 0):
                        P.op("pool", lambda e, k_=k_: e.indirect_dma_start(
                            out=d_xbuf, out_offset=bass.IndirectOffsetOnAxis(ap=dests[:, ti, k_:k_ + 1].bitcast(U32), axis=0),
                            in_=hnb[b][:], in_offset=None, bounds_check=bc_reg(e), oob_is_err=False), reads=[f"dests{ti}", f"hnb{b}"], writes=[f"d_xbuf{ti}_{k_}"], dma=True)

                nt2 = p2tiles if p2tiles else ntile
                import os
                STG = os.environ.get("P2STAGES", "a1,a2,a3,a4,b1,b2,b3,c1,c2,c3").split(",")
                _fn = dict(a1=a1, a2=a2, a3=a3, a4=a4, b1=b1, b2=b2, b3=b3, c1=c1, c2=c2, c3=c3)
                def wrap(name):
                    f = _fn[name]
                    return f if name in STG else (lambda t: None)
                a1, a2, a3, a4, b1, b2, b3, c1, c2, c3 = [wrap(n) for n in ("a1", "a2", "a3", "a4", "b1", "b2", "b3", "c1", "c2", "c3")]
                ok = lambda t: 0 <= t < nt2
                if "a1" in STG:
                    a0(0)
                for step in range(nt2 + 4):
                    tA, tB, tC, tD = step, step - 1, step - 2, step - 3
                    if ok(tA - 1): a4(tA - 1)
                    if ok(tC): b3(tC)
                    if ok(tA): a1(tA)
                    if ok(tB): b1(tB)
                    if ok(tD) and dbg != "h": c3(tD)
                    if ok(tA + 1) and "a1" in STG: a0(tA + 1)
                    if ok(tA): a2(tA)
                    if ok(tC): c1(tC)
                    if ok(tB): b2(tB)
                    if ok(tC) and dbg != "h": c2(tC)
                    if ok(tA): a3(tA)
                    if ok(tD) and dbg != "h" and "c3" in STG: dstage(tD)
                P.emit()
            if dbg in ("h", "p2"):
                return nc
        with ExitStack() as st:
            sb, ps = mk(st)
            Wgu = [sb(f"Wgu{i}", [128, 8, 1024], BF16) for i in range(2)]
            Wd = [sb(f"Wd{i}", [128, 4, D], BF16) for i in range(2)]
            xg = [sb(f"xg{i}", [128, 3, D], BF16) for i in range(2)]
            XT = [sb(f"XT{i}", [128, 8, CAP], BF16) for i in range(2)]
            hidT = sb("hidT", [128, 4, CAP], BF16)
            sil = [sb(f"sil{i}", [128, CAP]) for i in range(2)]
            yo = [sb(f"yo{i}", [128, 512], BF16) for i in range(4)]
            NSTG = 8
            stg = [sb(f"stg{i}", [128, 2048]) for i in range(NSTG)]
            pX = ps("pX", [128, 8, 128], BF16)
            pG = [ps(f"pG{i}", [128, 512]) for i in range(2)]
            pU = [ps(f"pUp{i}", [128, 512]) for i in range(2)]
            pDn = ps("pDn", [128, 3, 512])
            stg_i = [0]
            cast_i = [0]

            def wload(ex):
                b = ex % 2
                casts = []
                specs = []
                for (wname, off) in (("moe_w_gate", 0), ("moe_w_up", 512)):
                    for hc in range(2):
                        specs.append((wname, hc, 4, 512, lambda b=b, hc=hc, off=off: Wgu[b][:, 4 * hc:4 * hc + 4, off:off + 512], f"Wgu{b}"))
                for hc in range(2):
                    specs.append(("moe_w_down", hc, 2, 256, lambda b=b, hc=hc: Wd[b][:, 2 * hc:2 * hc + 2, :], f"Wd{b}"))
                for (wname, hc, cc, rows_, dstf, wtag) in specs:
                    sbi = stg_i[0] % NSTG
                    stg_i[0] += 1
                    P.op("sp", lambda e, ex=ex, wname=wname, hc=hc, sbi=sbi, cc=cc, rows_=rows_: e.dma_start(
                        out=stg[sbi][:].rearrange("p (c n) -> p c n", c=cc), in_=I[wname][ex, hc * rows_:(hc + 1) * rows_, :].rearrange("(c p) n -> p c n", p=128)),
                        writes=[f"stg{sbi}"], dma=True)

                    def cast(sbi=sbi, cc=cc, dstf=dstf, wtag=wtag):
                        ceng = ("act", "dve")[cast_i[0] % 2]
                        cast_i[0] += 1
                        srcv = stg[sbi][:].rearrange("p (c n) -> p c n", c=cc)
                        if ceng == "act":
                            P.op("act", lambda e: e.activation(out=dstf(), in_=srcv, func=AF.Copy), reads=[f"stg{sbi}"], writes=[wtag])
                        else:
                            P.op("dve", lambda e: e.tensor_copy(out=dstf(), in_=srcv), reads=[f"stg{sbi}"], writes=[wtag])
                    casts.append(cast)
                P.op("sp", lambda e, ex=ex, b=b: e.dma_start(out=xg[b][:], in_=d_xbuf[ex * CAP:(ex + 1) * CAP, :].rearrange("(s p) d -> p s d", p=128)), writes=[f"xg{b}"], dma=True)
                return casts

            def transposes(ex):
                b = ex % 2
                for s_ in range(3):
                    for c in range(8):
                        P.op("pe", lambda e, b=b, s_=s_, c=c: e.transpose(out=pX[:, c, :], in_=xg[b][:, s_, c * 128:(c + 1) * 128], identity=identb[:]), reads=[f"xg{b}", "identb"], writes=["pX"])
                    if s_ % 2 == 0:
                        P.op("dve", lambda e, s_=s_, b=b: e.tensor_copy(out=XT[b][:, :, s_ * 128:(s_ + 1) * 128], in_=pX[:]), reads=["pX"], writes=[f"XT{b}"])
                    else:
                        P.op("act", lambda e, s_=s_, b=b: e.activation(out=XT[b][:, :, s_ * 128:(s_ + 1) * 128], in_=pX[:], func=AF.Copy), reads=["pX"], writes=[f"XT{b}"])

            pending = wload(0)
            for cst in pending:
                cst()
            transposes(0)
            yo_i = [0]
            for ex in range(NEXP):
                b = ex % 2
                pending = wload(ex + 1) if ex + 1 < NEXP else []
                for m in range(4):
                    mb = m % 2
                    for c in range(8):
                        P.op("pe", lambda e, b=b, m=m, c=c, mb=mb: e.matmul(pG[mb][:, 0:CAP], lhsT=Wgu[b][:, c, m * 128:(m + 1) * 128], rhs=XT[b][:, c, :], start=(c == 0), stop=(c == 7)),
                             reads=[f"Wgu{b}", f"XT{b}"], writes=[f"pG{mb}"])
                    for c in range(8):
                        P.op("pe", lambda e, b=b, m=m, c=c, mb=mb: e.matmul(pU[mb][:, 0:CAP], lhsT=Wgu[b][:, c, 512 + m * 128:512 + (m + 1) * 128], rhs=XT[b][:, c, :], start=(c == 0), stop=(c == 7)),
                             reads=[f"Wgu{b}", f"XT{b}"], writes=[f"pUp{mb}"])
                    P.op("act", lambda e, mb=mb: e.activation(out=sil[mb][:], in_=pG[mb][:, 0:CAP], func=AF.Silu), reads=[f"pG{mb}"], writes=[f"sil{mb}"])
                    P.op("dve", lambda e, mb=mb, m=m: e.tensor_tensor(out=hidT[:, m, :], in0=pU[mb][:, 0:CAP], in1=sil[mb][:], op=ALU.mult), reads=[f"pUp{mb}", f"sil{mb}"], writes=["hidT"])
                    for cst in pending[m:m + 1] + (pending[4:6] if m == 3 else []):
                        cst()
                if ex + 1 < NEXP:
                    transposes(ex + 1)
                for s_ in range(3):
                    for nh in range(2):
                        bk = (2 * s_ + nh) % 3
                        for m in range(4):
                            P.op("pe", lambda e, b=b, s_=s_, nh=nh, m=m, bk=bk: e.matmul(pDn[:, bk, :], lhsT=hidT[:, m, s_ * 128:(s_ + 1) * 128], rhs=Wd[b][:, m, nh * 512:(nh + 1) * 512],
                                                                                        start=(m == 0), stop=(m == 3)), reads=["hidT", f"Wd{b}"], writes=[f"pDn{bk}"])
                        yb = yo_i[0] % 4
                        yo_i[0] += 1
                        if yb % 2 == 0:
                            P.op("act", lambda e, yb=yb, bk=bk: e.activation(out=yo[yb][:], in_=pDn[:, bk, :], func=AF.Copy), reads=[f"pDn{bk}"], writes=[f"yo{yb}"])
                        else:
                            P.op("dve", lambda e, yb=yb, bk=bk: e.tensor_copy(out=yo[yb][:], in_=pDn[:, bk, :]), reads=[f"pDn{bk}"], writes=[f"yo{yb}"])
                        r0 = ex * CAP + s_ * 128
                        P.op("act", lambda e, yb=yb, r0=r0, nh=nh: e.dma_start(out=d_ybuf[r0:r0 + 128, nh * 512:(nh + 1) * 512], in_=yo[yb][:]), reads=[f"yo{yb}"], writes=[f"d_ybuf{r0}_{nh}"], dma=True)
            P.emit()
        with ExitStack() as st:
            sb, ps = mk(st)
            NB4 = 4
            hh = [sb(f"hh{i}", [128, D]) for i in range(NB4)]
            y0 = [sb(f"y0{i}", [128, D], BF16) for i in range(NB4)]
            y1 = [sb(f"y1{i}", [128, D], BF16) for i in range(NB4)]
            ob = [sb(f"ob{i}", [128, D]) for i in range(NB4)]
            fnw = sb("fnw", [128, D])
            ss = sb("ss", [128, 1]); ms = sb("ms", [128, 1]); rs = sb("rs", [128, 1])
            P.op("sp", lambda e: e.dma_start(out=fnw[:], in_=I["final_norm_w"].partition_broadcast(128)), writes=["fnw"], dma=True)
            for i in range(NB4):
                P.op("pool", lambda e, i=i: e.memset(y0[i][:], 0.0), writes=[f"y0{i}"])
                P.op("pool", lambda e, i=i: e.memset(y1[i][:], 0.0), writes=[f"y1{i}"])
            def loads4(ti):
                b = ti % NB4
                rows = slice(ti * 128, (ti + 1) * 128)
                P.op("sp", lambda e, b=b, rows=rows: e.dma_start(out=hh[b][:], in_=d_hbuf[rows, :]), writes=[f"hh{b}"], dma=True)
                for k_, yy in enumerate((y0, y1)):
                    P.op("pool", lambda e, k_=k_, yy=yy, b=b, ti=ti: e.indirect_dma_start(
                        out=yy[b][:], out_offset=None, in_=d_ybuf, in_offset=bass.IndirectOffsetOnAxis(ap=dests[:, ti, k_:k_ + 1].bitcast(U32), axis=0),
                        bounds_check=bc_reg(e), oob_is_err=False), writes=[f"y{k_}{b}"], dma=True)

            for ti in range(min(NB4 - 1, ntile)):
                loads4(ti)
            for ti in range(ntile):
                b = ti % NB4
                rows = slice(ti * 128, (ti + 1) * 128)
                if ti + NB4 - 1 < ntile:
                    loads4(ti + NB4 - 1)
                P.op("dve", lambda e, b=b, ti=ti: e.scalar_tensor_tensor(out=hh[b][:], in0=y0[b][:], scalar=gates[:, ti, 0:1], in1=hh[b][:], op0=ALU.mult, op1=ALU.add),
                     reads=[f"y0{b}", f"hh{b}"], writes=[f"hh{b}"])
                P.op("dve", lambda e, b=b, ti=ti: e.scalar_tensor_tensor(out=hh[b][:], in0=y1[b][:], scalar=gates[:, ti, 1:2], in1=hh[b][:], op0=ALU.mult, op1=ALU.add),
                     reads=[f"y1{b}", f"hh{b}"], writes=[f"hh{b}"])
                P.op("act", lambda e, b=b: e.activation(out=ob[b][:], in_=hh[b][:], func=AF.Square, accum_out=ss[:]), reads=[f"hh{b}"], writes=[f"ob{b}", "ss"])
                P.op("dve", lambda e: e.tensor_scalar(out=ms[:], in0=ss[:], scalar1=1.0 / D, scalar2=EPS, op0=ALU.mult, op1=ALU.add), reads=["ss"], writes=["ms"])
                P.op("pool", lambda e: e.tensor_tensor(out=rs[:], in0=ms[:], in1=mhalf[:], op=ALU.pow), reads=["ms", "mhalf"], writes=["rs"])
                P.op("dve", lambda e, b=b: e.scalar_tensor_tensor(out=ob[b][:], in0=hh[b][:], scalar=rs[:], in1=fnw[:], op0=ALU.mult, op1=ALU.mult), reads=[f"hh{b}", "rs", "fnw"], writes=[f"ob{b}"])
                P.op("sp", lambda e, b=b, rows=rows: e.dma_start(out=out[rows, :], in_=ob[b][:]), reads=[f"ob{b}"], writes=["out"], dma=True)
            P.emit()
    return nc


def make_in_maps(inputs):
    x = np.asarray(inputs["x"], dtype=np.float32)
    params = {}
    for k in PARAM_SHAPES:
        a = np.asarray(inputs[k], dtype=np.float32)
        if k != "final_norm_w":
            a = a[0]
        params[k] = np.ascontiguousarray(a)
    consts = [host_consts(0), host_consts(1)]
    zeros = np.zeros((TOK, D), np.float32)
    in_maps = []
    for c in range(NCORES):
        b, half = c // 2, c % 2
        m = {"x": np.ascontiguousarray(x[b, half * TOK:(half + 1) * TOK]),
             "xpre": np.ascontiguousarray(x[b, 0:TOK]) if half == 1 else zeros}
        m.update(params)
        m.update(consts[half])
        in_maps.append(m)
    return in_maps


def kernel(**inputs):
    in_maps = make_in_maps(inputs)
    nc = build_nc()
    res = run_bass_kernel_spmd(nc, in_maps, core_ids=list(range(NCORES)))
    out = np.zeros((4, 2 * TOK, D), np.float32)
    for c in range(NCORES):
        b, half = c // 2, c % 2
        out[b, half * TOK:(half + 1) * TOK] = res.results[c]["out"]
    return out
```

```python
import math
from contextlib import ExitStack

import numpy as np
import concourse.bass as bass
import concourse.mybir as mybir
from concourse.bass_utils import run_bass_kernel_spmd

F32 = mybir.dt.float32
BF16 = mybir.dt.bfloat16
I32 = mybir.dt.int32
U32 = mybir.dt.uint32
ALU = mybir.AluOpType
AF = mybir.ActivationFunctionType
AX = mybir.AxisListType

NCORES = 8
TOK = 4096
NT = TOK // 128
D = 1024
NSUB = TOK // 8
CAP = 384
NEXP = 32
EPS = 1e-6
TWO_PI = 2.0 * math.pi
HS_STEPS = 9

ENGS = ("pe", "act", "dve", "pool", "sp")
NDMA = 16


class Prog:
    def __init__(self, nc, stack):
        self.nc = nc
        self.sem = {e: stack.enter_context(nc.semaphore("s_" + e)) for e in ENGS}
        self.dsem = {q: [stack.enter_context(nc.semaphore(f"d_{q}{i}")) for i in range(NDMA)]
                     for q in ("sp", "pool", "act")}
        self.cnt = {e: 0 for e in ENGS}
        self.dcnt = {q: 0 for q in self.dsem}
        self.phase = 0
        self.reset_phase()

    def reset_phase(self):
        self.phase += 1
        self.ops = []
        self.lw = {}
        self.rd = {}

    def op(self, eng, fn, reads=(), writes=(), dma=False):
        deps = set()
        raw = set()
        for r in reads:
            if r in self.lw:
                deps.add(self.lw[r])
                raw.add(self.lw[r])
        for w in writes:
            if w in self.lw:
                deps.add(self.lw[w])
            deps.update(self.rd.get(w, ()))
        idx = len(self.ops)
        if not dma:
            deps = {d for d in deps if d in raw or self.ops[d]["eng"] != eng or self.ops[d]["dma"]}
        self.ops.append(dict(eng=eng, fn=fn, deps=deps, dma=dma, need=False))
        for r in reads:
            self.rd.setdefault(r, []).append(idx)
        for w in writes:
            self.lw[w] = idx
            self.rd[w] = []
        return idx

    def emit(self):
        nc = self.nc
        ops = self.ops
        for o in ops:
            if o["eng"] == "pe":
                o["deps"] = {d for d in o["deps"] if ops[d]["eng"] != "pe"}
            for d in o["deps"]:
                ops[d]["need"] = True
        for o in ops:
            e = o["eng"]
            if o["dma"]:
                i = self.dcnt[e]
                self.dcnt[e] += 1
                o["sem"] = self.dsem[e][i % NDMA]
                o["val"] = 16 * (i // NDMA + 1)
                o["prev"] = 16 * (i // NDMA)
            elif o["need"]:
                self.cnt[e] += 1
                o["sem"] = self.sem[e]
                o["val"] = self.cnt[e]
        per = {e: [o for o in ops if o["eng"] == e] for e in ENGS}
        sem_final = [(self.sem[e], self.cnt[e]) for e in ENGS]
        for q in self.dsem:
            n = self.dcnt[q]
            for i in range(NDMA):
                if n > i:
                    sem_final.append((self.dsem[q][i], 16 * ((n - 1 - i) // NDMA + 1)))

        def run(engname, engobj):
            waited = {}

            def w(sem, val):
                if val <= 0:
                    return
                k = id(sem)
                if waited.get(k, 0) >= val:
                    return
                engobj.wait_ge(sem, val)
                waited[k] = val

            for o in per[engname]:
                for d in sorted(o["deps"]):
                    po = ops[d]
                    w(po["sem"], po["val"])
                if o["dma"]:
                    w(o["sem"], o["prev"])
                ins = o["fn"](engobj)
                if o["dma"]:
                    ins.then_inc(o["sem"], 16)
                elif o["need"]:
                    ins.then_inc(o["sem"], 1)
            for sem, val in sem_final:
                w(sem, val)

        with nc.Block() as block:
            @block.tensor
            def _(e):
                run("pe", e)

            @block.scalar
            def _(e):
                run("act", e)

            @block.vector
            def _(e):
                run("dve", e)

            @block.gpsimd
            def _(e):
                run("pool", e)

            @block.sync
            def _(e):
                run("sp", e)
        self.reset_phase()


def host_consts(half):
    c = {}
    c["identf"] = np.eye(128, dtype=np.float32)
    inv = np.power(np.float32(10000.0), -(np.arange(32, dtype=np.float32) / np.float32(32))).astype(np.float32)

    def rot(pos0, scale):
        pos = (pos0 + np.arange(TOK)).astype(np.float32)
        ang = (pos[:, None] * inv[None, :]).astype(np.float32).astype(np.float64)
        t = np.concatenate([np.cos(ang), np.sin(ang)], axis=1) * scale
        return np.ascontiguousarray(t.reshape(NT, 128, 64).transpose(1, 0, 2)).astype(np.float32)

    c["rot_q"] = rot(half * TOK, 1.0)
    c["rot_k"] = rot(half * TOK, 0.125)
    c["rot_kpre"] = rot(0, 0.125)
    gam = 1.0 - 2.0 ** (-5.0 - np.arange(8, dtype=np.float64))
    t = np.arange(128, dtype=np.float64)
    diff = t[None, :] - t[:, None]
    decT = np.where(diff[:, None, :] >= 0, gam[None, :, None] ** np.maximum(diff, 0)[:, None, :], 0.0)
    c["decT"] = decT.astype(np.float32)
    qd = np.zeros((128, 4, 128))
    g128 = np.zeros((128, 4))
    for j in range(4):
        for hl in range(2):
            qd[64 * hl:64 * hl + 64, j, :] = gam[2 * j + hl] ** (t + 1.0)[None, :]
            g128[64 * hl:64 * hl + 64, j] = gam[2 * j + hl] ** 128.0
    c["qdec"] = qd.astype(np.float32)
    c["gam128"] = g128.astype(np.float32)
    kd = np.zeros((128, 8, 64))
    for h in range(8):
        kd[:, h, :] = (gam[h] ** (127.0 - t))[:, None]
    c["kdec"] = kd.reshape(128, 512).astype(np.float32)
    bm = np.zeros((128, 128), np.float32)
    for b in range(8):
        bm[16 * b:16 * b + 16, 16 * b:16 * b + 16] = 1.0
    c["bmask"] = bm
    c["triu"] = np.triu(np.ones((128, 128), np.float32), 1)
    c["ecap"] = np.tile((np.arange(NEXP, dtype=np.float32) * CAP)[None, :], (128, 1))
    klist = list(range(9)) + [8 * 2 ** s_ for s_ in range(1, 9)]
    c["kvals"] = np.tile(np.asarray(klist, np.float32)[None, :, None], (128, 1, 16))
    return c


CONST_SHAPES = {
    "identf": [128, 128], "rot_q": [128, NT, 64], "rot_k": [128, NT, 64], "rot_kpre": [128, NT, 64],
    "decT": [128, 8, 128], "qdec": [128, 4, 128], "gam128": [128, 4], "kdec": [128, 512],
    "bmask": [128, 128], "triu": [128, 128], "ecap": [128, NEXP], "kvals": [128, 17, 16],
}

PARAM_SHAPES = {
    "norm1_w": [D], "w_in": [D, 2560], "s5_a_re": [32, 64], "s5_a_im": [32, 64],
    "s5_b_re": [32, 64, 16], "s5_b_im": [32, 64, 16], "s5_c_re": [32, 16, 64], "s5_c_im": [32, 16, 64],
    "s5_d": [512], "s5_log_dt": [32], "s5_w_glu": [512, 512], "s5_b_glu": [512], "ret_norm_w": [512],
    "w_out": [D, D], "norm2_w": [D], "router_group_w": [D, 4], "router_group_b": [4],
    "router_expert_w": [D, 32], "router_expert_b": [32],
    "moe_w_gate": [NEXP, D, 512], "moe_w_up": [NEXP, D, 512], "moe_w_down": [NEXP, 512, D],
    "final_norm_w": [D],
}


def build_nc(dbg=None, ntile=NT, skip_p1=False, cut=None, p2tiles=None):
    nc = bass.Bass("TRN2", target_bir_lowering=False)
    I = {}
    I["x"] = nc.dram_tensor("x", [TOK, D], F32, kind="ExternalInput").ap()
    I["xpre"] = nc.dram_tensor("xpre", [TOK, D], F32, kind="ExternalInput").ap()
    for k, s in PARAM_SHAPES.items():
        I[k] = nc.dram_tensor(k, s, F32, kind="ExternalInput").ap()
    for k, s in CONST_SHAPES.items():
        I[k] = nc.dram_tensor(k, s, F32, kind="ExternalInput").ap()
    out = nc.dram_tensor("out", [TOK, D], F32, kind="ExternalOutput").ap()
    dbg_out = None
    if dbg == "s5":
        dbg_out = nc.dram_tensor("dbg", [128, 4, TOK], BF16, kind="ExternalOutput").ap()
    if dbg == "h":
        dbg_out = nc.dram_tensor("dbg", [TOK, D], F32, kind="ExternalOutput").ap()
    d_inj = nc.dram_tensor("d_inj", [128, 4 * 2 * 8 * 128], BF16, kind="Internal").ap()
    d_fir = nc.dram_tensor("d_fir", [128, 4 * 8 * 128], BF16, kind="Internal").ap()
    d_nout = nc.dram_tensor("d_nout", [128, 8 * 2 * 16 * 64], BF16, kind="Internal").ap()
    d_xbuf = nc.dram_tensor("d_xbuf", [NEXP * CAP, D], BF16, kind="Internal").ap()
    d_ybuf = nc.dram_tensor("d_ybuf", [NEXP * CAP, D], BF16, kind="Internal").ap()
    d_hbuf = nc.dram_tensor("d_hbuf", [TOK, D], F32, kind="Internal").ap()

    with ExitStack() as gst:
        P = Prog(nc, gst)

        uid = [0]
        regcache = {}

        def bc_reg(e):
            if P.phase not in regcache:
                regcache[P.phase] = e.to_reg(NEXP * CAP - 1)
            return regcache[P.phase]

        def mk(stack):
            uid[0] += 1
            u = uid[0]
            sb = lambda n, s, d=F32: stack.enter_context(nc.sbuf_tensor(f"t{u}_{n}", list(s), d))
            ps = lambda n, s, d=F32: stack.enter_context(nc.psum_tensor(f"q{u}_{n}", list(s), d))
            return sb, ps

        gsb, _ = mk(gst)
        identf = gsb("identf", [128, 128])
        identb = gsb("identb", [128, 128], BF16)
        coef = gsb("coef", [128, HS_STEPS, 3, 16])
        S0 = gsb("S0", [128, 2, 16])
        R = gsb("R", [128, 4, 64])
        Rb = gsb("Rb", [128, 4, 128], BF16)
        gam128 = gsb("gam128", [128, 4])
        mhalf = gsb("mhalf", [128, 1])
        bglu = gsb("bglu", [128, 4])
        ss_dummy = gsb("ss_dummy", [128, 1])
        gates = gsb("gates", [128, NT, 2])
        dests = gsb("dests", [128, NT, 2], I32)

        P.op("sp", lambda e: e.dma_start(out=identf[:], in_=I["identf"]), writes=["identf"], dma=True)
        P.op("sp", lambda e: e.dma_start(out=gam128[:], in_=I["gam128"]), writes=["gam128"], dma=True)
        P.op("sp", lambda e: e.dma_start(out=bglu[:], in_=I["s5_b_glu"].rearrange("(c p) -> p c", p=128),
                                         allow_slow_non_contiguous=True), writes=["bglu"], dma=True)
        P.op("dve", lambda e: e.tensor_copy(out=identb[:], in_=identf[:]), reads=["identf"], writes=["identb"])
        P.op("pool", lambda e: e.memset(mhalf[:], -0.5), writes=["mhalf"])
        P.op("pool", lambda e: e.memset(R[:], 0.0), writes=["R"])
        P.op("pool", lambda e: e.memset(Rb[:], 0.0), writes=["Rb"])

        with ExitStack() as st:
            sb, ps = mk(st)
            lr = sb("lr", [128, 16]); li = sb("li", [128, 16]); ldt = sb("ldt", [128, 16])
            dt_ = sb("dt_", [128, 16]); ph = sb("ph", [128, 16]); lrdt = sb("lrdt", [128, 16])
            NPW = 9 + 8
            pw = sb("pw", [128, NPW, 2, 16])
            t1 = sb("t1", [128, 16]); t2 = sb("t2", [128, 16]); t3 = sb("t3", [128, 16]); t4 = sb("t4", [128, 16])
            ti = sb("ti", [128, 16], I32)
            mag = sb("mag", [128, 16]); sc = sb("sc", [128, 2, 16])
            br = sb("br", [128, 16, 16]); bi = sb("bi", [128, 16, 16])
            cr = sb("cr", [128, 16, 16]); ci = sb("ci", [128, 16, 16])
            bbr = sb("bbr", [128, 16, 16]); bbi = sb("bbi", [128, 16, 16])
            u1 = sb("u1", [128, 16, 16]); u2 = sb("u2", [128, 16, 16])
            zr = sb("zr", [128, 16]); zi = sb("zi", [128, 16])
            XP = [[sb(f"XP{a}{b}", [128, 16, 2, 16]) for b in range(2)] for a in range(2)]
            YP = [sb(f"YP{b}", [128, 16, 2, 16]) for b in range(2)]
            kvals = sb("kvals", [128, NPW, 16]); A0 = sb("A0", [128, NPW, 16]); M0 = sb("M0", [128, NPW, 16])
            A1_ = sb("A1_", [128, NPW, 16]); A2_ = sb("A2_", [128, NPW, 16]); Ai = sb("Ai", [128, NPW, 16], I32)
            Yall = sb("Yall", [128, 9, 2, 16, 16]); Xall = sb("Xall", [128, 8, 2, 16, 16])
            w1 = sb("w1", [128, 9, 16, 16]); w2 = sb("w2", [128, 9, 16, 16])
            INJ = sb("INJ", [128, 4, 2, 8, 128], BF16)
            FIR = sb("FIR", [128, 4, 8, 128], BF16)
            NOUT = sb("NOUT", [128, 8, 2, 8, 2, 2, 2, 16], BF16)
            bmask = sb("bmask", [128, 128]); dcol = sb("dcol", [128, 4])
            tmpF = sb("tmpF", [128, 4, 128])
            pI = ps("pI", [128, 4, 128]); pF = ps("pF", [128, 4, 128]); pCT = ps("pCT", [128, 4, 128])
            Cins = {"cr": sb("Cinr", [128, 4, 128]), "ci": sb("Cini", [128, 4, 128])}

            nc_allow = nc.allow_non_contiguous_dma(reason="tiny parameter re-layouts")
            st.enter_context(nc_allow)
            P.op("sp", lambda e: e.dma_start(out=lr[:], in_=I["s5_a_re"].rearrange("(a b) p -> (b p) a", b=2)), writes=["lr"], dma=True)
            P.op("sp", lambda e: e.dma_start(out=li[:], in_=I["s5_a_im"].rearrange("(a b) p -> (b p) a", b=2)), writes=["li"], dma=True)
            for e2 in range(2):
                P.op("sp", lambda e, e2=e2: e.dma_start(out=ldt[64 * e2:64 * e2 + 64, :],
                                                        in_=I["s5_log_dt"].rearrange("(a b) -> b a", b=2)[e2, :].partition_broadcast(64)),
                     writes=["ldt"], dma=True)
            P.op("sp", lambda e: e.dma_start(out=br[:], in_=I["s5_b_re"].rearrange("(a b) p j -> (b p) a j", b=2)), writes=["br"], dma=True)
            P.op("sp", lambda e: e.dma_start(out=bi[:], in_=I["s5_b_im"].rearrange("(a b) p j -> (b p) a j", b=2)), writes=["bi"], dma=True)
            for nm, dst, tg in (("s5_c_re", cr, "cr"), ("s5_c_im", ci, "ci")):
                Cin = Cins[tg]
                P.op("sp", lambda e, nm=nm, Cin=Cin: e.dma_start(out=Cin[:, :, 0:64], in_=I[nm].rearrange("(c g) h p -> (g h) c p", c=4)), writes=[f"Cin{tg}"], dma=True)
                P.op("act", lambda e, Cin=Cin: e.activation(out=Cin[:, :, 64:128], in_=Cin[:, :, 0:64], func=AF.Copy), reads=[f"Cin{tg}"], writes=[f"Cin{tg}b"])
                for ch in range(4):
                    P.op("pe", lambda e, ch=ch, Cin=Cin: e.transpose(out=pCT[:, ch, :], in_=Cin[:, ch, :], identity=identf[:]), reads=[f"Cin{tg}", f"Cin{tg}b", "identf"], writes=["pCT"])
                for e2 in range(2):
                    sl = slice(64 * e2, 64 * e2 + 64)
                    P.op("dve", lambda e, sl=sl, e2=e2, dst=dst: e.tensor_copy(
                        out=dst[sl, :, :].rearrange("p (c q) h -> p c q h", c=4),
                        in_=pCT[sl, :, :].rearrange("p c (q e h) -> p c q e h", q=4, e=2)[:, :, :, e2, :]), reads=["pCT"], writes=[tg])
            P.op("sp", lambda e: e.dma_start(out=bmask[:], in_=I["bmask"]), writes=["bmask"], dma=True)
            P.op("sp", lambda e: e.dma_start(out=dcol[:], in_=I["s5_d"].rearrange("(c p) -> p c", p=128)), writes=["dcol"], dma=True)

            P.op("act", lambda e: e.activation(out=dt_[:], in_=ldt[:], func=AF.Exp), reads=["ldt"], writes=["dt_"])
            P.op("dve", lambda e: e.tensor_tensor(out=ph[:], in0=li[:], in1=dt_[:], op=ALU.mult), reads=["li", "dt_"], writes=["ph"])
            P.op("dve", lambda e: e.tensor_tensor(out=lrdt[:], in0=lr[:], in1=dt_[:], op=ALU.mult), reads=["lr", "dt_"], writes=["lrdt"])
            P.op("sp", lambda e: e.dma_start(out=kvals[:], in_=I["kvals"]), writes=["kvals"], dma=True)
            bk17 = lambda a: a.unsqueeze(1).broadcast_to([128, NPW, 16])
            P.op("dve", lambda e: e.tensor_tensor(out=A0[:], in0=kvals[:], in1=bk17(ph[:]), op=ALU.mult), reads=["kvals", "ph"], writes=["A0"])
            P.op("dve", lambda e: e.tensor_tensor(out=M0[:], in0=kvals[:], in1=bk17(lrdt[:]), op=ALU.mult), reads=["kvals", "lrdt"], writes=["M0"])
            P.op("act", lambda e: e.activation(out=M0[:], in_=M0[:], func=AF.Exp), reads=["M0"], writes=["M0"])
            for cidx, off in ((0, 0.25), (1, 0.0)):
                P.op("dve", lambda e, off=off: e.tensor_scalar(out=A1_[:], in0=A0[:], scalar1=1.0 / TWO_PI, scalar2=64.0 + off, op0=ALU.mult, op1=ALU.add), reads=["A0"], writes=["A1"])
                P.op("dve", lambda e: e.tensor_copy(out=Ai[:], in_=A1_[:]), reads=["A1"], writes=["Ai"])
                P.op("dve", lambda e: e.tensor_copy(out=A2_[:], in_=Ai[:]), reads=["Ai"], writes=["A2"])
                P.op("dve", lambda e: e.tensor_tensor(out=A1_[:], in0=A1_[:], in1=A2_[:], op=ALU.subtract), reads=["A1", "A2"], writes=["A1"])
                P.op("dve", lambda e: e.tensor_single_scalar(out=A2_[:], in_=A1_[:], scalar=0.5, op=ALU.is_gt), reads=["A1"], writes=["A2"])
                P.op("dve", lambda e: e.tensor_tensor(out=A1_[:], in0=A1_[:], in1=A2_[:], op=ALU.subtract), reads=["A1", "A2"], writes=["A1"])
                P.op("act", lambda e: e.activation(out=A2_[:], in_=A1_[:], func=AF.Sin, scale=TWO_PI), reads=["A1"], writes=["A2"])
                P.op("dve", lambda e, cidx=cidx: e.tensor_tensor(out=pw[:, :, cidx, :], in0=A2_[:], in1=M0[:], op=ALU.mult), reads=["A2", "M0"], writes=["pw"])
            P.op("dve", lambda e: e.tensor_copy(out=coef[:, :, 0:2, :], in_=pw[:, 8:8 + HS_STEPS, :, :]), reads=["pw"], writes=["coef"])
            P.op("dve", lambda e: e.tensor_scalar(out=coef[:, :, 2, :], in0=pw[:, 8:8 + HS_STEPS, 1, :], scalar1=-1.0, scalar2=None, op0=ALU.mult), reads=["pw"], writes=["coef"])
            P.op("dve", lambda e: e.tensor_tensor(out=t1[:], in0=lr[:], in1=lr[:], op=ALU.mult), reads=["lr"], writes=["t1"])
            P.op("dve", lambda e: e.tensor_tensor(out=t2[:], in0=li[:], in1=li[:], op=ALU.mult), reads=["li"], writes=["t2"])
            P.op("dve", lambda e: e.tensor_tensor(out=t1[:], in0=t1[:], in1=t2[:], op=ALU.add), reads=["t1", "t2"], writes=["t1"])
            P.op("dve", lambda e: e.reciprocal(out=t1[:], in_=t1[:]), reads=["t1"], writes=["t1"])
            P.op("dve", lambda e: e.tensor_scalar(out=t2[:], in0=pw[:, 1, 0, :], scalar1=-1.0, scalar2=None, op0=ALU.add), reads=["pw"], writes=["t2"])
            P.op("dve", lambda e: e.tensor_tensor(out=t3[:], in0=t2[:], in1=lr[:], op=ALU.mult), reads=["t2", "lr"], writes=["t3"])
            P.op("dve", lambda e: e.tensor_tensor(out=t4[:], in0=pw[:, 1, 1, :], in1=li[:], op=ALU.mult), reads=["pw", "li"], writes=["t4"])
            P.op("dve", lambda e: e.tensor_tensor(out=t3[:], in0=t3[:], in1=t4[:], op=ALU.add), reads=["t3", "t4"], writes=["t3"])
            P.op("dve", lambda e: e.tensor_tensor(out=zr[:], in0=t3[:], in1=t1[:], op=ALU.mult), reads=["t3", "t1"], writes=["zr"])
            P.op("dve", lambda e: e.tensor_tensor(out=t3[:], in0=pw[:, 1, 1, :], in1=lr[:], op=ALU.mult), reads=["pw", "lr"], writes=["t3"])
            P.op("dve", lambda e: e.tensor_tensor(out=t4[:], in0=t2[:], in1=li[:], op=ALU.mult), reads=["t2", "li"], writes=["t4"])
            P.op("dve", lambda e: e.tensor_tensor(out=t3[:], in0=t3[:], in1=t4[:], op=ALU.subtract), reads=["t3", "t4"], writes=["t3"])
            P.op("dve", lambda e: e.tensor_tensor(out=zi[:], in0=t3[:], in1=t1[:], op=ALU.mult), reads=["t3", "t1"], writes=["zi"])

            def bc(a):
                return a.unsqueeze(2).broadcast_to([128, 16, 16])

            P.op("dve", lambda e: e.tensor_tensor(out=u1[:], in0=br[:], in1=bc(zr[:]), op=ALU.mult), reads=["br", "zr"], writes=["u1"])
            P.op("dve", lambda e: e.tensor_tensor(out=u2[:], in0=bi[:], in1=bc(zi[:]), op=ALU.mult), reads=["bi", "zi"], writes=["u2"])
            P.op("dve", lambda e: e.tensor_tensor(out=bbr[:], in0=u1[:], in1=u2[:], op=ALU.subtract), reads=["u1", "u2"], writes=["bbr"])
            P.op("dve", lambda e: e.tensor_tensor(out=u1[:], in0=bi[:], in1=bc(zr[:]), op=ALU.mult), reads=["bi", "zr"], writes=["u1"])
            P.op("dve", lambda e: e.tensor_tensor(out=u2[:], in0=br[:], in1=bc(zi[:]), op=ALU.mult), reads=["br", "zi"], writes=["u2"])
            P.op("dve", lambda e: e.tensor_tensor(out=bbi[:], in0=u1[:], in1=u2[:], op=ALU.add), reads=["u1", "u2"], writes=["bbi"])

            def cmul_all(dst, nk, k0, b_r, b_i, btags, dtag):
                pr = pw[:, k0:k0 + nk, 0, :].unsqueeze(3).broadcast_to([128, nk, 16, 16])
                pi = pw[:, k0:k0 + nk, 1, :].unsqueeze(3).broadcast_to([128, nk, 16, 16])
                Br = b_r.unsqueeze(1).broadcast_to([128, nk, 16, 16])
                Bi = b_i.unsqueeze(1).broadcast_to([128, nk, 16, 16])
                W1 = w1[:, 0:nk, :, :]
                W2 = w2[:, 0:nk, :, :]
                P.op("dve", lambda e: e.tensor_tensor(out=W1, in0=Br, in1=pr, op=ALU.mult), reads=btags + ["pw"], writes=["w1"])
                P.op("dve", lambda e: e.tensor_tensor(out=W2, in0=Bi, in1=pi, op=ALU.mult), reads=btags + ["pw"], writes=["w2"])
                P.op("dve", lambda e: e.tensor_tensor(out=dst[:, :, 0, :, :], in0=W1, in1=W2, op=ALU.subtract), reads=["w1", "w2"], writes=[dtag])
                P.op("dve", lambda e: e.tensor_tensor(out=W1, in0=Bi, in1=pr, op=ALU.mult), reads=btags + ["pw"], writes=["w1"])
                P.op("dve", lambda e: e.tensor_tensor(out=W2, in0=Br, in1=pi, op=ALU.mult), reads=btags + ["pw"], writes=["w2"])
                P.op("dve", lambda e: e.tensor_tensor(out=dst[:, :, 1, :, :], in0=W1, in1=W2, op=ALU.add), reads=["w1", "w2"], writes=[dtag])

            for a in range(2):
                for b in range(2):
                    P.op("pool", lambda e, a=a, b=b: e.memset(XP[a][b][:], 0.0), writes=[f"XP{a}{b}"])
            for b in range(2):
                P.op("pool", lambda e, b=b: e.memset(YP[b][:], 0.0), writes=[f"YP{b}"])
            P.op("pool", lambda e: e.memset(NOUT[:], 0.0), writes=["NOUT"])

            cmul_all(Yall, 9, 0, cr[:], ci[:], ["cr", "ci"], "Yall")
            P.op("dve", lambda e: e.tensor_scalar(out=Yall[:, :, 1, :, :], in0=Yall[:, :, 1, :, :], scalar1=-1.0, scalar2=None, op0=ALU.mult), reads=["Yall"], writes=["Yall"])
            for e2 in range(2):
                sl = slice(64 * e2, 64 * e2 + 64)
                for ri in range(2):
                    P.op("dve", lambda e, ri=ri, sl=sl, e2=e2: e.tensor_copy(out=YP[ri][sl, :, e2, :], in_=Yall[sl, 0, ri, :, :]), reads=["Yall"], writes=[f"YP{ri}"])
                for lo in range(2):
                    P.op("dve", lambda e, sl=sl, e2=e2, lo=lo: e.tensor_copy(
                        out=NOUT[sl, :, :, :, lo, lo, e2, :].rearrange("p r i hi h -> p (r i) hi h"),
                        in_=Yall[sl, 1:9, :, :, :].rearrange("p k r (hi lo) h -> p (k r) hi lo h", lo=2)[:, :, :, lo, :]),
                        reads=["Yall"], writes=["NOUT"])
            cmul_all(Xall, 8, 0, bbr[:], bbi[:], ["bbr", "bbi"], "Xall")
            for k in range(8):
                a = k % 2
                for ri in range(2):
                    for e2 in range(2):
                        sl = slice(64 * e2, 64 * e2 + 64)
                        eng = "pool" if e2 == 0 else "act"
                        if eng == "pool":
                            P.op("pool", lambda e, a=a, ri=ri, sl=sl, e2=e2, k=k: e.tensor_copy(out=XP[a][ri][sl, :, e2, :], in_=Xall[sl, k, ri, :, :]),
                                 reads=["Xall"], writes=[f"XP{a}{ri}"])
                        else:
                            P.op("act", lambda e, a=a, ri=ri, sl=sl, e2=e2, k=k: e.activation(out=XP[a][ri][sl, :, e2, :], in_=Xall[sl, k, ri, :, :], func=AF.Copy),
                                 reads=["Xall"], writes=[f"XP{a}{ri}"])
                for ri in range(2):
                    for ch in range(4):
                        P.op("pe", lambda e, a=a, ri=ri, ch=ch: e.matmul(
                            pI[:, ch, :], lhsT=XP[a][ri][:, 4 * ch:4 * ch + 4, :, :].rearrange("p a b c -> p (a b c)"),
                            rhs=identf[:], start=True, stop=True), reads=[f"XP{a}{ri}", "identf"], writes=["pI"])
                    P.op("act", lambda e, ri=ri, k=k: e.activation(out=INJ[:, :, ri, 7 - k, :], in_=pI[:], func=AF.Copy), reads=["pI"], writes=["INJ"])
                for ch in range(4):
                    for ri in range(2):
                        P.op("pe", lambda e, a=a, ri=ri, ch=ch: e.matmul(
                            pF[:, ch, :], lhsT=XP[a][ri][:, 4 * ch:4 * ch + 4, :, :].rearrange("p a b c -> p (a b c)"),
                            rhs=YP[ri][:, 4 * ch:4 * ch + 4, :, :].rearrange("p a b c -> p (a b c)"),
                            start=(ri == 0), stop=(ri == 1)), reads=[f"XP{a}{ri}", f"YP{ri}"], writes=["pF"])
                if k == 0:
                    P.op("dve", lambda e: e.tensor_tensor(out=tmpF[:], in0=pF[:], in1=bmask[:].unsqueeze(1).broadcast_to([128, 4, 128]), op=ALU.mult),
                         reads=["pF", "bmask"], writes=["tmpF"])
                    for ch in range(4):
                        P.op("dve", lambda e, ch=ch: e.scalar_tensor_tensor(out=FIR[:, ch, 0, :], in0=identf[:], scalar=dcol[:, ch:ch + 1], in1=tmpF[:, ch, :],
                                                                            op0=ALU.mult, op1=ALU.add), reads=["tmpF", "identf", "dcol"], writes=["FIR"])
                else:
                    P.op("dve", lambda e, k=k: e.tensor_tensor(out=FIR[:, :, k, :], in0=pF[:], in1=bmask[:].unsqueeze(1).broadcast_to([128, 4, 128]), op=ALU.mult),
                         reads=["pF", "bmask"], writes=["FIR"])
            P.op("sp", lambda e: e.dma_start(out=d_inj, in_=INJ[:].rearrange("p a b c d -> p (a b c d)")), reads=["INJ"], writes=["d_inj"], dma=True)
            P.op("sp", lambda e: e.dma_start(out=d_fir, in_=FIR[:].rearrange("p a b c -> p (a b c)")), reads=["FIR"], writes=["d_fir"], dma=True)
            P.op("sp", lambda e: e.dma_start(out=d_nout, in_=NOUT[:].rearrange("p a b c d e f g -> p (a b c d e f g)")), reads=["NOUT"], writes=["d_nout"], dma=True)
            P.emit()


        with ExitStack() as st_mix:
            sbm, _ = mk(st_mix)
            mixT = sbm("mixT", [128, 4, TOK], BF16)
            with ExitStack() as st_s5:
                sbs, _ = mk(st_s5)
                uT = sbs("uT", [128, 4, TOK], BF16)
                Sb = sbs("Sb", [128, 2, 16, NSUB], BF16)
                with ExitStack() as st:
                    sb, ps = mk(st)
                    Wb1 = sb("Wb1", [128, 8, 1536], BF16)
                    wst = [sb(f"wst{i}", [128, 768]) for i in range(2)]
                    n1col = sb("n1col", [128, 8])
                    INJs = sb("INJs", [128, 4, 2, 8, 128], BF16)
                    L = sb("L", [128, 2, 4, NSUB])
                    tmpA = [sb(f"tmpA{i}", [128, 2, NSUB]) for i in range(4)]
                    xt = [sb(f"xt{i}", [128, D]) for i in range(2)]
                    xbb = [sb(f"xb{i}", [128, D], BF16) for i in range(2)]
                    xT = [sb(f"xT{i}", [128, 8, 128], BF16) for i in range(2)]
                    ssb = sb("ssb", [128, 2]); msb = sb("msb", [128, 2]); rsb = sb("rsb", [128, 2])
                    rtm = [sb(f"rtm{i}", [128, 8, 32]) for i in range(4)]
                    rotk = sb("rotk", [128, 2, 64]); kdec = sb("kdec", [128, 512])
                    kr = sb("kr", [128, 8, 64])
                    ktil = sb("ktil", [128, 512], BF16); vb = sb("vb", [128, 512], BF16)
                    pT = ps("pT", [128, 8, 128], BF16)
                    pU = ps("pU", [128, 4, 128])
                    pbig = ps("pbig", [128, 4, 512])

                    P.op("sp", lambda e: e.dma_start(out=n1col[:], in_=I["norm1_w"].rearrange("(c p) -> p c", p=128), allow_slow_non_contiguous=True),
                         writes=["n1col"], dma=True)
                    P.op("sp", lambda e: e.dma_start(out=INJs[:].rearrange("p a b c d -> p (a b c d)"), in_=d_inj), writes=["INJs"], dma=True)
                    P.op("sp", lambda e: e.dma_start(out=kdec[:], in_=I["kdec"]), writes=["kdec"], dma=True)
                    for c in range(8):
                        for b in range(2):
                            if b == 0:
                                P.op("sp", lambda e, c=c, b=b: e.dma_start(out=wst[b][:, 0:512], in_=I["w_in"][c * 128:(c + 1) * 128, 0:512]), writes=[f"wst{b}"], dma=True)
                                P.op("sp", lambda e, c=c, b=b: e.dma_start(out=wst[b][:, 512:768], in_=I["w_in"][c * 128:(c + 1) * 128, 1024:1280]), writes=[f"wst{b}"], dma=True)
                                P.op("dve", lambda e, c=c, b=b: e.tensor_scalar(out=Wb1[:, c, 0:768], in0=wst[b][:], scalar1=n1col[:, c:c + 1], scalar2=None, op0=ALU.mult),
                                     reads=[f"wst{b}", "n1col"], writes=["Wb1"])
                            else:
                                P.op("sp", lambda e, c=c, b=b: e.dma_start(out=wst[b][:], in_=I["w_in"][c * 128:(c + 1) * 128, 1280:2048]), writes=[f"wst{b}"], dma=True)
                                P.op("act", lambda e, c=c, b=b: e.activation(out=Wb1[:, c, 768:1536], in_=wst[b][:], func=AF.Copy, scale=n1col[:, c:c + 1]),
                                     reads=[f"wst{b}", "n1col"], writes=["Wb1"])

                    def A1(ti, prefix, sl):
                        src = I["xpre"] if prefix else I["x"]
                        b = sl % 2
                        P.op("sp", lambda e: e.dma_start(out=xt[b][:], in_=src[ti * 128:(ti + 1) * 128, :]), writes=[f"xt{b}"], dma=True)
                        if prefix:
                            P.op("sp", lambda e: e.dma_start(out=rotk[:, b, :], in_=I["rot_kpre"][:, ti, :]), writes=[f"rotk{b}"], dma=True)
                        P.op("act", lambda e: e.activation(out=xbb[b][:], in_=xt[b][:], func=AF.Square, accum_out=ssb[:, b:b + 1]), reads=[f"xt{b}"], writes=[f"xb{b}", f"ss{b}"])
                        P.op("dve", lambda e: e.tensor_scalar(out=msb[:, b:b + 1], in0=ssb[:, b:b + 1], scalar1=1.0 / D, scalar2=EPS, op0=ALU.mult, op1=ALU.add), reads=[f"ss{b}"], writes=[f"ms{b}"])
                        P.op("pool", lambda e: e.tensor_tensor(out=rsb[:, b:b + 1], in0=msb[:, b:b + 1], in1=mhalf[:], op=ALU.pow), reads=[f"ms{b}", "mhalf"], writes=[f"rs{b}"])
                        P.op("act", lambda e: e.activation(out=xbb[b][:], in_=xt[b][:], func=AF.Copy, scale=rsb[:, b:b + 1]), reads=[f"xt{b}", f"rs{b}"], writes=[f"xb{b}"])

                    def A2(ti, prefix, sl):
                        b = sl % 2
                        for c in range(8):
                            P.op("pe", lambda e, c=c: e.transpose(out=pT[:, c, :], in_=xbb[b][:, c * 128:(c + 1) * 128], identity=identb[:]),
                                 reads=[f"xb{b}", "identb"], writes=["pT"])
                        P.op("dve", lambda e: e.tensor_copy(out=xT[b][:], in_=pT[:]), reads=["pT"], writes=[f"xT{b}"])

                    def B1(ti, prefix, sl):
                        b = sl % 2
                        for m in range(4):
                            for c in range(8):
                                P.op("pe", lambda e, m=m, c=c: e.matmul(pU[:, m, :], lhsT=Wb1[:, c, m * 128:(m + 1) * 128], rhs=xT[b][:, c, :],
                                                                        start=(c == 0), stop=(c == 7)), reads=["Wb1", f"xT{b}"], writes=["pU"])
                        if not prefix:
                            return
                        for c in range(8):
                            P.op("pe", lambda e, c=c: e.matmul(pbig[:, 0, :], lhsT=xT[b][:, c, :], rhs=Wb1[:, c, 512:1024], start=(c == 0), stop=(c == 7)),
                                 reads=["Wb1", f"xT{b}"], writes=["pb0"])
                        for c in range(8):
                            P.op("pe", lambda e, c=c: e.matmul(pbig[:, 1, :], lhsT=xT[b][:, c, :], rhs=Wb1[:, c, 1024:1536], start=(c == 0), stop=(c == 7)),
                                 reads=["Wb1", f"xT{b}"], writes=["pb1"])

                    def B2(ti, prefix, sl):
                        b = sl % 2
                        P.op("act", lambda e: e.activation(out=uT[:, :, ti * 128:(ti + 1) * 128], in_=pU[:], func=AF.Copy), reads=["pU"], writes=[f"uT{ti}"])
                        if not prefix:
                            return
                        pk = pbig[:, 0, :].rearrange("p (h d) -> p h d", h=8)
                        cosb = rotk[:, b, 0:32].unsqueeze(1).broadcast_to([128, 8, 32])
                        sinb = rotk[:, b, 32:64].unsqueeze(1).broadcast_to([128, 8, 32])
                        rtag = ["pb0", f"rotk{b}"]
                        P.op("dve", lambda e: e.tensor_tensor(out=rtm[0][:], in0=pk[:, :, 0:32], in1=cosb, op=ALU.mult), reads=rtag, writes=["rtm0"])
                        P.op("dve", lambda e: e.tensor_tensor(out=rtm[1][:], in0=pk[:, :, 32:64], in1=sinb, op=ALU.mult), reads=rtag, writes=["rtm1"])
                        P.op("dve", lambda e: e.tensor_tensor(out=rtm[2][:], in0=pk[:, :, 0:32], in1=sinb, op=ALU.mult), reads=rtag, writes=["rtm2"])
                        P.op("dve", lambda e: e.tensor_tensor(out=rtm[3][:], in0=pk[:, :, 32:64], in1=cosb, op=ALU.mult), reads=rtag, writes=["rtm3"])
                        P.op("pool", lambda e: e.tensor_tensor(out=kr[:, :, 0:32], in0=rtm[0][:], in1=rtm[1][:], op=ALU.subtract), reads=["rtm0", "rtm1"], writes=["kr1"])
                        P.op("pool", lambda e: e.tensor_tensor(out=kr[:, :, 32:64], in0=rtm[2][:], in1=rtm[3][:], op=ALU.add), reads=["rtm2", "rtm3"], writes=["kr2"])
                        P.op("dve", lambda e: e.tensor_tensor(out=ktil[:], in0=kr[:].rearrange("p h d -> p (h d)"), in1=kdec[:], op=ALU.mult),
                             reads=["kr1", "kr2", "kdec"], writes=["ktil"])
                        P.op("act", lambda e: e.activation(out=vb[:], in_=pbig[:, 1, :], func=AF.Copy), reads=["pb1"], writes=["vb"])
                        pkv = pbig[:, 2, 0:256].rearrange("p (j e) -> p j e", j=4)
                        for h in range(8):
                            j, hl = h // 2, h % 2
                            P.op("pe", lambda e, h=h, j=j, hl=hl: e.matmul(pkv[64 * hl:64 * hl + 64, j, :], lhsT=ktil[:, h * 64:(h + 1) * 64],
                                                                           rhs=vb[:, h * 64:(h + 1) * 64], start=True, stop=True),
                                 reads=["ktil", "vb"], writes=["pb2"])
                        for j in range(4):
                            P.op("dve", lambda e, j=j: e.scalar_tensor_tensor(out=R[:, j, :], in0=R[:, j, :], scalar=gam128[:, j:j + 1], in1=pkv[:, j, :],
                                                                              op0=ALU.mult, op1=ALU.add), reads=["pb2", "gam128", "R"], writes=["R"])

                    slot = [0]

                    def run_tiles(prefix):
                        seq = [(ti, prefix, slot[0] + ti) for ti in range(ntile)]
                        slot[0] += ntile
                        A1(*seq[0])
                        A2(*seq[0])
                        for i in range(len(seq)):
                            if i + 1 < len(seq):
                                A1(*seq[i + 1])
                            B1(*seq[i])
                            if i + 1 < len(seq):
                                A2(*seq[i + 1])
                            B2(*seq[i])

                    def inject(hf, main):
                        for gi in range(4):
                            g16 = 4 * hf + gi
                            ch, q = g16 // 4, g16 % 4
                            for ri in range(2):
                                bk = 2 * (gi % 2) + ri
                                n = NSUB - 1 if main else NSUB
                                o0 = 1 if main else 0
                                for r in range(8):
                                    P.op("pe", lambda e, ch=ch, q=q, ri=ri, r=r, bk=bk, n=n, o0=o0: e.matmul(
                                        pbig[:, bk, o0:o0 + n], lhsT=INJs[32 * q:32 * q + 32, ch, ri, r, :],
                                        rhs=uT[32 * q:32 * q + 32, ch, :].rearrange("p (c r) -> p c r", r=8)[:, 0:n, r],
                                        start=(r == 0), stop=(r == 7), tile_position=(32 * q, 0)),
                                        reads=["INJs"] + [f"uT{t}" for t in range(NT)], writes=[f"pb{bk}"])
                                P.op("act", lambda e, ri=ri, gi=gi, bk=bk, o0=o0, n=n: e.activation(out=L[:, ri, gi, o0:o0 + n], in_=pbig[:, bk, o0:o0 + n], func=AF.Copy),
                                     reads=[f"pb{bk}"], writes=[f"L{gi}"])
                            if main:
                                P.op("pool", lambda e, gi=gi, g16=g16: e.tensor_copy(out=L[:, :, gi, 0:1], in_=S0[:, :, g16:g16 + 1]), reads=["S0"], writes=[f"L{gi}"])

                    def scan(hf, main):
                        N = NSUB
                        for s in range(HS_STEPS):
                            d = 2 ** s
                            G = []
                            for gi in range(4):
                                g16 = 4 * hf + gi
                                bufs = [(L[:, 0, gi, :], L[:, 1, gi, :], L[:, :, gi, :], f"L{gi}"), (tmpA[gi][:, 0, :], tmpA[gi][:, 1, :], tmpA[gi][:, :, :], f"tmpA{gi}")]
                                G.append(bufs[s % 2] + bufs[(s + 1) % 2] + (coef[:, s, 0, g16:g16 + 1], coef[:, s, 1, g16:g16 + 1], coef[:, s, 2, g16:g16 + 1]))
                            for (sr, si, sall, stag, dr, di, dall, dtag, car, cai, cnai) in G:
                                P.op("pool", lambda e, dall=dall, sall=sall, d=d: e.tensor_copy(out=dall[:, :, 0:d], in_=sall[:, :, 0:d]), reads=[stag], writes=[dtag])
                            for (sr, si, sall, stag, dr, di, dall, dtag, car, cai, cnai) in G:
                                P.op("dve", lambda e, dr=dr, sr=sr, car=car, d=d: e.scalar_tensor_tensor(out=dr[:, d:N], in0=sr[:, 0:N - d], scalar=car, in1=sr[:, d:N], op0=ALU.mult, op1=ALU.add),
                                     reads=[stag, "coef"], writes=[dtag])
                            for (sr, si, sall, stag, dr, di, dall, dtag, car, cai, cnai) in G:
                                P.op("dve", lambda e, di=di, si=si, car=car, d=d: e.scalar_tensor_tensor(out=di[:, d:N], in0=si[:, 0:N - d], scalar=car, in1=si[:, d:N], op0=ALU.mult, op1=ALU.add),
                                     reads=[stag, "coef"], writes=[dtag])
                            for (sr, si, sall, stag, dr, di, dall, dtag, car, cai, cnai) in G:
                                P.op("dve", lambda e, dr=dr, si=si, cnai=cnai, d=d: e.scalar_tensor_tensor(out=dr[:, d:N], in0=si[:, 0:N - d], scalar=cnai, in1=dr[:, d:N], op0=ALU.mult, op1=ALU.add),
                                     reads=[stag, "coef"], writes=[dtag])
                            for (sr, si, sall, stag, dr, di, dall, dtag, car, cai, cnai) in G:
                                P.op("dve", lambda e, di=di, sr=sr, cai=cai, d=d: e.scalar_tensor_tensor(out=di[:, d:N], in0=sr[:, 0:N - d], scalar=cai, in1=di[:, d:N], op0=ALU.mult, op1=ALU.add),
                                     reads=[stag, "coef"], writes=[dtag])
                        for gi in range(4):
                            g16 = 4 * hf + gi
                            if main:
                                P.op("act", lambda e, g16=g16, gi=gi: e.activation(out=Sb[:, :, g16, :], in_=tmpA[gi][:], func=AF.Copy), reads=[f"tmpA{gi}"], writes=["Sb"])
                            else:
                                P.op("act", lambda e, g16=g16, gi=gi: e.activation(out=S0[:, :, g16:g16 + 1], in_=tmpA[gi][:, :, N - 1:N], func=AF.Copy), reads=[f"tmpA{gi}"], writes=["S0"])

                    def tree(hf):
                        n = NSUB
                        for s in range(HS_STEPS):
                            n //= 2
                            G = []
                            for gi in range(4):
                                g16 = 4 * hf + gi
                                bufs = [(L[:, :, gi, :], f"L{gi}"), (tmpA[gi][:, :, :], f"tmpA{gi}")]
                                src, stag = bufs[s % 2]
                                dst, dtag = bufs[(s + 1) % 2]
                                ev = lambda ri, src=src, n=n: src[:, ri, 0:2 * n].rearrange("p (m t) -> p m t", t=2)[:, :, 0]
                                od = lambda ri, src=src, n=n: src[:, ri, 0:2 * n].rearrange("p (m t) -> p m t", t=2)[:, :, 1]
                                G.append((ev, od, stag, dst, dtag, coef[:, s, 0, g16:g16 + 1], coef[:, s, 1, g16:g16 + 1], coef[:, s, 2, g16:g16 + 1]))
                            for (ev, od, stag, dst, dtag, car, cai, cnai) in G:
                                P.op("dve", lambda e, dst=dst, ev=ev, od=od, car=car, n=n: e.scalar_tensor_tensor(out=dst[:, 0, 0:n], in0=ev(0), scalar=car, in1=od(0), op0=ALU.mult, op1=ALU.add),
                                     reads=[stag, "coef"], writes=[dtag])
                            for (ev, od, stag, dst, dtag, car, cai, cnai) in G:
                                P.op("dve", lambda e, dst=dst, ev=ev, od=od, car=car, n=n: e.scalar_tensor_tensor(out=dst[:, 1, 0:n], in0=ev(1), scalar=car, in1=od(1), op0=ALU.mult, op1=ALU.add),
                                     reads=[stag, "coef"], writes=[dtag])
                            for (ev, od, stag, dst, dtag, car, cai, cnai) in G:
                                P.op("dve", lambda e, dst=dst, ev=ev, cnai=cnai, n=n: e.scalar_tensor_tensor(out=dst[:, 0, 0:n], in0=ev(1), scalar=cnai, in1=dst[:, 0, 0:n], op0=ALU.mult, op1=ALU.add),
                                     reads=[stag, "coef"], writes=[dtag])
                            for (ev, od, stag, dst, dtag, car, cai, cnai) in G:
                                P.op("dve", lambda e, dst=dst, ev=ev, cai=cai, n=n: e.scalar_tensor_tensor(out=dst[:, 1, 0:n], in0=ev(0), scalar=cai, in1=dst[:, 1, 0:n], op0=ALU.mult, op1=ALU.add),
                                     reads=[stag, "coef"], writes=[dtag])
                        for gi in range(4):
                            g16 = 4 * hf + gi
                            P.op("act", lambda e, g16=g16, gi=gi: e.activation(out=S0[:, :, g16:g16 + 1], in_=tmpA[gi][:, :, 0:1], func=AF.Copy), reads=[f"tmpA{gi}"], writes=["S0"])

                    run_tiles(True)
                    for hf in range(4):
                        inject(hf, False)
                        tree(hf)
                    run_tiles(False)
                    for hf in range(4):
                        inject(hf, True)
                        scan(hf, True)
                    if skip_p1:
                        P.reset_phase()
                    P.emit()

                with ExitStack() as st:
                    sb, ps = mk(st)
                    FIRs = sb("FIRs", [128, 4, 8, 128], BF16)
                    NOUTs = sb("NOUTs", [128, 8, 2, 16, 64], BF16)
                    Wg = sb("Wg", [128, 4, 512], BF16)
                    yf = sb("yf", [128, 4, 512]); sq = sb("sq", [128, 4, 512]); ygb = sb("ygb", [128, 4, 512], BF16)
                    pY = ps("pY", [128, 4, 512]); pZ = ps("pZ", [128, 4, 512])
                    P.op("sp", lambda e: e.dma_start(out=FIRs[:].rearrange("p a b c -> p (a b c)"), in_=d_fir), writes=["FIRs"], dma=True)
                    P.op("sp", lambda e: e.dma_start(out=NOUTs[:].rearrange("p a b c d -> p (a b c d)"), in_=d_nout), writes=["NOUTs"], dma=True)
                    P.op("pool", lambda e: e.dma_start(out=Wg[:], in_=I["s5_w_glu"].rearrange("(c p) n -> p c n", p=128)), writes=["Wg"], dma=True)
                    GC = 2.0 * math.sqrt(2.0 / math.pi)
                    for bk in range(ntile // 4):
                        cols = slice(512 * bk, 512 * bk + 512)
                        for ch in range(4):
                            py3 = pY[:, ch, :].rearrange("p (c r) -> p c r", r=8)
                            u3 = uT[:, ch, cols].rearrange("p (c r) -> p c r", r=8)
                            for tau in range(8):
                                P.op("pe", lambda e, ch=ch, tau=tau, py3=py3, u3=u3: e.matmul(
                                    py3[:, :, tau:8], lhsT=FIRs[:, ch, tau, :], rhs=u3[:, :, 0:8 - tau], start=(tau == 0), stop=False, skip_group_check=True),
                                    reads=["FIRs", "uT"], writes=[f"pY{ch}"])
                            for q in range(4):
                                g16 = 4 * ch + q
                                hc = q // 2
                                py3h = pY[64 * hc:64 * hc + 64, ch, :].rearrange("p (c r) -> p c r", r=8)
                                for r in range(8):
                                    for ri in range(2):
                                        P.op("pe", lambda e, g16=g16, r=r, ri=ri, py3h=py3h, bk=bk: e.matmul(
                                            py3h[:, :, r], lhsT=NOUTs[:, r, ri, g16, :], rhs=Sb[:, ri, g16, 64 * bk:64 * bk + 64],
                                            start=False, stop=True, skip_group_check=True), reads=["NOUTs", "Sb"], writes=[f"pY{ch}"])
                        ptags = [f"pY{ch}" for ch in range(4)]
                        P.op("act", lambda e: e.activation(out=sq[:], in_=pY[:], func=AF.Square, scale=math.sqrt(0.044715)), reads=ptags, writes=["sq"])
                        P.op("dve", lambda e: e.scalar_tensor_tensor(out=sq[:], in0=sq[:], scalar=1.0, in1=pY[:], op0=ALU.add, op1=ALU.mult), reads=ptags + ["sq"], writes=["sq"])
                        P.op("act", lambda e: e.activation(out=sq[:], in_=sq[:], func=AF.Sigmoid, scale=GC), reads=["sq"], writes=["sq"])
                        P.op("dve", lambda e: e.tensor_tensor(out=yf[:], in0=pY[:], in1=sq[:], op=ALU.mult), reads=ptags + ["sq"], writes=["yf"])
                        P.op("act", lambda e: e.activation(out=ygb[:], in_=yf[:], func=AF.Copy), reads=["yf"], writes=["ygb"])
                        for m in range(4):
                            for c in range(4):
                                P.op("pe", lambda e, m=m, c=c: e.matmul(pZ[:, m, :], lhsT=Wg[:, c, m * 128:(m + 1) * 128], rhs=ygb[:, c, :], start=(c == 0), stop=(c == 3)),
                                     reads=["Wg", "ygb"], writes=[f"pZ{m}"])
                            P.op("act", lambda e, m=m: e.activation(out=sq[:, m, :], in_=pZ[:, m, :], func=AF.Sigmoid, bias=bglu[:, m:m + 1]),
                                 reads=[f"pZ{m}", "bglu", "yf"], writes=[f"sg{m}"])
                            P.op("dve", lambda e, m=m, cols=cols: e.tensor_tensor(out=mixT[:, m, cols], in0=yf[:, m, :], in1=sq[:, m, :], op=ALU.mult),
                                 reads=["yf", f"sg{m}"], writes=["mixT"])
                        P.op("pool", lambda e: e.memset(ss_dummy[:], 0.0), writes=["sq"] + [f"sg{m}" for m in range(4)])
                    if dbg == "s5":
                        P.op("sp", lambda e: e.dma_start(out=dbg_out, in_=mixT[:]), reads=["mixT"], writes=["dbg"], dma=True)
                    if skip_p1:
                        P.reset_phase()
                    P.emit()
            if dbg == "s5":
                return nc
            BIG = 1.0e4
            with ExitStack() as st:
                sb, ps = mk(st)
                Wb2 = sb("Wb2", [128, 8, 2048], BF16)
                wst = [sb(f"wst{i}", [128, 1024]) for i in range(2)]
                n1col = sb("n1col", [128, 8])
                Wo = sb("Wo", [128, 8, D], BF16)
                rt = sb("rt", [128, 2, 2, 64])
                decT = sb("decT", [128, 8, 128]); qdec = sb("qdec", [128, 4, 128]); kdec = sb("kdec", [128, 512])
                n2w = sb("n2w", [128, D]); rnw = sb("rnw", [128, 512])
                Wr = sb("Wr", [128, 8, 36]); bfull = sb("bfull", [128, 36])
                triub = sb("triub", [128, 128], BF16); onesb = sb("onesb", [128, 128], BF16)
                tmpc = sb("tmpc", [128, 128])
                base_b = sb("base_b", [128, NEXP]); ecapl = sb("ecapl", [128, NEXP])
                xt = [sb(f"xt{i}", [128, D]) for i in range(4)]
                xbb = [sb(f"xb{i}", [128, D], BF16) for i in range(2)]
                xT = sb("xT", [128, 8, 128], BF16)
                ssA = sb("ssA", [128, 2]); msA = sb("msA", [128, 2]); rsA = sb("rsA", [128, 2])
                ssC = sb("ssC", [128, 1]); msC = sb("msC", [128, 1]); rsC = sb("rsC", [128, 1])
                kr = sb("kr", [128, 8, 64]); ta = sb("ta", [128, 8, 32]); tb = sb("tb", [128, 8, 32])
                rtmp = [[sb(f"rtmp{a}{i}", [128, 8, 32]) for i in range(4)] for a in range(2)]
                qrb = sb("qrb", [128, 8, 64], BF16); krb = sb("krb", [128, 512], BF16)
                ktil = [sb(f"ktil{i}", [128, 512], BF16) for i in range(2)]
                vb = [sb(f"vb{i}", [128, 512], BF16) for i in range(2)]
                sg = [sb(f"sg{i}", [128, 512]) for i in range(2)]
                qT = [sb(f"qT{i}", [128, 4, 128], BF16) for i in range(2)]
                qTd = [sb(f"qTd{i}", [128, 4, 128], BF16) for i in range(2)]
                kT = [sb(f"kT{i}", [128, 4, 128], BF16) for i in range(2)]
                PT = sb("PT", [128, 8, 128], BF16)
                osq = sb("osq", [128, 512]); oc = sb("oc", [128, 8, 64])
                s1 = sb("s1", [128, 8]); s2 = sb("s2", [128, 8]); mean = sb("mean", [128, 8]); var = sb("var", [128, 8]); rstd = sb("rstd", [128, 8])
                mh8 = sb("mh8", [128, 8])
                yret = sb("yret", [128, 512], BF16)
                yretT = [sb(f"yretT{i}", [128, 4, 128], BF16) for i in range(2)]
                hh = sb("hh", [128, D]); hn = sb("hn", [128, D])
                hnb = [sb(f"hnb{i}", [128, D], BF16) for i in range(2)]
                hnT = sb("hnT", [128, 8, 128])
                lg = sb("lg", [128, 36]); em = sb("em", [128, 32]); em2 = sb("em2", [128, 32])
                goh = sb("goh", [128, 4]); pen = sb("pen", [128, 4])
                maskb = sb("maskb", [128, 32], BF16); posf = sb("posf", [128, 32]); okm = sb("okm", [128, 32]); jk = sb("jk", [128, 32])
                sm = sb("sm", [128, 16]); ex5 = sb("ex5", [128, 5]); ex5t = sb("ex5t", [128, 5])
                oh12 = sb("oh12", [128, 2, 32]); t2 = sb("t2", [128, 2, 32]); d2 = sb("d2", [128, 2]); ok2 = sb("ok2", [128, 2]); pad2 = sb("pad2", [128, 2])
                dstf = sb("dstf", [128, 2])
                oh1 = oh12[:, 0, :]; oh2 = oh12[:, 1, :]
                pT = ps("pT", [128, 8, 128], BF16)
                pP = ps("pP", [128, 2, 512])
                pS = ps("pS", [128, 2, 512])
                pO = ps("pO", [128, 512])
                pK = ps("pK", [128, 512])
                pH = ps("pH", [128, 512])

                P.op("sp", lambda e: e.dma_start(out=n1col[:], in_=I["norm1_w"].rearrange("(c p) -> p c", p=128), allow_slow_non_contiguous=True),
                     writes=["n1col"], dma=True)
                for c in range(8):
                    for b in range(2):
                        P.op("sp", lambda e, c=c, b=b: e.dma_start(out=wst[b][:], in_=I["w_in"][c * 128:(c + 1) * 128, 512 + 1024 * b:512 + 1024 * (b + 1)]),
                             writes=[f"wst{b}"], dma=True)
                        if b == 0:
                            P.op("dve", lambda e, c=c, b=b: e.tensor_scalar(out=Wb2[:, c, 0:1024], in0=wst[b][:], scalar1=n1col[:, c:c + 1], scalar2=None, op0=ALU.mult),
                                 reads=[f"wst{b}", "n1col"], writes=["Wb2"])
                        else:
                            P.op("act", lambda e, c=c, b=b: e.activation(out=Wb2[:, c, 1024:2048], in_=wst[b][:], func=AF.Copy, scale=n1col[:, c:c + 1]),
                                 reads=[f"wst{b}", "n1col"], writes=["Wb2"])
                P.op("pool", lambda e: e.dma_start(out=Wo[:], in_=I["w_out"].rearrange("(c p) n -> p c n", p=128)), writes=["Wo"], dma=True)
                P.op("sp", lambda e: e.dma_start(out=decT[:], in_=I["decT"]), writes=["decT"], dma=True)
                P.op("sp", lambda e: e.dma_start(out=qdec[:], in_=I["qdec"]), writes=["qdec"], dma=True)
                P.op("sp", lambda e: e.dma_start(out=kdec[:], in_=I["kdec"]), writes=["kdec"], dma=True)
                P.op("sp", lambda e: e.dma_start(out=n2w[:], in_=I["norm2_w"].partition_broadcast(128)), writes=["n2w"], dma=True)
                P.op("sp", lambda e: e.dma_start(out=rnw[:], in_=I["ret_norm_w"].partition_broadcast(128)), writes=["rnw"], dma=True)
                P.op("sp", lambda e: e.dma_start(out=Wr[:, :, 0:4], in_=I["router_group_w"].rearrange("(c p) n -> p c n", p=128), allow_slow_non_contiguous=True),
                     writes=["Wr"], dma=True)
                P.op("sp", lambda e: e.dma_start(out=Wr[:, :, 4:36], in_=I["router_expert_w"].rearrange("(c p) n -> p c n", p=128), allow_slow_non_contiguous=True),
                     writes=["Wr"], dma=True)
                P.op("sp", lambda e: e.dma_start(out=bfull[:, 0:4], in_=I["router_group_b"].partition_broadcast(128)), writes=["bfull"], dma=True)
                P.op("sp", lambda e: e.dma_start(out=bfull[:, 4:36], in_=I["router_expert_b"].partition_broadcast(128)), writes=["bfull"], dma=True)
                P.op("sp", lambda e: e.dma_start(out=tmpc[:], in_=I["triu"]), writes=["tmpc"], dma=True)
                P.op("dve", lambda e: e.tensor_copy(out=triub[:], in_=tmpc[:]), reads=["tmpc"], writes=["triub"])
                P.op("pool", lambda e: e.memset(onesb[:], 1.0), writes=["onesb"])
                P.op("sp", lambda e: e.dma_start(out=base_b[:], in_=I["ecap"]), writes=["base_b"], dma=True)
                P.op("sp", lambda e: e.dma_start(out=ecapl[:], in_=I["ecap"]), writes=["ecapl"], dma=True)
                P.op("dve", lambda e: e.tensor_scalar(out=ecapl[:], in0=ecapl[:], scalar1=float(CAP), scalar2=None, op0=ALU.add), reads=["ecapl"], writes=["ecapl"])
                P.op("pool", lambda e: e.memset(mh8[:], -0.5), writes=["mh8"])

                def upd_Rb():
                    for hl in range(2):
                        sl = slice(64 * hl, 64 * hl + 64)
                        P.op("act", lambda e, sl=sl, hl=hl: e.activation(out=Rb[sl, :, 64 * hl:64 * hl + 64], in_=R[sl, :, :], func=AF.Copy), reads=["R"], writes=["Rb"])
                upd_Rb()

                def rotary(bank, tab, out1, out2, wtags):
                    pk = pP[:, bank, :].rearrange("p (h d) -> p h d", h=8)
                    cosb = tab[:, 0:32].unsqueeze(1).broadcast_to([128, 8, 32])
                    sinb = tab[:, 32:64].unsqueeze(1).broadcast_to([128, 8, 32])
                    rtag = [f"pP{bank}", "rt"]
                    P.op("dve", lambda e: e.tensor_tensor(out=ta[:], in0=pk[:, :, 0:32], in1=cosb, op=ALU.mult), reads=rtag, writes=["ta"])
                    P.op("dve", lambda e: e.tensor_tensor(out=tb[:], in0=pk[:, :, 32:64], in1=sinb, op=ALU.mult), reads=rtag, writes=["tb"])
                    P.op("pool", lambda e: e.tensor_tensor(out=out1, in0=ta[:], in1=tb[:], op=ALU.subtract), reads=["ta", "tb"], writes=[wtags[0]])
                    P.op("dve", lambda e: e.tensor_tensor(out=ta[:], in0=pk[:, :, 0:32], in1=sinb, op=ALU.mult), reads=rtag, writes=["ta"])
                    P.op("dve", lambda e: e.tensor_tensor(out=tb[:], in0=pk[:, :, 32:64], in1=cosb, op=ALU.mult), reads=rtag, writes=["tb"])
                    P.op("pool", lambda e: e.tensor_tensor(out=out2, in0=ta[:], in1=tb[:], op=ALU.add), reads=["ta", "tb"], writes=[wtags[1]])

                def a0(ti):
                    b4, b = ti % 4, ti % 2
                    rows = slice(ti * 128, (ti + 1) * 128)
                    P.op("sp", lambda e: e.dma_start(out=xt[b4][:], in_=I["x"][rows, :]), writes=[f"xt{b4}"], dma=True)
                    P.op("sp", lambda e: e.dma_start(out=rt[:, b, 0, :], in_=I["rot_q"][:, ti, :]), writes=[f"rt{b}"], dma=True)
                    P.op("sp", lambda e: e.dma_start(out=rt[:, b, 1, :], in_=I["rot_k"][:, ti, :]), writes=[f"rt{b}"], dma=True)
                    P.op("act", lambda e: e.activation(out=xbb[b][:], in_=xt[b4][:], func=AF.Square, accum_out=ssA[:, b:b + 1]), reads=[f"xt{b4}"], writes=[f"xb{b}", f"ssA{b}"])
                    P.op("dve", lambda e: e.tensor_scalar(out=msA[:, b:b + 1], in0=ssA[:, b:b + 1], scalar1=1.0 / D, scalar2=EPS, op0=ALU.mult, op1=ALU.add), reads=[f"ssA{b}"], writes=[f"msA{b}"])
                    P.op("pool", lambda e: e.tensor_tensor(out=rsA[:, b:b + 1], in0=msA[:, b:b + 1], in1=mhalf[:], op=ALU.pow), reads=[f"msA{b}", "mhalf"], writes=[f"rsA{b}"])
                    P.op("act", lambda e: e.activation(out=xbb[b][:], in_=xt[b4][:], func=AF.Copy, scale=rsA[:, b:b + 1]), reads=[f"xt{b4}", f"rsA{b}"], writes=[f"xb{b}"])

                def a1(ti):
                    b = ti % 2
                    for c in range(8):
                        P.op("pe", lambda e, c=c: e.transpose(out=pT[:, c, :], in_=xbb[b][:, c * 128:(c + 1) * 128], identity=identb[:]), reads=[f"xb{b}", "identb"], writes=["pT"])
                    P.op("dve", lambda e: e.tensor_copy(out=xT[:], in_=pT[:]), reads=["pT"], writes=["xT"])

                def proj(blk, bank):
                    for c in range(8):
                        P.op("pe", lambda e, c=c: e.matmul(pP[:, bank, :], lhsT=xT[:, c, :], rhs=Wb2[:, c, blk * 512:(blk + 1) * 512], start=(c == 0), stop=(c == 7)),
                             reads=["xT", "Wb2"], writes=[f"pP{bank}"])

                def a2(ti):
                    b = ti % 2
                    proj(0, 0)
                    proj(1, 1)
                    rtb = rt[:, b, :, :]
                    pk_rt = [f"rt{b}"]
                    pk = pP[:, 0, :].rearrange("p (h d) -> p h d", h=8)
                    def rot(bank, tab, out1, out2, wtags):
                        pk = pP[:, bank, :].rearrange("p (h d) -> p h d", h=8)
                        cosb = tab[:, 0:32].unsqueeze(1).broadcast_to([128, 8, 32])
                        sinb = tab[:, 32:64].unsqueeze(1).broadcast_to([128, 8, 32])
                        rtag = [f"pP{bank}", f"rt{b}"]
                        T = rtmp[bank]
                        tg = [f"rtmp{bank}{i}" for i in range(4)]
                        P.op("dve", lambda e: e.tensor_tensor(out=T[0][:], in0=pk[:, :, 0:32], in1=cosb, op=ALU.mult), reads=rtag, writes=[tg[0]])
                        P.op("dve", lambda e: e.tensor_tensor(out=T[1][:], in0=pk[:, :, 32:64], in1=sinb, op=ALU.mult), reads=rtag, writes=[tg[1]])
                        P.op("dve", lambda e: e.tensor_tensor(out=T[2][:], in0=pk[:, :, 0:32], in1=sinb, op=ALU.mult), reads=rtag, writes=[tg[2]])
                        P.op("dve", lambda e: e.tensor_tensor(out=T[3][:], in0=pk[:, :, 32:64], in1=cosb, op=ALU.mult), reads=rtag, writes=[tg[3]])
                        P.op("pool", lambda e: e.tensor_tensor(out=out1, in0=T[0][:], in1=T[1][:], op=ALU.subtract), reads=[tg[0], tg[1]], writes=[wtags[0]])
                        P.op("pool", lambda e: e.tensor_tensor(out=out2, in0=T[2][:], in1=T[3][:], op=ALU.add), reads=[tg[2], tg[3]], writes=[wtags[1]])
                    rot(0, rt[:, b, 0, :], qrb[:, :, 0:32], qrb[:, :, 32:64], ["qrb1", "qrb2"])
                    rot(1, rt[:, b, 1, :], kr[:, :, 0:32], kr[:, :, 32:64], ["kr1", "kr2"])
                    P.op("act", lambda e: e.activation(out=krb[:], in_=kr[:].rearrange("p h d -> p (h d)"), func=AF.Copy), reads=["kr1", "kr2"], writes=["krb"])
                    P.op("pool", lambda e: e.tensor_tensor(out=ktil[b][:], in0=kr[:].rearrange("p h d -> p (h d)"), in1=kdec[:], op=ALU.mult), reads=["kr1", "kr2", "kdec"], writes=[f"ktil{b}"])

                def a3(ti):
                    b = ti % 2
                    proj(2, 0)
                    proj(3, 1)
                    P.op("act", lambda e: e.activation(out=vb[b][:], in_=pP[:, 0, :], func=AF.Copy), reads=["pP0"], writes=[f"vb{b}"])
                    P.op("act", lambda e: e.activation(out=sg[b][:], in_=pP[:, 1, :], func=AF.Silu), reads=["pP1"], writes=[f"sg{b}"])
                    P.op("pool", lambda e: e.tensor_tensor(out=sg[b][:], in0=sg[b][:], in1=rnw[:], op=ALU.mult), reads=[f"sg{b}", "rnw"], writes=[f"sg{b}"])

                def a4(ti):
                    b = ti % 2
                    for j in range(4):
                        P.op("pe", lambda e, j=j: e.transpose(out=pT[:, j, :], in_=qrb[:].rearrange("p h d -> p (h d)")[:, j * 128:(j + 1) * 128], identity=identb[:]),
                             reads=["qrb1", "qrb2", "identb"], writes=["pT"])
                    for j in range(4):
                        P.op("pe", lambda e, j=j: e.transpose(out=pT[:, 4 + j, :], in_=krb[:, j * 128:(j + 1) * 128], identity=identb[:]), reads=["krb", "identb"], writes=["pT"])
                    P.op("act", lambda e: e.activation(out=qT[b][:], in_=pT[:, 0:4, :], func=AF.Copy), reads=["pT"], writes=[f"qT{b}"])
                    P.op("act", lambda e: e.activation(out=kT[b][:], in_=pT[:, 4:8, :], func=AF.Copy), reads=["pT"], writes=[f"kT{b}"])
                    P.op("pool", lambda e: e.tensor_tensor(out=qTd[b][:], in0=qT[b][:], in1=qdec[:], op=ALU.mult), reads=[f"qT{b}", "qdec"], writes=[f"qTd{b}"])

                def b1(ti):
                    b = ti % 2
                    for h in range(8):
                        j, hl = h // 2, h % 2
                        sl = slice(64 * hl, 64 * hl + 64)
                        P.op("pe", lambda e, j=j, hl=hl, sl=sl: e.matmul(pS[:, hl, j * 128:(j + 1) * 128], lhsT=kT[b][sl, j, :], rhs=qT[b][sl, j, :], start=True, stop=True),
                             reads=[f"kT{b}", f"qT{b}"], writes=[f"pS{hl}"])
                    for hl in range(2):
                        P.op("dve", lambda e, hl=hl: e.tensor_tensor(out=PT[:].rearrange("p (j l) t -> p j l t", l=2)[:, :, hl, :],
                                                                     in0=pS[:, hl, :].rearrange("p (j t) -> p j t", j=4),
                                                                     in1=decT[:].rearrange("p (j l) t -> p j l t", l=2)[:, :, hl, :], op=ALU.mult),
                             reads=[f"pS{hl}", "decT"], writes=[f"PT{hl}"])

                def b2(ti):
                    b = ti % 2
                    for h in range(8):
                        P.op("pe", lambda e, h=h: e.matmul(pO[:, h * 64:(h + 1) * 64], lhsT=PT[:, h, :], rhs=vb[b][:, h * 64:(h + 1) * 64], start=(h == 0), stop=False,
                                                           skip_group_check=True), reads=["PT0", "PT1", f"vb{b}"], writes=["pO"])
                    for j in range(4):
                        P.op("pe", lambda e, j=j: e.matmul(pO[:, j * 128:(j + 1) * 128], lhsT=qTd[b][:, j, :], rhs=Rb[:, j, :], start=False, stop=True,
                                                           skip_group_check=True), reads=[f"qTd{b}", "Rb"], writes=["pO"])
                    import os
                    SUB = int(os.environ.get("P2SUB", "9"))
                    if SUB < 1:
                        return
                    pkv = pK[:, 0:256].rearrange("p (j e) -> p j e", j=4)
                    for h in range(8):
                        j, hl = h // 2, h % 2
                        P.op("pe", lambda e, h=h, j=j, hl=hl: e.matmul(pkv[64 * hl:64 * hl + 64, j, :], lhsT=ktil[b][:, h * 64:(h + 1) * 64], rhs=vb[b][:, h * 64:(h + 1) * 64],
                                                                       start=True, stop=True), reads=[f"ktil{b}", f"vb{b}"], writes=["pKkv"])
                    for j in range(4):
                        P.op("dve", lambda e, j=j: e.scalar_tensor_tensor(out=R[:, j, :], in0=R[:, j, :], scalar=gam128[:, j:j + 1], in1=pkv[:, j, :], op0=ALU.mult, op1=ALU.add),
                             reads=["pKkv", "gam128", "R"], writes=["R"])
                    if SUB < 2:
                        return
                    upd_Rb()
                    if SUB < 3:
                        return
                    o3 = pO[:].rearrange("p (h d) -> p h d", h=8)
                    P.op("dve", lambda e: e.tensor_reduce(out=s1[:], in_=o3, axis=AX.X, op=ALU.add), reads=["pO"], writes=["s1"])
                    P.op("act", lambda e: e.activation(out=osq[:], in_=pO[:], func=AF.Square), reads=["pO"], writes=["osq"])
                    P.op("dve", lambda e: e.tensor_reduce(out=s2[:], in_=osq[:].rearrange("p (h d) -> p h d", h=8), axis=AX.X, op=ALU.add), reads=["osq"], writes=["s2"])
                    P.op("dve", lambda e: e.tensor_scalar(out=mean[:], in0=s1[:], scalar1=1.0 / 64, scalar2=None, op0=ALU.mult), reads=["s1"], writes=["mean"])
                    P.op("dve", lambda e: e.tensor_tensor(out=var[:], in0=mean[:], in1=mean[:], op=ALU.mult), reads=["mean"], writes=["var"])
                    P.op("dve", lambda e: e.scalar_tensor_tensor(out=var[:], in0=s2[:], scalar=1.0 / 64, in1=var[:], op0=ALU.mult, op1=ALU.subtract), reads=["s2", "var"], writes=["var"])
                    P.op("dve", lambda e: e.tensor_scalar(out=var[:], in0=var[:], scalar1=EPS, scalar2=None, op0=ALU.add), reads=["var"], writes=["var"])
                    P.op("pool", lambda e: e.tensor_tensor(out=rstd[:], in0=var[:], in1=mh8[:], op=ALU.pow), reads=["var", "mh8"], writes=["rstd"])
                    P.op("dve", lambda e: e.tensor_tensor(out=oc[:], in0=o3, in1=mean[:].unsqueeze(2).broadcast_to([128, 8, 64]), op=ALU.subtract), reads=["pO", "mean"], writes=["oc"])
                    P.op("dve", lambda e: e.tensor_tensor(out=oc[:], in0=oc[:], in1=rstd[:].unsqueeze(2).broadcast_to([128, 8, 64]), op=ALU.mult), reads=["oc", "rstd"], writes=["oc"])
                    P.op("dve", lambda e: e.tensor_tensor(out=yret[:], in0=oc[:].rearrange("p h d -> p (h d)"), in1=sg[b][:], op=ALU.mult), reads=["oc", f"sg{b}"], writes=["yret"])

                def b3(ti):
                    b = ti % 2
                    for j in range(4):
                        P.op("pe", lambda e, j=j: e.transpose(out=pT[:, j, :], in_=yret[:, j * 128:(j + 1) * 128], identity=identb[:]), reads=["yret", "identb"], writes=["pT"])
                    P.op("act", lambda e: e.activation(out=yretT[b][:], in_=pT[:, 0:4, :], func=AF.Copy), reads=["pT"], writes=[f"yretT{b}"])

                def c1(ti):
                    b3_, b = ti % 4, ti % 2
                    rows = slice(ti * 128, (ti + 1) * 128)
                    for nh in range(2):
                        cs = slice(nh * 512, (nh + 1) * 512)
                        for c in range(4):
                            P.op("pe", lambda e, c=c, cs=cs: e.matmul(pH[:], lhsT=mixT[:, c, rows], rhs=Wo[:, c, cs], start=(c == 0), stop=False), reads=["Wo"], writes=["pH"])
                        for c in range(4):
                            P.op("pe", lambda e, c=c, cs=cs: e.matmul(pH[:], lhsT=yretT[b][:, c, :], rhs=Wo[:, 4 + c, cs], start=False, stop=(c == 3)),
                                 reads=["Wo", f"yretT{b}"], writes=["pH"])
                        P.op("dve", lambda e, cs=cs: e.tensor_tensor(out=hh[:, cs], in0=pH[:], in1=xt[b3_][:, cs], op=ALU.add), reads=["pH", f"xt{b3_}"], writes=[f"hh{nh}"])
                    P.op("sp", lambda e: e.dma_start(out=d_hbuf[rows, :], in_=hh[:]), reads=["hh0", "hh1"], writes=[f"d_hbuf{ti}"], dma=True)
                    if dbg == "h":
                        P.op("sp", lambda e: e.dma_start(out=dbg_out[rows, :], in_=hh[:]), reads=["hh0", "hh1"], writes=[f"dbg{ti}"], dma=True)
                    P.op("act", lambda e: e.activation(out=hn[:], in_=hh[:], func=AF.Square, accum_out=ssC[:]), reads=["hh0", "hh1"], writes=["hn", "ssC"])
                    P.op("dve", lambda e: e.tensor_scalar(out=msC[:], in0=ssC[:], scalar1=1.0 / D, scalar2=EPS, op0=ALU.mult, op1=ALU.add), reads=["ssC"], writes=["msC"])
                    P.op("pool", lambda e: e.tensor_tensor(out=rsC[:], in0=msC[:], in1=mhalf[:], op=ALU.pow), reads=["msC", "mhalf"], writes=["rsC"])
                    P.op("dve", lambda e: e.scalar_tensor_tensor(out=hn[:], in0=hh[:], scalar=rsC[:], in1=n2w[:], op0=ALU.mult, op1=ALU.mult), reads=["hh0", "hh1", "rsC", "n2w"], writes=["hn"])
                    P.op("act", lambda e: e.activation(out=hnb[b][:], in_=hn[:], func=AF.Copy), reads=["hn"], writes=[f"hnb{b}"])

                def c2(ti):
                    banks = [(pH, "pH"), (pO, "pO")]
                    for half in range(2):
                        bk, btag = banks[half]
                        bk3 = bk[:].rearrange("p (c t) -> p c t", c=4)
                        for c in range(4):
                            cc = 4 * half + c
                            P.op("pe", lambda e, c=c, cc=cc, bk3=bk3: e.transpose(out=bk3[:, c, :], in_=hn[:, cc * 128:(cc + 1) * 128], identity=identf[:]), reads=["hn", "identf"], writes=[btag])
                    for half in range(2):
                        bk, btag = banks[half]
                        bk3 = bk[:].rearrange("p (c t) -> p c t", c=4)
                        P.op("act", lambda e, half=half, bk3=bk3: e.activation(out=hnT[:, 4 * half:4 * half + 4, :], in_=bk3, func=AF.Copy), reads=[btag], writes=[f"hnT{half}"])

                def c3(ti):
                    b = ti % 2
                    for c in range(8):
                        P.op("pe", lambda e, c=c: e.matmul(pH[:, 0:36], lhsT=hnT[:, c, :], rhs=Wr[:, c, :], start=(c == 0), stop=(c == 7)), reads=["hnT0", "hnT1", "Wr"], writes=["pH"])
                    P.op("dve", lambda e: e.tensor_tensor(out=lg[:], in0=pH[:, 0:36], in1=bfull[:], op=ALU.add), reads=["pH", "bfull"], writes=["lg"])
                    c_ = lambda i: sm[:, i:i + 1]
                    import os
                    C3SUB = int(os.environ.get("C3SUB", "9"))
                    if C3SUB < 2:
                        return
                    P.op("dve", lambda e: e.tensor_reduce(out=c_(0), in_=lg[:, 0:4], axis=AX.X, op=ALU.max), reads=["lg"], writes=["sm0"])
                    P.op("dve", lambda e: e.tensor_scalar(out=goh[:], in0=lg[:, 0:4], scalar1=c_(0), scalar2=None, op0=ALU.is_equal), reads=["lg", "sm0"], writes=["goh"])
                    P.op("dve", lambda e: e.tensor_scalar(out=pen[:], in0=goh[:], scalar1=BIG, scalar2=-BIG, op0=ALU.mult, op1=ALU.add), reads=["goh"], writes=["pen"])
                    P.op("dve", lambda e: e.tensor_tensor(out=em[:].rearrange("p (g k) -> p g k", g=4), in0=lg[:, 4:36].rearrange("p (g k) -> p g k", g=4),
                                                          in1=pen[:].unsqueeze(2).broadcast_to([128, 4, 8]), op=ALU.add), reads=["lg", "pen"], writes=["em"])
                    P.op("dve", lambda e: e.tensor_reduce(out=c_(4), in_=em[:], axis=AX.X, op=ALU.max), reads=["em"], writes=["sm4"])
                    P.op("dve", lambda e: e.tensor_scalar(out=oh1, in0=em[:], scalar1=c_(4), scalar2=None, op0=ALU.is_equal), reads=["em", "sm4"], writes=["oh1"])
                    P.op("dve", lambda e: e.scalar_tensor_tensor(out=em2[:], in0=oh1, scalar=-BIG, in1=em[:], op0=ALU.mult, op1=ALU.add), reads=["oh1", "em"], writes=["em2"])
                    P.op("dve", lambda e: e.tensor_reduce(out=c_(5), in_=em2[:], axis=AX.X, op=ALU.max), reads=["em2"], writes=["sm5"])
                    P.op("dve", lambda e: e.tensor_scalar(out=oh2, in0=em2[:], scalar1=c_(5), scalar2=None, op0=ALU.is_equal), reads=["em2", "sm5"], writes=["oh2"])
                    P.op("dve", lambda e: e.tensor_scalar(out=ex5[:, 0:4], in0=lg[:, 0:4], scalar1=c_(0), scalar2=None, op0=ALU.subtract), reads=["lg", "sm0"], writes=["ex5a"])
                    P.op("dve", lambda e: e.tensor_tensor(out=ex5[:, 4:5], in0=c_(5), in1=c_(4), op=ALU.subtract), reads=["sm4", "sm5"], writes=["ex5b"])
                    P.op("act", lambda e: e.activation(out=ex5[:], in_=ex5[:], func=AF.Sigmoid), reads=["ex5a", "ex5b"], writes=["ex5s"])
                    P.op("dve", lambda e: e.tensor_scalar(out=ex5t[:], in0=ex5[:], scalar1=-1.0, scalar2=1.0, op0=ALU.mult, op1=ALU.add), reads=["ex5s"], writes=["ex5t"])
                    P.op("dve", lambda e: e.reciprocal(out=ex5t[:], in_=ex5t[:]), reads=["ex5t"], writes=["ex5t"])
                    P.op("dve", lambda e: e.tensor_tensor(out=ex5[:], in0=ex5[:], in1=ex5t[:], op=ALU.mult), reads=["ex5s", "ex5t"], writes=["ex5"])
                    P.op("dve", lambda e: e.tensor_reduce(out=c_(2), in_=ex5[:, 0:4], axis=AX.X, op=ALU.add), reads=["ex5"], writes=["sm2"])
                    P.op("dve", lambda e: e.reciprocal(out=c_(3), in_=c_(2)), reads=["sm2"], writes=["sm3"])
                    P.op("dve", lambda e: e.tensor_scalar(out=c_(8), in0=ex5[:, 4:5], scalar1=1.0, scalar2=None, op0=ALU.add), reads=["ex5"], writes=["sm8"])
                    P.op("dve", lambda e: e.reciprocal(out=c_(8), in_=c_(8)), reads=["sm8"], writes=["sm8"])
                    P.op("dve", lambda e: e.tensor_tensor(out=c_(9), in0=ex5[:, 4:5], in1=c_(8), op=ALU.mult), reads=["ex5", "sm8"], writes=["sm9"])
                    P.op("dve", lambda e: e.tensor_tensor(out=maskb[:], in0=oh1, in1=oh2, op=ALU.add), reads=["oh1", "oh2"], writes=["maskb"])

                def dstage(ti):
                    b = ti % 2
                    c_ = lambda i: sm[:, i:i + 1]
                    P.op("pe", lambda e: e.matmul(pH[:, 64:96], lhsT=triub[:], rhs=maskb[:], start=True, stop=True), reads=["triub", "maskb", "lg"], writes=["pH", "pHp"])
                    P.op("pe", lambda e: e.matmul(pH[:, 128:160], lhsT=onesb[:], rhs=maskb[:], start=True, stop=True), reads=["onesb", "maskb", "lg"], writes=["pH", "pHc"])
                    P.op("dve", lambda e: e.tensor_tensor(out=posf[:], in0=pH[:, 64:96], in1=base_b[:], op=ALU.add), reads=["pHp", "base_b"], writes=["posf"])
                    P.op("dve", lambda e: e.tensor_tensor(out=base_b[:], in0=pH[:, 128:160], in1=base_b[:], op=ALU.add), reads=["pHc", "base_b", "posf"], writes=["base_b"])
                    P.op("pool", lambda e: e.memset(ss_dummy[:], 0.0), reads=["posf", "base_b"], writes=["pH"])
                    P.op("dve", lambda e: e.tensor_tensor(out=okm[:], in0=posf[:], in1=ecapl[:], op=ALU.is_lt), reads=["posf", "ecapl"], writes=["okm"])
                    P.op("dve", lambda e: e.tensor_tensor(out=t2[:], in0=oh12[:], in1=posf[:].unsqueeze(1).broadcast_to([128, 2, 32]), op=ALU.mult), reads=["oh1", "oh2", "posf"], writes=["t2"])
                    P.op("dve", lambda e: e.tensor_reduce(out=d2[:], in_=t2[:], axis=AX.X, op=ALU.add), reads=["t2"], writes=["d2"])
                    P.op("dve", lambda e: e.tensor_tensor(out=t2[:], in0=oh12[:], in1=okm[:].unsqueeze(1).broadcast_to([128, 2, 32]), op=ALU.mult), reads=["oh1", "oh2", "okm", "d2"], writes=["t2"])
                    P.op("dve", lambda e: e.tensor_reduce(out=ok2[:], in_=t2[:], axis=AX.X, op=ALU.add), reads=["t2"], writes=["ok2"])
                    P.op("dve", lambda e: e.scalar_tensor_tensor(out=gates[:, ti, :], in0=sm[:, 8:10], scalar=c_(3), in1=ok2[:], op0=ALU.mult, op1=ALU.mult),
                         reads=["sm8", "sm9", "sm3", "ok2"], writes=[f"gates{ti}"])
                    P.op("dve", lambda e: e.tensor_tensor(out=dstf[:], in0=d2[:], in1=ok2[:], op=ALU.mult), reads=["d2", "ok2"], writes=["dstf"])
                    P.op("dve", lambda e: e.tensor_scalar(out=pad2[:], in0=ok2[:], scalar1=-1.0e6, scalar2=1.0e6, op0=ALU.mult, op1=ALU.add), reads=["ok2"], writes=["pad2"])
                    P.op("dve", lambda e: e.tensor_tensor(out=dstf[:], in0=dstf[:], in1=pad2[:], op=ALU.add), reads=["dstf", "pad2"], writes=["dstf"])
                    P.op("dve", lambda e: e.tensor_copy(out=dests[:, ti, :], in_=dstf[:]), reads=["dstf"], writes=[f"dests{ti}"])
                    import os
                    for k_ in range(2 if os.environ.get("NOSCAT") is None else 0):
                        P.op("pool", lambda e, k_=k_: e.indirect_dma_start(
                            out=d_xbuf, out_offset=bass.IndirectOffsetOnAxis(ap=dests[:, ti, k_:k_ + 1].bitcast(U32), axis=0),
                            in_=hnb[b][:], in_offset=None, bounds_check=bc_reg(e), oob_is_err=False), reads=[f"dests{ti}", f"hnb{b}"], writes=[f"d_xbuf{ti}_{k_}"], dma=True)

                nt2 = p2tiles if p2tiles else ntile
                import os
                STG = os.environ.get("P2STAGES", "a1,a2,a3,a4,b1,b2,b3,c1,c2,c3").split(",")
                _fn = dict(a1=a1, a2=a2, a3=a3, a4=a4, b1=b1, b2=b2, b3=b3, c1=c1, c2=c2, c3=c3)
                def wrap(name):
                    f = _fn[name]
                    return f if name in STG else (lambda t: None)
                a1, a2, a3, a4, b1, b2, b3, c1, c2, c3 = [wrap(n) for n in ("a1", "a2", "a3", "a4", "b1", "b2", "b3", "c1", "c2", "c3")]
                ok = lambda t: 0 <= t < nt2
                if "a1" in STG:
                    a0(0)
                for step in range(nt2 + 4):
                    tA, tB, tC, tD = step, step - 1, step - 2, step - 3
                    if ok(tA - 1): a4(tA - 1)
                    if ok(tC): b3(tC)
                    if ok(tA): a1(tA)
                    if ok(tB): b1(tB)
                    if ok(tD) and dbg != "h": c3(tD)
                    if ok(tA + 1) and "a1" in STG: a0(tA + 1)
                    if ok(tA): a2(tA)
                    if ok(tC): c1(tC)
                    if ok(tA): a3(tA)
                    if ok(tB): b2(tB)
                    if ok(tC) and dbg != "h": c2(tC)
                    if ok(tD) and dbg != "h" and "c3" in STG: dstage(tD)
                P.emit()
            if dbg in ("h", "p2"):
                return nc
        with ExitStack() as st:
            sb, ps = mk(st)
            Wgu = [sb(f"Wgu{i}", [128, 8, 1024], BF16) for i in range(2)]
            Wd = [sb(f"Wd{i}", [128, 4, D], BF16) for i in range(2)]
            xg = [sb(f"xg{i}", [128, 3, D], BF16) for i in range(2)]
            XT = [sb(f"XT{i}", [128, 8, CAP], BF16) for i in range(2)]
            hidT = sb("hidT", [128, 4, CAP], BF16)
            sil = [sb(f"sil{i}", [128, CAP]) for i in range(2)]
            yo = [sb(f"yo{i}", [128, 512], BF16) for i in range(4)]
            NSTG = 8
            stg = [sb(f"stg{i}", [128, 2048]) for i in range(NSTG)]
            pX = ps("pX", [128, 8, 128], BF16)
            pG = [ps(f"pG{i}", [128, 512]) for i in range(2)]
            pU = [ps(f"pUp{i}", [128, 512]) for i in range(2)]
            pDn = ps("pDn", [128, 3, 512])
            stg_i = [0]
            cast_i = [0]

            def wload(ex):
                b = ex % 2
                casts = []
                specs = []
                for (wname, off) in (("moe_w_gate", 0), ("moe_w_up", 512)):
                    for hc in range(2):
                        specs.append((wname, hc, 4, 512, lambda b=b, hc=hc, off=off: Wgu[b][:, 4 * hc:4 * hc + 4, off:off + 512], f"Wgu{b}"))
                for hc in range(2):
                    specs.append(("moe_w_down", hc, 2, 256, lambda b=b, hc=hc: Wd[b][:, 2 * hc:2 * hc + 2, :], f"Wd{b}"))
                for (wname, hc, cc, rows_, dstf, wtag) in specs:
                    sbi = stg_i[0] % NSTG
                    stg_i[0] += 1
                    P.op("sp", lambda e, ex=ex, wname=wname, hc=hc, sbi=sbi, cc=cc, rows_=rows_: e.dma_start(
                        out=stg[sbi][:].rearrange("p (c n) -> p c n", c=cc), in_=I[wname][ex, hc * rows_:(hc + 1) * rows_, :].rearrange("(c p) n -> p c n", p=128)),
                        writes=[f"stg{sbi}"], dma=True)

                    def cast(sbi=sbi, cc=cc, dstf=dstf, wtag=wtag):
                        ceng = ("act", "dve")[cast_i[0] % 2]
                        cast_i[0] += 1
                        srcv = stg[sbi][:].rearrange("p (c n) -> p c n", c=cc)
                        if ceng == "act":
                            P.op("act", lambda e: e.activation(out=dstf(), in_=srcv, func=AF.Copy), reads=[f"stg{sbi}"], writes=[wtag])
                        else:
                            P.op("dve", lambda e: e.tensor_copy(out=dstf(), in_=srcv), reads=[f"stg{sbi}"], writes=[wtag])
                    casts.append(cast)
                P.op("sp", lambda e, ex=ex, b=b: e.dma_start(out=xg[b][:], in_=d_xbuf[ex * CAP:(ex + 1) * CAP, :].rearrange("(s p) d -> p s d", p=128)), writes=[f"xg{b}"], dma=True)
                return casts

            def transposes(ex):
                b = ex % 2
                for s_ in range(3):
                    for c in range(8):
                        P.op("pe", lambda e, b=b, s_=s_, c=c: e.transpose(out=pX[:, c, :], in_=xg[b][:, s_, c * 128:(c + 1) * 128], identity=identb[:]), reads=[f"xg{b}", "identb"], writes=["pX"])
                    if s_ % 2 == 0:
                        P.op("dve", lambda e, s_=s_, b=b: e.tensor_copy(out=XT[b][:, :, s_ * 128:(s_ + 1) * 128], in_=pX[:]), reads=["pX"], writes=[f"XT{b}"])
                    else:
                        P.op("act", lambda e, s_=s_, b=b: e.activation(out=XT[b][:, :, s_ * 128:(s_ + 1) * 128], in_=pX[:], func=AF.Copy), reads=["pX"], writes=[f"XT{b}"])

            pending = wload(0)
            for cst in pending:
                cst()
            transposes(0)
            yo_i = [0]
            for ex in range(NEXP):
                b = ex % 2
                pending = wload(ex + 1) if ex + 1 < NEXP else []
                for m in range(4):
                    mb = m % 2
                    for c in range(8):
                        P.op("pe", lambda e, b=b, m=m, c=c, mb=mb: e.matmul(pG[mb][:, 0:CAP], lhsT=Wgu[b][:, c, m * 128:(m + 1) * 128], rhs=XT[b][:, c, :], start=(c == 0), stop=(c == 7)),
                             reads=[f"Wgu{b}", f"XT{b}"], writes=[f"pG{mb}"])
                    for c in range(8):
                        P.op("pe", lambda e, b=b, m=m, c=c, mb=mb: e.matmul(pU[mb][:, 0:CAP], lhsT=Wgu[b][:, c, 512 + m * 128:512 + (m + 1) * 128], rhs=XT[b][:, c, :], start=(c == 0), stop=(c == 7)),
                             reads=[f"Wgu{b}", f"XT{b}"], writes=[f"pUp{mb}"])
                    P.op("act", lambda e, mb=mb: e.activation(out=sil[mb][:], in_=pG[mb][:, 0:CAP], func=AF.Silu), reads=[f"pG{mb}"], writes=[f"sil{mb}"])
                    P.op("dve", lambda e, mb=mb, m=m: e.tensor_tensor(out=hidT[:, m, :], in0=pU[mb][:, 0:CAP], in1=sil[mb][:], op=ALU.mult), reads=[f"pUp{mb}", f"sil{mb}"], writes=["hidT"])
                    for cst in pending[m:m + 1] + (pending[4:6] if m == 3 else []):
                        cst()
                if ex + 1 < NEXP:
                    transposes(ex + 1)
                for s_ in range(3):
                    for nh in range(2):
                        bk = (2 * s_ + nh) % 3
                        for m in range(4):
                            P.op("pe", lambda e, b=b, s_=s_, nh=nh, m=m, bk=bk: e.matmul(pDn[:, bk, :], lhsT=hidT[:, m, s_ * 128:(s_ + 1) * 128], rhs=Wd[b][:, m, nh * 512:(nh + 1) * 512],
                                                                                        start=(m == 0), stop=(m == 3)), reads=["hidT", f"Wd{b}"], writes=[f"pDn{bk}"])
                        yb = yo_i[0] % 4
                        yo_i[0] += 1
                        if yb % 2 == 0:
                            P.op("act", lambda e, yb=yb, bk=bk: e.activation(out=yo[yb][:], in_=pDn[:, bk, :], func=AF.Copy), reads=[f"pDn{bk}"], writes=[f"yo{yb}"])
                        else:
                            P.op("dve", lambda e, yb=yb, bk=bk: e.tensor_copy(out=yo[yb][:], in_=pDn[:, bk, :]), reads=[f"pDn{bk}"], writes=[f"yo{yb}"])
                        r0 = ex * CAP + s_ * 128
                        P.op("act", lambda e, yb=yb, r0=r0, nh=nh: e.dma_start(out=d_ybuf[r0:r0 + 128, nh * 512:(nh + 1) * 512], in_=yo[yb][:]), reads=[f"yo{yb}"], writes=[f"d_ybuf{r0}_{nh}"], dma=True)
            P.emit()
        with ExitStack() as st:
            sb, ps = mk(st)
            NB4 = 4
            hh = [sb(f"hh{i}", [128, D]) for i in range(NB4)]
            y0 = [sb(f"y0{i}", [128, D], BF16) for i in range(NB4)]
            y1 = [sb(f"y1{i}", [128, D], BF16) for i in range(NB4)]
            ob = [sb(f"ob{i}", [128, D]) for i in range(NB4)]
            fnw = sb("fnw", [128, D])
            ss = sb("ss", [128, 1]); ms = sb("ms", [128, 1]); rs = sb("rs", [128, 1])
            P.op("sp", lambda e: e.dma_start(out=fnw[:], in_=I["final_norm_w"].partition_broadcast(128)), writes=["fnw"], dma=True)
            for i in range(NB4):
                P.op("pool", lambda e, i=i: e.memset(y0[i][:], 0.0), writes=[f"y0{i}"])
                P.op("pool", lambda e, i=i: e.memset(y1[i][:], 0.0), writes=[f"y1{i}"])
            def loads4(ti):
                b = ti % NB4
                rows = slice(ti * 128, (ti + 1) * 128)
                P.op("sp", lambda e, b=b, rows=rows: e.dma_start(out=hh[b][:], in_=d_hbuf[rows, :]), writes=[f"hh{b}"], dma=True)
                for k_, yy in enumerate((y0, y1)):
                    P.op("pool", lambda e, k_=k_, yy=yy, b=b, ti=ti: e.indirect_dma_start(
                        out=yy[b][:], out_offset=None, in_=d_ybuf, in_offset=bass.IndirectOffsetOnAxis(ap=dests[:, ti, k_:k_ + 1].bitcast(U32), axis=0),
                        bounds_check=bc_reg(e), oob_is_err=False), writes=[f"y{k_}{b}"], dma=True)

            for ti in range(min(NB4 - 1, ntile)):
                loads4(ti)
            for ti in range(ntile):
                b = ti % NB4
                rows = slice(ti * 128, (ti + 1) * 128)
                if ti + NB4 - 1 < ntile:
                    loads4(ti + NB4 - 1)
                P.op("dve", lambda e, b=b, ti=ti: e.scalar_tensor_tensor(out=hh[b][:], in0=y0[b][:], scalar=gates[:, ti, 0:1], in1=hh[b][:], op0=ALU.mult, op1=ALU.add),
                     reads=[f"y0{b}", f"hh{b}"], writes=[f"hh{b}"])
                P.op("dve", lambda e, b=b, ti=ti: e.scalar_tensor_tensor(out=hh[b][:], in0=y1[b][:], scalar=gates[:, ti, 1:2], in1=hh[b][:], op0=ALU.mult, op1=ALU.add),
                     reads=[f"y1{b}", f"hh{b}"], writes=[f"hh{b}"])
                P.op("act", lambda e, b=b: e.activation(out=ob[b][:], in_=hh[b][:], func=AF.Square, accum_out=ss[:]), reads=[f"hh{b}"], writes=[f"ob{b}", "ss"])
                P.op("dve", lambda e: e.tensor_scalar(out=ms[:], in0=ss[:], scalar1=1.0 / D, scalar2=EPS, op0=ALU.mult, op1=ALU.add), reads=["ss"], writes=["ms"])
                P.op("pool", lambda e: e.tensor_tensor(out=rs[:], in0=ms[:], in1=mhalf[:], op=ALU.pow), reads=["ms", "mhalf"], writes=["rs"])
                P.op("dve", lambda e, b=b: e.scalar_tensor_tensor(out=ob[b][:], in0=hh[b][:], scalar=rs[:], in1=fnw[:], op0=ALU.mult, op1=ALU.mult), reads=[f"hh{b}", "rs", "fnw"], writes=[f"ob{b}"])
                P.op("sp", lambda e, b=b, rows=rows: e.dma_start(out=out[rows, :], in_=ob[b][:]), reads=[f"ob{b}"], writes=["out"], dma=True)
            P.emit()
    return nc


def make_in_maps(inputs):
    x = np.asarray(inputs["x"], dtype=np.float32)
    params = {}
    for k in PARAM_SHAPES:
        a = np.asarray(inputs[k], dtype=np.float32)
        if k != "final_norm_w":
            a = a[0]
        params[k] = np.ascontiguousarray(a)
    consts = [host_consts(0), host_consts(1)]
    zeros = np.zeros((TOK, D), np.float32)
    in_maps = []
    for c in range(NCORES):
        b, half = c // 2, c % 2
        m = {"x": np.ascontiguousarray(x[b, half * TOK:(half + 1) * TOK]),
             "xpre": np.ascontiguousarray(x[b, 0:TOK]) if half == 1 else zeros}
        m.update(params)
        m.update(consts[half])
        in_maps.append(m)
    return in_maps


def kernel(**inputs):
    in_maps = make_in_maps(inputs)
    nc = build_nc()
    res = run_bass_kernel_spmd(nc, in_maps, core_ids=list(range(NCORES)))
    out = np.zeros((4, 2 * TOK, D), np.float32)
    for c in range(NCORES):
        b, half = c // 2, c % 2
        out[b, half * TOK:(half + 1) * TOK] = res.results[c]["out"]
    return out
```
